# Optimizing a Trainium2 kernel written in Bass

```python
import math
import jax, jax.numpy as jnp
from jax import lax
import numpy as np

D_MODEL = 1024
BATCH = 8
SEQ = 4096
DEPTH = 4
DEC_BATCH = 2
DEC_SEQ = 8192
PAST_LEN = 128

GRID_W = 64
Q_BLOCK = 128
DIFF_HEADS = 8
DIFF_HEAD_DIM = 64
DIFF_ROT_DIM = DIFF_HEAD_DIM // 4
ROPE_THETA = 500000.0
GQA_Q_HEADS = 16
GQA_KV_HEADS = 4
GQA_HEAD_DIM = 64
AXIAL_THETA = 10000.0
D_FF_DENSE = 2816
N_EXPERTS = 8
TOP_K = 2
D_FF_EXPERT = 3584
NORM_EPS = 1e-6
N_DENSE = (DEPTH + 1) // 2
N_MOE = DEPTH // 2
N_MOD = 6

DIFF_QK_W = DIFF_HEADS * 2 * DIFF_HEAD_DIM
DIFF_V_W = DIFF_HEADS * 2 * DIFF_HEAD_DIM
GQA_Q_W = GQA_Q_HEADS * GQA_HEAD_DIM
GQA_KV_W = GQA_KV_HEADS * GQA_HEAD_DIM
GATE_W = 2 * D_MODEL
IN_SPLITS = [DIFF_QK_W,
             2 * DIFF_QK_W,
             2 * DIFF_QK_W + DIFF_V_W,
             2 * DIFF_QK_W + DIFF_V_W + GQA_Q_W,
             2 * DIFF_QK_W + DIFF_V_W + GQA_Q_W + GQA_KV_W,
             2 * DIFF_QK_W + DIFF_V_W + GQA_Q_W + 2 * GQA_KV_W]
IN_WIDTH = 2 * DIFF_QK_W + DIFF_V_W + GQA_Q_W + 2 * GQA_KV_W + GATE_W

kernel_name = "hybrid_diffattn_axialgqa_moe_encoder"


def rmsnorm(x, g):
    xf = x.astype(jnp.float32)
    y = xf * lax.rsqrt(jnp.mean(xf * xf, axis=-1, keepdims=True) + NORM_EPS)
    return (y * g.astype(jnp.float32)).astype(x.dtype)


def apply_rope(x, pos, theta):
    half = x.shape[-1] // 2
    inv_freq = jnp.exp(-math.log(theta) * jnp.arange(half, dtype=jnp.float32) / half)
    ang = pos.astype(jnp.float32)[:, None] * inv_freq[None, :]
    bshape = (1, pos.shape[0]) + (1,) * (x.ndim - 3) + (half,)
    cos = jnp.cos(ang).reshape(bshape).astype(x.dtype)
    sin = jnp.sin(ang).reshape(bshape).astype(x.dtype)
    x1, x2 = x[..., :half], x[..., half:]
    return jnp.concatenate([x1 * cos - x2 * sin, x2 * cos + x1 * sin], axis=-1)


def diff_attention(q, k, v, lam):
    B, S, H, _, d = q.shape
    qb = jnp.moveaxis(q.reshape(B, S // Q_BLOCK, Q_BLOCK, H, 2, d), 1, 0)
    scale = d ** -0.5

    def block(qi):
        s = jnp.einsum('bqhmd,bkhmd->bhmqk', qi, k, preferred_element_type=jnp.float32) * scale
        p = jax.nn.softmax(s, axis=-1)
        a = (p[:, :, 0] - lam * p[:, :, 1]).astype(v.dtype)
        return jnp.einsum('bhqk,bkhe->bqhe', a, v)

    o = lax.map(block, qb)
    return jnp.moveaxis(o, 0, 1).reshape(B, S, H, v.shape[-1])


def gqa_attention(q, k, v):
    B, S, G, R, d = q.shape
    qb = jnp.moveaxis(q.reshape(B, S // Q_BLOCK, Q_BLOCK, G, R, d), 1, 0)
    scale = d ** -0.5

    def block(qi):
        s = jnp.einsum('bqgrd,bkgd->bgrqk', qi, k, preferred_element_type=jnp.float32) * scale
        p = jax.nn.softmax(s, axis=-1).astype(v.dtype)
        return jnp.einsum('bgrqk,bkgd->bqgrd', p, v)

    o = lax.map(block, qb)
    return jnp.moveaxis(o, 0, 1).reshape(B, S, G * R * d)


def mixing(h, pos, row, col, lam_init, w_in, diff_lambda, diff_subln, gqa_q_norm, gqa_k_norm, w_out):
    B, S, _ = h.shape
    proj = h @ w_in
    dq, dk, dv, gq, gk, gv, gl = jnp.split(proj, IN_SPLITS, axis=-1)

    dq = dq.reshape(B, S, DIFF_HEADS, 2, DIFF_HEAD_DIM)
    dk = dk.reshape(B, S, DIFF_HEADS, 2, DIFF_HEAD_DIM)
    dv = dv.reshape(B, S, DIFF_HEADS, 2 * DIFF_HEAD_DIM)
    dq = jnp.concatenate([apply_rope(dq[..., :DIFF_ROT_DIM], pos, ROPE_THETA), dq[..., DIFF_ROT_DIM:]], axis=-1)
    dk = jnp.concatenate([apply_rope(dk[..., :DIFF_ROT_DIM], pos, ROPE_THETA), dk[..., DIFF_ROT_DIM:]], axis=-1)
    lp = diff_lambda.astype(jnp.float32)
    lam = jnp.exp(jnp.sum(lp[0] * lp[1])) - jnp.exp(jnp.sum(lp[2] * lp[3])) + lam_init
    a = diff_attention(dq, dk, dv, lam)
    a = (rmsnorm(a, diff_subln) * (1.0 - lam_init)).reshape(B, S, DIFF_HEADS * 2 * DIFF_HEAD_DIM)

    half = GQA_HEAD_DIM // 2
    gq = rmsnorm(gq.reshape(B, S, GQA_Q_HEADS, GQA_HEAD_DIM), gqa_q_norm)
    gk = rmsnorm(gk.reshape(B, S, GQA_KV_HEADS, GQA_HEAD_DIM), gqa_k_norm)
    gv = gv.reshape(B, S, GQA_KV_HEADS, GQA_HEAD_DIM)
    gq = jnp.concatenate([apply_rope(gq[..., :half], row, AXIAL_THETA), apply_rope(gq[..., half:], col, AXIAL_THETA)], axis=-1)
    gk = jnp.concatenate([apply_rope(gk[..., :half], row, AXIAL_THETA), apply_rope(gk[..., half:], col, AXIAL_THETA)], axis=-1)
    gq = gq.reshape(B, S, GQA_KV_HEADS, GQA_Q_HEADS // GQA_KV_HEADS, GQA_HEAD_DIM)
    b = gqa_attention(gq, gk, gv)

    g = jax.nn.sigmoid(gl.astype(jnp.float32)).astype(h.dtype)
    merged = g[..., :D_MODEL] * a + g[..., D_MODEL:] * b
    return merged @ w_out


def swiglu(h, w_gate, w_up, w_down):
    return (jax.nn.silu(h @ w_gate) * (h @ w_up)) @ w_down


def moe_swiglu(h, router_w, w_gate, w_up, w_down):
    logits = jnp.einsum('bsd,de->bse', h, router_w, preferred_element_type=jnp.float32)
    top_v, top_i = lax.top_k(logits, TOP_K)
    top_w = jax.nn.softmax(top_v, axis=-1)
    gates = jnp.sum(jax.nn.one_hot(top_i, N_EXPERTS, dtype=jnp.float32) * top_w[..., None], axis=-2)
    out = jnp.zeros(h.shape, jnp.float32)
    for e in range(N_EXPERTS):
        out = out + gates[..., e:e + 1] * swiglu(h, w_gate[e], w_up[e], w_down[e]).astype(jnp.float32)
    return out.astype(h.dtype)


def trunk(x, c, w_ada, b_ada, norm1, w_in, diff_lambda, diff_subln, gqa_q_norm, gqa_k_norm, w_out, norm2,
          ffn_w_gate, ffn_w_up, ffn_w_down, router_w, moe_w_gate, moe_w_up, moe_w_down, final_norm):
    B, S, _ = x.shape
    rows = S // GRID_W
    pos = jnp.arange(S, dtype=jnp.int32)
    row = jnp.repeat(jnp.arange(rows, dtype=jnp.int32), GRID_W)
    col = jnp.tile(jnp.arange(GRID_W, dtype=jnp.int32), rows)
    c_act = jax.nn.silu(c)
    for l in range(DEPTH):
        mod = (c_act @ w_ada[l] + b_ada[l])[:, None, :]
        sh1, sc1, g1, sh2, sc2, g2 = jnp.split(mod, N_MOD, axis=-1)
        lam_init = 0.8 - 0.6 * math.exp(-0.3 * l)
        h = rmsnorm(x, norm1[l]) * (1.0 + sc1) + sh1
        x = x + g1 * mixing(h, pos, row, col, lam_init, w_in[l], diff_lambda[l], diff_subln[l],
                            gqa_q_norm[l], gqa_k_norm[l], w_out[l])
        h = rmsnorm(x, norm2[l]) * (1.0 + sc2) + sh2
        if l % 2 == 0:
            y = swiglu(h, ffn_w_gate[l // 2], ffn_w_up[l // 2], ffn_w_down[l // 2])
        else:
            y = moe_swiglu(h, router_w[l // 2], moe_w_gate[l // 2], moe_w_up[l // 2], moe_w_down[l // 2])
        x = x + g2 * y
    return rmsnorm(x, final_norm)


def setup_inputs(seed: int = 0) -> dict:
    key = jax.random.key(seed)
    ks = jax.random.split(key, 24)
    f32 = jnp.float32

    def nrm(k, shape, scale):
        return jax.random.normal(k, shape, f32) * scale

    def gain(k, shape):
        return 1.0 + 0.02 * jax.random.normal(k, shape, f32)

    return {
        "x_prompt": nrm(ks[0], (BATCH, SEQ, D_MODEL), 1.0),
        "x_sample": nrm(ks[1], (DEC_BATCH, DEC_SEQ, D_MODEL), 1.0),
        "c_prompt": nrm(ks[2], (BATCH, D_MODEL), 1.0),
        "c_sample": nrm(ks[3], (DEC_BATCH, D_MODEL), 1.0),
        "w_ada": nrm(ks[4], (DEPTH, D_MODEL, N_MOD * D_MODEL), 0.5 * D_MODEL ** -0.5),
        "b_ada": nrm(ks[5], (DEPTH, N_MOD * D_MODEL), 0.02),
        "norm1": gain(ks[6], (DEPTH, D_MODEL)),
        "w_in": nrm(ks[7], (DEPTH, D_MODEL, IN_WIDTH), D_MODEL ** -0.5),
        "diff_lambda": nrm(ks[8], (DEPTH, 4, DIFF_HEAD_DIM), 0.1),
        "diff_subln": gain(ks[9], (DEPTH, 2 * DIFF_HEAD_DIM)),
        "gqa_q_norm": gain(ks[10], (DEPTH, GQA_HEAD_DIM)),
        "gqa_k_norm": gain(ks[11], (DEPTH, GQA_HEAD_DIM)),
        "w_out": nrm(ks[12], (DEPTH, D_MODEL, D_MODEL), D_MODEL ** -0.5),
        "norm2": gain(ks[13], (DEPTH, D_MODEL)),
        "ffn_w_gate": nrm(ks[14], (N_DENSE, D_MODEL, D_FF_DENSE), D_MODEL ** -0.5),
        "ffn_w_up": nrm(ks[15], (N_DENSE, D_MODEL, D_FF_DENSE), D_MODEL ** -0.5),
        "ffn_w_down": nrm(ks[16], (N_DENSE, D_FF_DENSE, D_MODEL), D_FF_DENSE ** -0.5),
        "router_w": nrm(ks[17], (N_MOE, D_MODEL, N_EXPERTS), D_MODEL ** -0.5),
        "moe_w_gate": nrm(ks[18], (N_MOE, N_EXPERTS, D_MODEL, D_FF_EXPERT), D_MODEL ** -0.5),
        "moe_w_up": nrm(ks[19], (N_MOE, N_EXPERTS, D_MODEL, D_FF_EXPERT), D_MODEL ** -0.5),
        "moe_w_down": nrm(ks[20], (N_MOE, N_EXPERTS, D_FF_EXPERT, D_MODEL), D_FF_EXPERT ** -0.5),
        "final_norm": gain(ks[21], (D_MODEL,)),
    }


def reference(x_prompt, x_sample, c_prompt, c_sample, w_ada, b_ada, norm1, w_in, diff_lambda, diff_subln,
              gqa_q_norm, gqa_k_norm, w_out, norm2, ffn_w_gate, ffn_w_up, ffn_w_down, router_w,
              moe_w_gate, moe_w_up, moe_w_down, final_norm):
    y_prompt = trunk(x_prompt, c_prompt, w_ada, b_ada, norm1, w_in, diff_lambda, diff_subln, gqa_q_norm,
                     gqa_k_norm, w_out, norm2, ffn_w_gate, ffn_w_up, ffn_w_down, router_w,
                     moe_w_gate, moe_w_up, moe_w_down, final_norm)
    y_sample = trunk(x_sample, c_sample, w_ada, b_ada, norm1, w_in, diff_lambda, diff_subln, gqa_q_norm,
                     gqa_k_norm, w_out, norm2, ffn_w_gate, ffn_w_up, ffn_w_down, router_w,
                     moe_w_gate, moe_w_up, moe_w_down, final_norm)
    return (y_prompt, y_sample)
```

```python
import math
import os
from contextlib import ExitStack

import numpy as np
import concourse.bass as bass
import concourse.mybir as mybir
from concourse.bass_utils import run_bass_kernel_spmd

F32 = mybir.dt.float32
BF16 = mybir.dt.bfloat16
AF = mybir.ActivationFunctionType
ALU = mybir.AluOpType
AX = mybir.AxisListType

D = 1024
KC = 8
EPS = 1e-6
DIFF_THETA = 500000.0
AX_THETA = 10000.0
GRID_W = 64
NE = 8


class Cfg:
    def __init__(self, L, S1, S2, S2L, DFF, DFE):
        self.L, self.S1, self.S2, self.S2L, self.DFF, self.DFE = L, S1, S2, S2L, DFF, DFE
        self.T = S1 + S2L
        self.TK = S1 + S2


_PH = [0]


class Trk:
    __slots__ = ("w", "r")

    def __init__(self):
        self.w = []
        self.r = {}


class Sched:
    ENG = ("pe", "act", "dve", "pool", "sp")
    ND = 6

    _shared = None

    def __init__(self, nc, es, tag):
        self.nc = nc
        self.lists = {e: [] for e in self.ENG}
        self.pend = {e: [] for e in self.ENG}
        sh = Sched._shared
        if sh is None or sh["nc"] is not nc:
            gs = Sched._gs
            sh = {"nc": nc,
                  "sem": {e: gs.enter_context(nc.semaphore(f"s_{e}")) for e in self.ENG},
                  "cnt": {e: 0 for e in self.ENG},
                  "waited": {e: {} for e in self.ENG},
                  "dsem": {q: [gs.enter_context(nc.semaphore(f"d_{q}{i}")) for i in range(self.ND)] for q in ("sp", "pool")},
                  "dcnt": {q: [0] * self.ND for q in ("sp", "pool")},
                  "dlast": {q: [None] * self.ND for q in ("sp", "pool")},
                  "dnext": {q: 0 for q in ("sp", "pool")}}
            Sched._shared = sh
        self.sem, self.cnt, self.waited = sh["sem"], sh["cnt"], sh["waited"]
        self.dsem, self.dcnt, self.dlast, self.dnext = sh["dsem"], sh["dcnt"], sh["dlast"], sh["dnext"]

    def _wait(self, e, toks):
        for t in toks:
            if t is None:
                continue
            sem, val = t
            k = id(sem)
            if self.waited[e].get(k, 0) < val:
                self.waited[e][k] = val
                self.lists[e].append(lambda E, sem=sem, val=val: E.wait_ge(sem, val))

    @staticmethod
    def _deps(reads, writes):
        deps = []
        for b in reads:
            deps += b.w
        for b in writes:
            deps += b.w
            deps += list(b.r.values())
        return deps

    @staticmethod
    def _commit(tok, reads, writes):
        k = id(tok[0])
        for b in reads:
            b.r[k] = tok
        for b in writes:
            b.w = [tok]
            b.r = {}

    def op(self, e, fn, reads=(), writes=(), sig=True):
        self._wait(e, self._deps(reads, writes))
        self.pend[e].append((reads, writes))
        if not sig:
            self.lists[e].append(fn)
            return None
        self.cnt[e] += 1
        sem = self.sem[e]
        tok = (sem, self.cnt[e])
        self.lists[e].append(lambda E, fn=fn, sem=sem: fn(E).then_inc(sem, 1))
        for rs, ws in self.pend[e]:
            self._commit(tok, rs, ws)
        self.pend[e] = []
        return tok

    def dma(self, q, out, in_, reads=(), writes=()):
        i = self.dnext[q]
        self.dnext[q] = (i + 1) % self.ND
        self._wait(q, self._deps(reads, writes) + [self.dlast[q][i]])
        self.dcnt[q][i] += 16
        sem = self.dsem[q][i]
        tok = (sem, self.dcnt[q][i])
        self.dlast[q][i] = tok
        self.lists[q].append(lambda E, out=out, in_=in_, sem=sem: E.dma_start(out=out, in_=in_).then_inc(sem, 16))
        self._commit(tok, reads, writes)
        return tok

    def emit(self):
        _PH[0] += 1
        if _PH[0] > int(os.environ.get('KSTOP', '9999')) or _PH[0] == int(os.environ.get('KSKIP', '-1')):
            return
        allt = [t for q in ("sp", "pool") for t in self.dlast[q]]
        allt += [(self.sem[e], self.cnt[e]) for e in self.ENG if self.cnt[e] > 0 and e != "sp"]
        for e in self.ENG:
            self._wait(e, allt)
        with self.nc.Block() as blk:
            def run(key):
                def f(E):
                    for g in self.lists[key]:
                        g(E)
                return f
            blk.tensor(run("pe"))
            blk.scalar(run("act"))
            blk.vector(run("dve"))
            blk.gpsimd(run("pool"))
            blk.sync(run("sp"))


_UID = [0]


class Ring:
    def __init__(self, nc, es, name, shape, dtype, n, psum=False):
        mk = nc.psum_tensor if psum else nc.sbuf_tensor
        _UID[0] += 1
        self.t = [es.enter_context(mk(f"{name}_{_UID[0]}_{i}", shape, dtype)) for i in range(n)]
        self.k = [Trk() for _ in range(n)]
        self.i = 0

    def next(self):
        i = self.i
        self.i = (i + 1) % len(self.t)
        return self.t[i], self.k[i]


def one(nc, es, name, shape, dtype, psum=False):
    r = Ring(nc, es, name, shape, dtype, 1, psum)
    return r.t[0], r.k[0]


def build(cfg):
    nc = bass.Bass("TRN2", target_bir_lowering=False)
    L, S1, S2, S2L, T, TK, DFF, DFE = cfg.L, cfg.S1, cfg.S2, cfg.S2L, cfg.T, cfg.TK, cfg.DFF, cfg.DFE
    NCH = T // 512

    def din(name, shape):
        return nc.dram_tensor(name, list(shape), F32, kind="ExternalInput").ap()

    x_in = din("x", [T, D])
    cvec = din("cvec", [2, D])
    cmats = din("cmats", [128, 5, 128])
    tabs = din("tabs", [4, 128, T])
    w_ada = din("w_ada", [L, D, 6 * D])
    b_ada = din("b_ada", [L, 6 * D])
    norm1 = din("norm1", [L, D])
    w_in = din("w_in", [L, D, 6656])
    diff_lambda = din("diff_lambda", [L, 4, 64])
    diff_subln = din("diff_subln", [L, 128])
    gqa_q_norm = din("gqa_q_norm", [L, 64])
    gqa_k_norm = din("gqa_k_norm", [L, 64])
    w_out = din("w_out", [L, D, D])
    norm2 = din("norm2", [L, D])
    ND_, NM_ = (L + 1) // 2, L // 2
    ffn_w_gate = din("ffn_w_gate", [ND_, D, DFF])
    ffn_w_up = din("ffn_w_up", [ND_, D, DFF])
    ffn_w_down = din("ffn_w_down", [ND_, DFF, D])
    router_w = din("router_w", [max(NM_, 1), D, NE])
    moe_w_gate = din("moe_w_gate", [max(NM_, 1), NE, D, DFE])
    moe_w_up = din("moe_w_up", [max(NM_, 1), NE, D, DFE])
    moe_w_down = din("moe_w_down", [max(NM_, 1), NE, DFE, D])
    final_norm = din("final_norm", [D])
    y_out = nc.dram_tensor("y", [T, D], F32, kind="ExternalOutput").ap()

    def scr(name, shape, dt):
        return nc.dram_tensor(name, list(shape), dt).ap()

    xres = scr("xres", [T, D], F32)
    hT = scr("hT", [128, KC, T], BF16)
    QT = scr("QT", [16, 128, T], BF16)
    KT = scr("KT", [12, 128, TK], BF16)
    Vs = scr("Vs", [TK, 1280], BF16)
    GT = scr("GT", [16, 128, T], BF16)
    mT = scr("mT", [128, KC, T], BF16)
    modg = scr("modg", [L, 2, 2, 128, D], F32)
    yacc = scr("yacc", [T, D], F32)

    def slot_of(tok0):
        return 0 if tok0 < S1 else 1

    def key_off(tok0):
        return tok0 if tok0 < S1 else tok0

    with ExitStack() as gs:
        Sched._gs = gs
        Sched._shared = None
        cm = gs.enter_context(nc.sbuf_tensor("cm", [128, 5, 128], BF16))
        IDENT, PD, PG, BONES, ONES = (cm[:, i, :] for i in range(5))
        modf = gs.enter_context(nc.sbuf_tensor("modf", [128, L, 4, KC, 2], F32))
        fnorm = gs.enter_context(nc.sbuf_tensor("fnorm", [128, KC], F32))
        wsub = gs.enter_context(nc.sbuf_tensor("wsub", [128, L], F32))
        nlam = gs.enter_context(nc.sbuf_tensor("nlam", [128, L], F32))
        gq8 = gs.enter_context(nc.sbuf_tensor("gq8", [128, L], F32))
        gk8 = gs.enter_context(nc.sbuf_tensor("gk8", [128, L], F32))

        with ExitStack() as es, nc.allow_non_contiguous_dma("tiny feature-major parameter loads"):
            S = Sched(nc, es, "p0")
            E = es.enter_context
            kcm, kmodf, kmisc = Trk(), Trk(), Trk()
            S.dma("pool", cm[:], cmats, writes=[kcm])
            cT32, kcT32 = one(nc, es, "cT32", [128, 2, KC], F32)
            cTb, kcTb = one(nc, es, "cTb", [128, 2, KC], BF16)
            crep, kcrep = one(nc, es, "crep", [128, KC, 2, 128], BF16)
            n1, kn1 = one(nc, es, "n1", [128, L, KC], F32)
            n2, kn2 = one(nc, es, "n2", [128, L, KC], F32)
            bfm, kbfm = one(nc, es, "bfm", [128, L, 48], F32)
            sub, ksub = one(nc, es, "sub", [128, L], F32)
            gqn, kgqn = one(nc, es, "gqn", [128, L], F32)
            gkn, kgkn = one(nc, es, "gkn", [128, L], F32)
            dl, kdl = one(nc, es, "dl", [128, L, 4, 64], F32)
            for s_ in range(2):
                S.dma("sp", cT32[:, s_, :], cvec[s_].rearrange("(c p) -> p c", p=128), writes=[kcT32])
            S.dma("sp", n1[:], norm1.rearrange("l (c p) -> p l c", p=128), writes=[kn1])
            S.dma("sp", n2[:], norm2.rearrange("l (c p) -> p l c", p=128), writes=[kn2])
            S.dma("sp", fnorm[:], final_norm.rearrange("(c p) -> p c", p=128), writes=[kmisc])
            S.dma("sp", bfm[:], b_ada.rearrange("l (c p) -> p l c", p=128), writes=[kbfm])
            S.dma("sp", sub[:], diff_subln.rearrange("l p -> p l"), writes=[ksub])
            for hh in range(2):
                S.dma("sp", gqn[hh * 64:(hh + 1) * 64, :], gqa_q_norm.rearrange("l p -> p l"), writes=[kgqn])
                S.dma("sp", gkn[hh * 64:(hh + 1) * 64, :], gqa_k_norm.rearrange("l p -> p l"), writes=[kgkn])
            S.dma("sp", dl[:].rearrange("p l a d -> p (l a d)"),
                  diff_lambda.rearrange("l a d -> (l a d)").partition_broadcast(128), writes=[kdl])
            S.op("act", lambda A: A.activation(out=cTb[:], in_=cT32[:], func=AF.Silu), reads=[kcT32], writes=[kcTb])
            for s in range(2):
                S.op("dve", lambda V, s=s: V.tensor_copy(crep[:, :, s, :], cTb[:, s, :].unsqueeze(2).to_broadcast([128, KC, 128])),
                     reads=[kcTb], writes=[kcrep])
            tmp, ktmp = one(nc, es, "p0tmp", [128, 64], F32)
            sc4, ksc4 = one(nc, es, "sc4", [128, 4], F32)
            for l in range(L):
                lam_init = 0.8 - 0.6 * math.exp(-0.3 * l)
                for m in range(2):
                    S.op("dve", lambda V, l=l, m=m: V.tensor_tensor(out=tmp[:], in0=dl[:, l, 2 * m, :], in1=dl[:, l, 2 * m + 1, :], op=ALU.mult),
                         reads=[kdl], writes=[ktmp])
                    S.op("dve", lambda V, m=m: V.tensor_reduce(out=sc4[:, m:m + 1], in_=tmp[:], axis=AX.X, op=ALU.add),
                         reads=[ktmp], writes=[ksc4])
                S.op("act", lambda A: A.activation(out=sc4[:, 2:4], in_=sc4[:, 0:2], func=AF.Exp), reads=[ksc4], writes=[ksc4])
                S.op("dve", lambda V, l=l, li=lam_init: V.scalar_tensor_tensor(out=nlam[:, l:l + 1], in0=sc4[:, 3:4], scalar=-li, in1=sc4[:, 2:3],
                                                                            op0=ALU.add, op1=ALU.subtract),
                     reads=[ksc4], writes=[kmisc])
                S.op("dve", lambda V, l=l, li=lam_init: V.tensor_scalar(out=wsub[:, l:l + 1], in0=sub[:, l:l + 1], scalar1=(1.0 - li) * math.sqrt(128.0),
                                                                         scalar2=None, op0=ALU.mult),
                     reads=[ksub], writes=[kmisc])
                S.op("dve", lambda V, l=l: V.tensor_scalar(out=gq8[:, l:l + 1], in0=gqn[:, l:l + 1], scalar1=8.0, scalar2=None, op0=ALU.mult),
                     reads=[kgqn], writes=[kmisc])
                S.op("dve", lambda V, l=l: V.tensor_scalar(out=gk8[:, l:l + 1], in0=gkn[:, l:l + 1], scalar1=8.0, scalar2=None, op0=ALU.mult),
                     reads=[kgkn], writes=[kmisc])
            wr = Ring(nc, es, "wada", [128, KC, 512], BF16, 2)
            br = Ring(nc, es, "bbc", [128, 512], F32, 2)
            gr = Ring(nc, es, "gout", [128, 512], F32, 2)
            pr = Ring(nc, es, "p0ps", [128, 512], F32, 2, psum=True)
            V4 = {0: 0, 1: 1, 3: 2, 4: 3}
            for l in range(L):
                for j in range(12):
                    vec, half = j // 2, j % 2
                    wt, kw = wr.next()
                    S.dma("pool", wt[:], w_ada[l, :, j * 512:(j + 1) * 512].rearrange("(kc p) n -> p kc n", p=128), writes=[kw])
                    if vec in (2, 5):
                        which = 0 if vec == 2 else 1
                        bb, kb = br.next()
                        S.dma("sp", bb[:], b_ada[l, j * 512:(j + 1) * 512].partition_broadcast(128), writes=[kb])
                        for s in range(2):
                            ps, kp = pr.next()
                            for kc in range(KC):
                                S.op("pe", lambda P, ps=ps, wt=wt, s=s, kc=kc: P.matmul(ps[:], crep[:, kc, s, :], wt[:, kc, :], start=(kc == 0), stop=(kc == KC - 1)),
                                     reads=[kcrep, kw], writes=[kp], sig=(kc == KC - 1))
                            go, kg = gr.next()
                            S.op("dve", lambda V, go=go, ps=ps, bb=bb: V.tensor_tensor(out=go[:], in0=ps[:], in1=bb[:], op=ALU.add),
                                 reads=[kp, kb], writes=[kg])
                            S.dma("sp", modg[l, which, s, :, half * 512:(half + 1) * 512], go[:], reads=[kg])
                    else:
                        v4 = V4[vec]
                        for cb in range(4):
                            c = half * 4 + cb
                            ps, kp = pr.next()
                            for kc in range(KC):
                                S.op("pe", lambda P, ps=ps, wt=wt, cb=cb, kc=kc: P.matmul(ps[:, 0:2], wt[:, kc, cb * 128:(cb + 1) * 128], cTb[:, :, kc], start=(kc == 0), stop=(kc == KC - 1)),
                                     reads=[kcTb, kw], writes=[kp], sig=(kc == KC - 1))
                            S.op("dve", lambda V, ps=ps, l=l, v4=v4, c=c, vec=vec: V.tensor_scalar(out=modf[:, l, v4, c, :], in0=ps[:, 0:2], scalar1=bfm[:, l, vec * 8 + c:vec * 8 + c + 1],
                                                                                                   scalar2=None, op0=ALU.add),
                                 reads=[kp, kbfm], writes=[kmodf])
                for v4, nn, kn in ((1, n1, kn1), (3, n2, kn2)):
                    for s in range(2):
                        S.op("dve", lambda V, l=l, v4=v4, s=s, nn=nn: V.scalar_tensor_tensor(out=modf[:, l, v4, :, s], in0=modf[:, l, v4, :, s], scalar=1.0, in1=nn[:, l, :],
                                                                                           op0=ALU.add, op1=ALU.mult),
                             reads=[kmodf, kn], writes=[kmodf])
            S.emit()

        def norm_phase(tag, src, l, v_sh, v_s):
            with ExitStack() as es:
                S = Sched(nc, es, tag)
                xr = Ring(nc, es, "nx", [128, D], F32, 3)
                jr = Ring(nc, es, "njunk", [128, D], BF16, 1)
                sr = Ring(nc, es, "nss", [128, 4], F32, 4)
                xnr = Ring(nc, es, "nxn", [128, D], BF16, 3)
                tr = Ring(nc, es, "nT", [128, KC, 512], BF16, 1, psum=True)
                hr = Ring(nc, es, "nh", [128, KC, 512], BF16, 2)
                for ci in range(NCH):
                    s = slot_of(ci * 512)
                    pT, kT = tr.next()
                    for tt in range(4):
                        r0 = ci * 512 + tt * 128
                        xt, kx = xr.next()
                        S.dma("sp", xt[:], src[r0:r0 + 128, :], writes=[kx])
                        jt, kj = jr.next()
                        st, ks = sr.next()
                        S.op("dve", lambda V, st=st: V.memset(st[:, 0:1], 0.0), writes=[ks])
                        S.op("act", lambda A, jt=jt, xt=xt, st=st: A.activation(out=jt[:], in_=xt[:], func=AF.Square, accum_out=st[:, 0:1]),
                             reads=[kx], writes=[kj, ks])
                        S.op("act", lambda A, st=st: A.activation(out=st[:, 1:2], in_=st[:, 0:1], func=AF.Sqrt, scale=1.0 / D, bias=EPS),
                             reads=[ks], writes=[ks])
                        S.op("dve", lambda V, st=st: V.reciprocal(st[:, 2:3], st[:, 1:2]), reads=[ks], writes=[ks])
                        xn, kxn = xnr.next()
                        S.op("pool", lambda G, xn=xn, xt=xt, st=st: G.tensor_scalar(out=xn[:], in0=xt[:], scalar1=st[:, 2:3], scalar2=None, op0=ALU.mult),
                             reads=[kx, ks], writes=[kxn])
                        for c in range(KC):
                            S.op("pe", lambda P, pT=pT, xn=xn, c=c, tt=tt: P.transpose(pT[:, c, tt * 128:(tt + 1) * 128], xn[:, c * 128:(c + 1) * 128], IDENT),
                                 reads=[kxn], writes=[kT], sig=(c == KC - 1))
                    ht, kh = hr.next()
                    for c in range(KC):
                        if c % 2 == 0:
                            S.op("dve", lambda V, ht=ht, pT=pT, c=c, s=s: V.tensor_scalar(out=ht[:, c, :], in0=pT[:, c, :], scalar1=modf[:, l, v_s, c, s:s + 1],
                                                                                         scalar2=modf[:, l, v_sh, c, s:s + 1], op0=ALU.mult, op1=ALU.add),
                                 reads=[kT], writes=[kh])
                        else:
                            S.op("act", lambda A, ht=ht, pT=pT, c=c, s=s: A.activation(out=ht[:, c, :], in_=pT[:, c, :], func=AF.Identity,
                                                                                      scale=modf[:, l, v_s, c, s:s + 1], bias=modf[:, l, v_sh, c, s:s + 1]),
                                 reads=[kT], writes=[kh])
                    S.dma("sp", hT[:, :, ci * 512:(ci + 1) * 512], ht[:], reads=[kh])
                S.emit()

        def proj_phase(tag, l):
            with ExitStack() as es:
                S = Sched(nc, es, tag + "a")
                wq, kwq = one(nc, es, "wq", [128, KC, 3584], BF16)
                wsrc = w_in[l].rearrange("(kc p) n -> p kc n", p=128)
                S.dma("pool", wq[:, :, 0:2048], wsrc[:, :, 0:2048], writes=[kwq])
                S.dma("pool", wq[:, :, 2048:3072], wsrc[:, :, 3072:4096], writes=[kwq])
                for g in range(4):
                    for hh in range(2):
                        c0 = 3072 + g * 128 + hh * 64
                        S.dma("pool", wq[:, :, c0:c0 + 64], wsrc[:, :, 4096 + g * 64:4096 + (g + 1) * 64], writes=[kwq])
                hr = Ring(nc, es, "ph", [128, KC, 512], BF16, 2)
                tbr = Ring(nc, es, "ptab", [128, 4, 512], F32, 2)
                pr = Ring(nc, es, "pps", [128, 512], F32, 4, psum=True)
                p2 = Ring(nc, es, "pps2", [128, 512], F32, 3, psum=True)
                qsr = Ring(nc, es, "pqs", [128, 512], BF16, 3)
                sqr = Ring(nc, es, "psq", [128, 512], BF16, 2)
                rrr = Ring(nc, es, "prr", [128, 512], F32, 2)
                t1r = Ring(nc, es, "pt1", [128, 512], F32, 3)
                t2r = Ring(nc, es, "pt2", [128, 512], F32, 3)
                outr = Ring(nc, es, "pout", [128, 512], BF16, 4)
                for ci in range(NCH):
                    t0 = ci * 512
                    k0 = key_off(t0)
                    ht, kh = hr.next()
                    S.dma("sp", ht[:], hT[:, :, t0:t0 + 512], writes=[kh])
                    tb, ktb = tbr.next()
                    S.dma("sp", tb[:], tabs[:, :, t0:t0 + 512].rearrange("a p t -> p a t"), writes=[ktb])
                    for blk in range(28):
                        ps, kp = pr.next()
                        for kc in range(KC):
                            S.op("pe", lambda P, ps=ps, ht=ht, blk=blk, kc=kc: P.matmul(ps[:], wq[:, kc, blk * 128:(blk + 1) * 128], ht[:, kc, :], start=(kc == 0), stop=(kc == KC - 1)),
                                 reads=[kwq, kh], writes=[kp], sig=(kc == KC - 1))
                        qs, kq = qsr.next()
                        if blk < 16:
                            S.op("act", lambda A, qs=qs, ps=ps: A.copy(qs[:], ps[:]), reads=[kp], writes=[kq])
                            PM, ti = PD, 0
                            dst = QT[blk, :, t0:t0 + 512] if blk < 8 else KT[blk - 8, :, k0:k0 + 512]
                        else:
                            sq, ksq = sqr.next()
                            S.op("act", lambda A, sq=sq, ps=ps: A.activation(out=sq[:], in_=ps[:], func=AF.Square), reads=[kp], writes=[ksq])
                            pss, kps = p2.next()
                            S.op("pe", lambda P, pss=pss, sq=sq: P.matmul(pss[:], BONES, sq[:], start=True, stop=True), reads=[ksq], writes=[kps])
                            rr, krr = rrr.next()
                            S.op("act", lambda A, rr=rr, pss=pss: A.activation(out=rr[:], in_=pss[:], func=AF.Sqrt, scale=1.0, bias=64.0 * EPS),
                                 reads=[kps], writes=[krr])
                            S.op("dve", lambda V, rr=rr: V.reciprocal(rr[:], rr[:]), reads=[krr], writes=[krr])
                            gv = gq8 if blk < 24 else gk8
                            S.op("dve", lambda V, qs=qs, ps=ps, rr=rr, gv=gv: V.scalar_tensor_tensor(out=qs[:], in0=ps[:], scalar=gv[:, l:l + 1], in1=rr[:], op0=ALU.mult, op1=ALU.mult),
                                 reads=[kp, krr], writes=[kq])
                            PM, ti = PG, 2
                            dst = QT[8 + blk - 16, :, t0:t0 + 512] if blk < 24 else KT[8 + blk - 24, :, k0:k0 + 512]
                        pp, kpp = p2.next()
                        S.op("pe", lambda P, pp=pp, qs=qs, PM=PM: P.matmul(pp[:], PM, qs[:], start=True, stop=True), reads=[kq], writes=[kpp])
                        t1, kt1 = t1r.next()
                        S.op("pool", lambda G, t1=t1, qs=qs, tb=tb, ti=ti: G.tensor_tensor(out=t1[:], in0=qs[:], in1=tb[:, ti, :], op=ALU.mult),
                             reads=[kq, ktb], writes=[kt1])
                        t2, kt2 = t2r.next()
                        S.op("dve", lambda V, t2=t2, pp=pp, tb=tb, ti=ti: V.tensor_tensor(out=t2[:], in0=pp[:], in1=tb[:, ti + 1, :], op=ALU.mult),
                             reads=[kpp, ktb], writes=[kt2])
                        ot, ko = outr.next()
                        S.op("pool", lambda G, ot=ot, t1=t1, t2=t2: G.tensor_tensor(out=ot[:], in0=t1[:], in1=t2[:], op=ALU.add),
                             reads=[kt1, kt2], writes=[ko])
                        S.dma("sp", dst, ot[:], reads=[ko])
                S.emit()
            with ExitStack() as es:
                S = Sched(nc, es, tag + "b")
                wg, kwg = one(nc, es, "wg", [128, KC, 3328], BF16)
                wsrc = w_in[l].rearrange("(kc p) n -> p kc n", p=128)
                S.dma("pool", wg[:, :, 0:2048], wsrc[:, :, 4608:6656], writes=[kwg])
                S.dma("pool", wg[:, :, 2048:3072], wsrc[:, :, 2048:3072], writes=[kwg])
                S.dma("pool", wg[:, :, 3072:3328], wsrc[:, :, 4352:4608], writes=[kwg])
                hr = Ring(nc, es, "qh", [128, KC, 512], BF16, 2)
                pr = Ring(nc, es, "qps", [128, 512], F32, 6, psum=True)
                outr = Ring(nc, es, "qout", [128, 512], BF16, 4)
                vr = Ring(nc, es, "qv", [128, 1280], BF16, 3)
                for ci in range(NCH):
                    t0 = ci * 512
                    k0 = key_off(t0)
                    ht, kh = hr.next()
                    S.dma("sp", ht[:], hT[:, :, t0:t0 + 512], writes=[kh])
                    for blk in range(16):
                        ps, kp = pr.next()
                        for kc in range(KC):
                            S.op("pe", lambda P, ps=ps, ht=ht, blk=blk, kc=kc: P.matmul(ps[:], wg[:, kc, blk * 128:(blk + 1) * 128], ht[:, kc, :], start=(kc == 0), stop=(kc == KC - 1)),
                                 reads=[kwg, kh], writes=[kp], sig=(kc == KC - 1))
                        ot, ko = outr.next()
                        S.op("act", lambda A, ot=ot, ps=ps: A.activation(out=ot[:], in_=ps[:], func=AF.Sigmoid), reads=[kp], writes=[ko])
                        S.dma("sp", GT[blk, :, t0:t0 + 512], ot[:], reads=[ko])
                    for tt in range(4):
                        vt, kv = vr.next()
                        for n0, nn in ((0, 512), (512, 512), (1024, 256)):
                            ps, kp = pr.next()
                            for kc in range(KC):
                                S.op("pe", lambda P, ps=ps, ht=ht, tt=tt, kc=kc, n0=n0, nn=nn: P.matmul(ps[:, 0:nn], ht[:, kc, tt * 128:(tt + 1) * 128], wg[:, kc, 2048 + n0:2048 + n0 + nn],
                                                                                                        start=(kc == 0), stop=(kc == KC - 1)),
                                     reads=[kwg, kh], writes=[kp], sig=(kc == KC - 1))
                            eng = "dve" if n0 == 512 else "act"
                            if eng == "dve":
                                S.op("dve", lambda V, vt=vt, ps=ps, n0=n0, nn=nn: V.tensor_copy(vt[:, n0:n0 + nn], ps[:, 0:nn]), reads=[kp], writes=[kv])
                            else:
                                S.op("act", lambda A, vt=vt, ps=ps, n0=n0, nn=nn: A.copy(vt[:, n0:n0 + nn], ps[:, 0:nn]), reads=[kp], writes=[kv])
                        S.dma("sp", Vs[k0 + tt * 128:k0 + (tt + 1) * 128, :], vt[:], reads=[kv])
                S.emit()

        def attn_phase(tag, l):
            with ExitStack() as es:
                S = Sched(nc, es, tag)
                SKM = max(S1, S2)
                ktd = Ring(nc, es, "aktd", [128, SKM], BF16, 2)
                ktg = Ring(nc, es, "aktg", [128, SKM], BF16, 2)
                vdr = Ring(nc, es, "avd", [128, SKM // 128, 128], BF16, 2)
                vgr = Ring(nc, es, "avg", [128, SKM // 128, 64], BF16, 2)
                qdr = Ring(nc, es, "aqd", [128, 512], BF16, 2)
                qgr = Ring(nc, es, "aqg", [128, 512], BF16, 2)
                gar = Ring(nc, es, "aga", [128, 512], BF16, 2)
                gbr = Ring(nc, es, "agb", [128, 512], BF16, 2)
                scr_ = Ring(nc, es, "asc", [128, 2, 512], F32, 2, psum=True)
                acc = Ring(nc, es, "aacc", [128, 512], F32, 4, psum=True)
                er = Ring(nc, es, "ae", [128, 2, 512], BF16, 3)
                fr = Ring(nc, es, "af", [128, 512], F32, 8)
                sqr = Ring(nc, es, "asq", [128, 512], BF16, 2)
                mor = Ring(nc, es, "amo", [128, 512], BF16, 2)
                for s in range(2):
                    SK = S1 if s == 0 else S2
                    kb = 0 if s == 0 else S1
                    q_lo, q_hi = (0, S1) if s == 0 else (S1, T)
                    nkt = SK // 128
                    kg_t = vg_t = None
                    for j in range(8):
                        kd, kkd = ktd.next()
                        S.dma("sp", kd[:, 0:SK], KT[j, :, kb:kb + SK], writes=[kkd])
                        vd, kvd = vdr.next()
                        S.dma("sp", vd[:, 0:nkt, :], Vs[kb:kb + SK, j * 128:(j + 1) * 128].rearrange("(kt p) e -> p kt e", p=128), writes=[kvd])
                        if j % 2 == 0:
                            g = j // 2
                            kg_t, kkg = ktg.next()
                            S.dma("sp", kg_t[:, 0:SK], KT[8 + g, :, kb:kb + SK], writes=[kkg])
                            vg_t, kvg = vgr.next()
                            S.dma("sp", vg_t[:, 0:nkt, :], Vs[kb:kb + SK, 1024 + g * 64:1024 + (g + 1) * 64].rearrange("(kt p) e -> p kt e", p=128), writes=[kvg])
                        for t0 in range(q_lo, q_hi, 512):
                            qd, kqd = qdr.next()
                            S.dma("sp", qd[:], QT[j, :, t0:t0 + 512], writes=[kqd])
                            qg, kqg = qgr.next()
                            S.dma("sp", qg[:], QT[8 + j, :, t0:t0 + 512], writes=[kqg])
                            ga, kga = gar.next()
                            S.dma("sp", ga[:], GT[j, :, t0:t0 + 512], writes=[kga])
                            gb, kgb = gbr.next()
                            S.dma("sp", gb[:], GT[8 + j, :, t0:t0 + 512], writes=[kgb])
                            ov1, ko1 = acc.next()
                            ov2, ko2 = acc.next()
                            rb1, kr1 = acc.next()
                            rb2, kr2 = acc.next()
                            for kt in range(nkt):
                                sc, ksc = scr_.next()
                                for m in range(2):
                                    S.op("pe", lambda P, sc=sc, kd=kd, qd=qd, kt=kt, m=m: P.matmul(sc[:, m, :], kd[m * 64:(m + 1) * 64, kt * 128:(kt + 1) * 128], qd[m * 64:(m + 1) * 64, :],
                                                                                                  start=True, stop=True),
                                         reads=[kkd, kqd], writes=[ksc], sig=(m == 1))
                                et, ke = er.next()
                                S.op("act", lambda A, et=et, sc=sc: A.activation(out=et[:], in_=sc[:], func=AF.Exp, scale=0.125), reads=[ksc], writes=[ke])
                                st, sp_ = (kt == 0), (kt == nkt - 1)
                                S.op("pe", lambda P, ov1=ov1, vd=vd, et=et, kt=kt, st=st, sp_=sp_: P.matmul(ov1[:], vd[:, kt, :], et[:, 0, :], start=st, stop=sp_),
                                     reads=[kvd, ke], writes=[ko1], sig=False)
                                S.op("pe", lambda P, ov2=ov2, vd=vd, et=et, kt=kt, st=st, sp_=sp_: P.matmul(ov2[:], vd[:, kt, :], et[:, 1, :], start=st, stop=sp_),
                                     reads=[kvd, ke], writes=[ko2], sig=False)
                                S.op("pe", lambda P, rb1=rb1, et=et, st=st, sp_=sp_: P.matmul(rb1[:], ONES, et[:, 0, :], start=st, stop=sp_),
                                     reads=[ke], writes=[kr1], sig=False)
                                S.op("pe", lambda P, rb2=rb2, et=et, st=st, sp_=sp_: P.matmul(rb2[:], ONES, et[:, 1, :], start=st, stop=sp_),
                                     reads=[ke], writes=[kr2], sig=True)
                            r1, k1 = fr.next()
                            S.op("dve", lambda V, r1=r1, rb1=rb1: V.reciprocal(r1[:], rb1[:]), reads=[kr1], writes=[k1])
                            r2, k2 = fr.next()
                            S.op("dve", lambda V, r2=r2, rb2=rb2: V.reciprocal(r2[:], rb2[:]), reads=[kr2], writes=[k2])
                            a1, ka1 = fr.next()
                            S.op("dve", lambda V, a1=a1, ov1=ov1, r1=r1: V.tensor_tensor(out=a1[:], in0=ov1[:], in1=r1[:], op=ALU.mult), reads=[ko1, k1], writes=[ka1])
                            a2, ka2 = fr.next()
                            S.op("dve", lambda V, a2=a2, ov2=ov2, r2=r2: V.tensor_tensor(out=a2[:], in0=ov2[:], in1=r2[:], op=ALU.mult), reads=[ko2, k2], writes=[ka2])
                            S.op("dve", lambda G, a1=a1, a2=a2: G.scalar_tensor_tensor(out=a1[:], in0=a2[:], scalar=nlam[:, l:l + 1], in1=a1[:], op0=ALU.mult, op1=ALU.add),
                                 reads=[ka2, ka1], writes=[ka1])
                            sq, ksq = sqr.next()
                            S.op("pool", lambda G, sq=sq, a1=a1: G.tensor_tensor(out=sq[:], in0=a1[:], in1=a1[:], op=ALU.mult), reads=[ka1], writes=[ksq])
                            ssq, kss = acc.next()
                            S.op("pe", lambda P, ssq=ssq, sq=sq: P.matmul(ssq[:], ONES, sq[:], start=True, stop=True), reads=[ksq], writes=[kss])
                            rn, krn = fr.next()
                            S.op("act", lambda A, rn=rn, ssq=ssq: A.activation(out=rn[:], in_=ssq[:], func=AF.Sqrt, scale=1.0, bias=128.0 * EPS), reads=[kss], writes=[krn])
                            S.op("dve", lambda V, rn=rn: V.reciprocal(rn[:], rn[:]), reads=[krn], writes=[krn])
                            S.op("dve", lambda V, a1=a1, rn=rn: V.scalar_tensor_tensor(out=a1[:], in0=a1[:], scalar=wsub[:, l:l + 1], in1=rn[:], op0=ALU.mult, op1=ALU.mult),
                                 reads=[ka1, krn], writes=[ka1])
                            S.op("pool", lambda G, a1=a1, ga=ga: G.tensor_tensor(out=a1[:], in0=a1[:], in1=ga[:], op=ALU.mult), reads=[ka1, kga], writes=[ka1])
                            ovg, kog = acc.next()
                            rbg, krg = acc.next()
                            for kt in range(nkt):
                                sc, ksc = scr_.next()
                                for m in range(2):
                                    S.op("pe", lambda P, sc=sc, kg_t=kg_t, qg=qg, kt=kt, m=m: P.matmul(sc[:, m, :], kg_t[m * 64:(m + 1) * 64, kt * 128:(kt + 1) * 128], qg[m * 64:(m + 1) * 64, :],
                                                                                                      start=True, stop=True),
                                         reads=[kkg, kqg], writes=[ksc], sig=(m == 1))
                                et, ke = er.next()
                                S.op("act", lambda A, et=et, sc=sc: A.activation(out=et[:], in_=sc[:], func=AF.Exp, scale=0.125), reads=[ksc], writes=[ke])
                                st, sp_ = (kt == 0), (kt == nkt - 1)
                                for m in range(2):
                                    S.op("pe", lambda P, ovg=ovg, vg_t=vg_t, et=et, kt=kt, m=m, st=st, sp_=sp_: P.matmul(ovg[m * 64:(m + 1) * 64, :], vg_t[:, kt, :], et[:, m, :], start=st, stop=sp_),
                                         reads=[kvg, ke], writes=[kog], sig=False)
                                for m in range(2):
                                    S.op("pe", lambda P, rbg=rbg, et=et, m=m, st=st, sp_=sp_: P.matmul(rbg[m * 64:(m + 1) * 64, :], cm[:, 4, 0:64], et[:, m, :], start=st, stop=sp_),
                                         reads=[ke], writes=[krg], sig=(m == 1))
                            rg, krg2 = fr.next()
                            S.op("dve", lambda V, rg=rg, rbg=rbg: V.reciprocal(rg[:], rbg[:]), reads=[krg], writes=[krg2])
                            bt, kbt = fr.next()
                            S.op("dve", lambda V, bt=bt, ovg=ovg, rg=rg: V.tensor_tensor(out=bt[:], in0=ovg[:], in1=rg[:], op=ALU.mult), reads=[kog, krg2], writes=[kbt])
                            S.op("pool", lambda G, bt=bt, gb=gb: G.tensor_tensor(out=bt[:], in0=bt[:], in1=gb[:], op=ALU.mult), reads=[kbt, kgb], writes=[kbt])
                            mo, kmo = mor.next()
                            S.op("pool", lambda G, mo=mo, bt=bt, a1=a1: G.tensor_tensor(out=mo[:], in0=bt[:], in1=a1[:], op=ALU.add), reads=[kbt, ka1], writes=[kmo])
                            S.dma("sp", mT[:, j, t0:t0 + 512], mo[:], reads=[kmo])
                S.emit()

        def outproj_phase(tag, l, src):
            with ExitStack() as es:
                S = Sched(nc, es, tag)
                wo, kwo = one(nc, es, "wo", [128, KC, D], BF16)
                S.dma("pool", wo[:], w_out[l].rearrange("(kc p) n -> p kc n", p=128), writes=[kwo])
                g1 = [one(nc, es, f"g1_{s}", [128, D], F32) for s in range(2)]
                for s in range(2):
                    S.dma("sp", g1[s][0][:], modg[l, 0, s], writes=[g1[s][1]])
                mr = Ring(nc, es, "om", [128, KC, 512], BF16, 2)
                xr = Ring(nc, es, "ox", [128, D], F32, 3)
                pr = Ring(nc, es, "ops", [128, 2, 512], F32, 2, psum=True)
                yr = Ring(nc, es, "oy", [128, D], F32, 3)
                for ci in range(NCH):
                    t0 = ci * 512
                    s = slot_of(t0)
                    mt, kmt = mr.next()
                    S.dma("sp", mt[:], mT[:, :, t0:t0 + 512], writes=[kmt])
                    for tt in range(4):
                        r0 = t0 + tt * 128
                        xt, kx = xr.next()
                        S.dma("sp", xt[:], src[r0:r0 + 128, :], writes=[kx])
                        ps, kp = pr.next()
                        for h in range(2):
                            for kc in range(KC):
                                S.op("pe", lambda P, ps=ps, mt=mt, tt=tt, kc=kc, h=h: P.matmul(ps[:, h, :], mt[:, kc, tt * 128:(tt + 1) * 128], wo[:, kc, h * 512:(h + 1) * 512],
                                                                                              start=(kc == 0), stop=(kc == KC - 1)),
                                     reads=[kmt, kwo], writes=[kp], sig=(h == 1 and kc == KC - 1))
                        yt, ky = yr.next()
                        S.op("dve", lambda V, yt=yt, ps=ps, s=s: V.tensor_tensor(out=yt[:].rearrange("p (a b) -> p a b", a=2), in0=ps[:], in1=g1[s][0][:].rearrange("p (a b) -> p a b", a=2), op=ALU.mult),
                             reads=[kp, g1[s][1]], writes=[ky])
                        S.op("pool", lambda G, yt=yt, xt=xt: G.tensor_tensor(out=yt[:], in0=yt[:], in1=xt[:], op=ALU.add), reads=[ky, kx], writes=[ky])
                        S.dma("sp", xres[r0:r0 + 128, :], yt[:], reads=[ky])
                S.emit()

        def ffn_pass(tag, l, wg_src, wu_src, wd_src, f0, nf, mode, e_idx, gates_t, first, last):
            with ExitStack() as es:
                S = Sched(nc, es, tag)
                wg, kwg = one(nc, es, "fwg", [128, KC, nf * 128], BF16)
                wu, kwu = one(nc, es, "fwu", [128, KC, nf * 128], BF16)
                wd, kwd = one(nc, es, "fwd", [128, nf, D], BF16)
                S.dma("pool", wg[:], wg_src[:, f0:f0 + nf * 128].rearrange("(kc p) n -> p kc n", p=128), writes=[kwg])
                S.dma("pool", wu[:], wu_src[:, f0:f0 + nf * 128].rearrange("(kc p) n -> p kc n", p=128), writes=[kwu])
                S.dma("pool", wd[:], wd_src[f0:f0 + nf * 128, :].rearrange("(f p) n -> p f n", p=128), writes=[kwd])
                g2 = [one(nc, es, f"g2_{s}", [128, D], F32) for s in range(2)]
                if mode == "dense" or last:
                    for s in range(2):
                        S.dma("sp", g2[s][0][:], modg[l, 1, s], writes=[g2[s][1]])
                hr = Ring(nc, es, "fh", [128, KC, 512], BF16, 2)
                pr = Ring(nc, es, "fps", [128, 2, 512], F32, 2, psum=True)
                po = Ring(nc, es, "fpo", [128, 2, 512], F32, 2, psum=True)
                sr = Ring(nc, es, "fsg", [128, 512], F32, 2)
                ar = Ring(nc, es, "fact", [128, nf, 512], BF16, 2)
                xr = Ring(nc, es, "fx", [128, D], F32, 2)
                yr = Ring(nc, es, "fy", [128, D], F32, 2)
                for ci in range(NCH):
                    t0 = ci * 512
                    s = slot_of(t0)
                    ht, kh = hr.next()
                    S.dma("sp", ht[:], hT[:, :, t0:t0 + 512], writes=[kh])
                    at, ka = ar.next()
                    for f in range(nf):
                        ps, kp = pr.next()
                        for wi, (ww, kw) in enumerate(((wg, kwg), (wu, kwu))):
                            for kc in range(KC):
                                S.op("pe", lambda P, ps=ps, ht=ht, ww=ww, wi=wi, f=f, kc=kc: P.matmul(ps[:, wi, :], ww[:, kc, f * 128:(f + 1) * 128], ht[:, kc, :], start=(kc == 0), stop=(kc == KC - 1)),
                                     reads=[kw, kh], writes=[kp], sig=(wi == 1 and kc == KC - 1))
                        sg, ksg = sr.next()
                        S.op("act", lambda A, sg=sg, ps=ps: A.activation(out=sg[:], in_=ps[:, 0, :], func=AF.Silu), reads=[kp], writes=[ksg])
                        S.op("dve", lambda V, at=at, f=f, sg=sg, ps=ps: V.tensor_tensor(out=at[:, f, :], in0=ps[:, 1, :], in1=sg[:], op=ALU.mult), reads=[kp, ksg], writes=[ka])
                    for tt in range(4):
                        r0 = t0 + tt * 128
                        ps, kp = po.next()
                        for h in range(2):
                            for f in range(nf):
                                S.op("pe", lambda P, ps=ps, at=at, tt=tt, f=f, h=h: P.matmul(ps[:, h, :], at[:, f, tt * 128:(tt + 1) * 128], wd[:, f, h * 512:(h + 1) * 512], start=(f == 0), stop=(f == nf - 1)),
                                     reads=[ka, kwd], writes=[kp], sig=(h == 1 and f == nf - 1))
                        psf = ps[:]
                        v3 = lambda t: t[:].rearrange("p (a b) -> p a b", a=2)
                        yt, ky = yr.next()
                        xt, kx = xr.next()
                        if mode == "dense":
                            S.dma("sp", xt[:], xres[r0:r0 + 128, :], writes=[kx])
                            S.op("dve", lambda V, yt=yt, psf=psf, s=s: V.tensor_tensor(out=v3(yt), in0=psf, in1=v3(g2[s][0]), op=ALU.mult), reads=[kp, g2[s][1]], writes=[ky])
                            S.op("pool", lambda G, yt=yt, xt=xt: G.tensor_tensor(out=yt[:], in0=yt[:], in1=xt[:], op=ALU.add), reads=[ky, kx], writes=[ky])
                            S.dma("sp", xres[r0:r0 + 128, :], yt[:], reads=[ky])
                        else:
                            gcol = gates_t[0][:, r0 // 128, e_idx:e_idx + 1]
                            if first:
                                S.op("dve", lambda V, yt=yt, psf=psf, gcol=gcol: V.tensor_scalar(out=v3(yt), in0=psf, scalar1=gcol, scalar2=None, op0=ALU.mult),
                                     reads=[kp, gates_t[1]], writes=[ky])
                            else:
                                S.dma("sp", xt[:], yacc[r0:r0 + 128, :], writes=[kx])
                                S.op("dve", lambda V, yt=yt, psf=psf, gcol=gcol, xt=xt: V.scalar_tensor_tensor(out=v3(yt), in0=psf, scalar=gcol, in1=v3(xt), op0=ALU.mult, op1=ALU.add),
                                     reads=[kp, gates_t[1], kx], writes=[ky])
                            if last:
                                x2, kx2 = xr.next()
                                S.dma("sp", x2[:], xres[r0:r0 + 128, :], writes=[kx2])
                                S.op("pool", lambda G, yt=yt, s=s: G.tensor_tensor(out=yt[:], in0=yt[:], in1=g2[s][0][:], op=ALU.mult), reads=[ky, g2[s][1]], writes=[ky])
                                S.op("pool", lambda G, yt=yt, x2=x2: G.tensor_tensor(out=yt[:], in0=yt[:], in1=x2[:], op=ALU.add), reads=[ky, kx2], writes=[ky])
                                S.dma("sp", xres[r0:r0 + 128, :], yt[:], reads=[ky])
                            else:
                                S.dma("sp", yacc[r0:r0 + 128, :], yt[:], reads=[ky])
                S.emit()

        def router_phase(tag, l, gates_t):
            li = l // 2
            with ExitStack() as es:
                S = Sched(nc, es, tag)
                rw, krw = one(nc, es, "rw", [128, KC, NE], F32)
                with nc.allow_non_contiguous_dma("router weights are tiny"):
                    S.dma("sp", rw[:], router_w[li].rearrange("(kc p) e -> p kc e", p=128), writes=[krw])
                idf, kidf = one(nc, es, "idf", [128, 128], F32)
                S.dma("sp", idf[:], cmats[:, 0, :], writes=[kidf])
                xr = Ring(nc, es, "rx", [128, D], F32, 3)
                jr = Ring(nc, es, "rjunk", [128, D], BF16, 1)
                sr = Ring(nc, es, "rss", [128, 4], F32, 4)
                pT = Ring(nc, es, "rT", [128, KC, 128], F32, 2, psum=True)
                hr = Ring(nc, es, "rh", [128, KC, 128], F32, 2)
                pl = Ring(nc, es, "rpl", [128, 512], F32, 2, psum=True)
                lr = Ring(nc, es, "rl", [128, 4, NE], F32, 3)
                mr = Ring(nc, es, "rm", [128, 8], F32, 3)
                gt, kg = gates_t
                for ti in range(T // 128):
                    r0 = ti * 128
                    s = slot_of(r0)
                    xt, kx = xr.next()
                    S.dma("sp", xt[:], xres[r0:r0 + 128, :], writes=[kx])
                    jt, kj = jr.next()
                    st, ks = sr.next()
                    S.op("dve", lambda V, st=st: V.memset(st[:, 0:1], 0.0), writes=[ks])
                    S.op("act", lambda A, jt=jt, xt=xt, st=st: A.activation(out=jt[:], in_=xt[:], func=AF.Square, accum_out=st[:, 0:1]), reads=[kx], writes=[kj, ks])
                    S.op("act", lambda A, st=st: A.activation(out=st[:, 1:2], in_=st[:, 0:1], func=AF.Sqrt, scale=1.0 / D, bias=EPS), reads=[ks], writes=[ks])
                    S.op("dve", lambda V, st=st: V.reciprocal(st[:, 2:3], st[:, 1:2]), reads=[ks], writes=[ks])
                    S.op("pool", lambda G, xt=xt, st=st: G.tensor_scalar(out=xt[:], in0=xt[:], scalar1=st[:, 2:3], scalar2=None, op0=ALU.mult), reads=[kx, ks], writes=[kx])
                    pt, kpt = pT.next()
                    for c in range(KC):
                        S.op("pe", lambda P, pt=pt, xt=xt, c=c: P.transpose(pt[:, c, :], xt[:, c * 128:(c + 1) * 128], idf[:]), reads=[kx, kidf], writes=[kpt], sig=(c == KC - 1))
                    ht, kh = hr.next()
                    for c in range(KC):
                        S.op("dve", lambda V, ht=ht, pt=pt, c=c, s=s: V.tensor_scalar(out=ht[:, c, :], in0=pt[:, c, :], scalar1=modf[:, l, 3, c, s:s + 1], scalar2=modf[:, l, 2, c, s:s + 1],
                                                                                     op0=ALU.mult, op1=ALU.add), reads=[kpt], writes=[kh])
                    lg, klg = pl.next()
                    for kc in range(KC):
                        S.op("pe", lambda P, lg=lg, ht=ht, kc=kc: P.matmul(lg[:, 0:NE], ht[:, kc, :], rw[:, kc, :], start=(kc == 0), stop=(kc == KC - 1)),
                             reads=[kh, krw], writes=[klg], sig=(kc == KC - 1))
                    lt, klt = lr.next()
                    mt, kmt = mr.next()
                    S.op("dve", lambda V, lt=lt, lg=lg: V.tensor_copy(lt[:, 0, :], lg[:, 0:NE]), reads=[klg], writes=[klt])
                    S.op("dve", lambda V, lt=lt, mt=mt: V.tensor_reduce(out=mt[:, 0:1], in_=lt[:, 0, :], axis=AX.X, op=ALU.max), reads=[klt], writes=[kmt])
                    S.op("dve", lambda V, lt=lt, mt=mt: V.tensor_scalar(out=lt[:, 1, :], in0=lt[:, 0, :], scalar1=mt[:, 0:1], scalar2=None, op0=ALU.is_ge), reads=[klt, kmt], writes=[klt])
                    S.op("dve", lambda V, lt=lt: V.scalar_tensor_tensor(out=lt[:, 2, :], in0=lt[:, 1, :], scalar=-1e30, in1=lt[:, 0, :], op0=ALU.mult, op1=ALU.add), reads=[klt], writes=[klt])
                    S.op("dve", lambda V, lt=lt, mt=mt: V.tensor_reduce(out=mt[:, 1:2], in_=lt[:, 2, :], axis=AX.X, op=ALU.max), reads=[klt], writes=[kmt])
                    S.op("dve", lambda V, lt=lt, mt=mt: V.tensor_scalar(out=lt[:, 3, :], in0=lt[:, 0, :], scalar1=mt[:, 1:2], scalar2=None, op0=ALU.is_ge), reads=[klt, kmt], writes=[klt])
                    S.op("dve", lambda V, mt=mt: V.tensor_scalar(out=mt[:, 2:3], in0=mt[:, 0:1], scalar1=-1.0, scalar2=None, op0=ALU.mult), reads=[kmt], writes=[kmt])
                    S.op("act", lambda A, lt=lt, mt=mt: A.activation(out=lt[:, 1, :], in_=lt[:, 0, :], func=AF.Exp, bias=mt[:, 2:3], scale=1.0), reads=[klt, kmt], writes=[klt])
                    S.op("dve", lambda V, lt=lt: V.tensor_tensor(out=lt[:, 2, :], in0=lt[:, 1, :], in1=lt[:, 3, :], op=ALU.mult), reads=[klt], writes=[klt])
                    S.op("dve", lambda V, lt=lt, mt=mt: V.tensor_reduce(out=mt[:, 3:4], in_=lt[:, 2, :], axis=AX.X, op=ALU.add), reads=[klt], writes=[kmt])
                    S.op("dve", lambda V, mt=mt: V.reciprocal(mt[:, 4:5], mt[:, 3:4]), reads=[kmt], writes=[kmt])
                    S.op("dve", lambda V, lt=lt, mt=mt, ti=ti: V.tensor_scalar(out=gt[:, ti, :], in0=lt[:, 2, :], scalar1=mt[:, 4:5], scalar2=None, op0=ALU.mult), reads=[klt, kmt], writes=[kg])
                S.emit()

        def final_phase(tag, src):
            with ExitStack() as es:
                S = Sched(nc, es, tag)
                fb, kfb = one(nc, es, "fnb", [128, D], F32)
                S.dma("sp", fb[:], final_norm.partition_broadcast(128), writes=[kfb])
                xr = Ring(nc, es, "zx", [128, D], F32, 3)
                jr = Ring(nc, es, "zjunk", [128, D], BF16, 1)
                sr = Ring(nc, es, "zss", [128, 4], F32, 4)
                for ti in range(T // 128):
                    r0 = ti * 128
                    xt, kx = xr.next()
                    S.dma("sp", xt[:], src[r0:r0 + 128, :], writes=[kx])
                    jt, kj = jr.next()
                    st, ks = sr.next()
                    S.op("dve", lambda V, st=st: V.memset(st[:, 0:1], 0.0), writes=[ks])
                    S.op("act", lambda A, jt=jt, xt=xt, st=st: A.activation(out=jt[:], in_=xt[:], func=AF.Square, accum_out=st[:, 0:1]), reads=[kx], writes=[kj, ks])
                    S.op("act", lambda A, st=st: A.activation(out=st[:, 1:2], in_=st[:, 0:1], func=AF.Sqrt, scale=1.0 / D, bias=EPS), reads=[ks], writes=[ks])
                    S.op("dve", lambda V, st=st: V.reciprocal(st[:, 2:3], st[:, 1:2]), reads=[ks], writes=[ks])
                    S.op("dve", lambda V, xt=xt, st=st: V.scalar_tensor_tensor(out=xt[:], in0=xt[:], scalar=st[:, 2:3], in1=fb[:], op0=ALU.mult, op1=ALU.mult), reads=[kx, ks, kfb], writes=[kx])
                    S.dma("sp", y_out[r0:r0 + 128, :], xt[:], reads=[kx])
                S.emit()

        gates_tile = gs.enter_context(nc.sbuf_tensor("gatesw", [128, T // 128, NE], F32))
        gates_t = (gates_tile, Trk())
        for l in range(L):
            src = x_in if l == 0 else xres
            norm_phase(f"n{l}a", src, l, 0, 1)
            proj_phase(f"p{l}", l)
            attn_phase(f"a{l}", l)
            outproj_phase(f"o{l}", l, src)
            norm_phase(f"n{l}b", (x_in if os.environ.get("KXIN") else xres), l, 2, 3)
            if l % 2 == 0:
                li = l // 2
                nf_all = DFF // 128
                halves = [(0, nf_all // 2), (nf_all // 2, nf_all - nf_all // 2)]
                for hi, (fs, nf) in enumerate(halves):
                    ffn_pass(f"f{l}_{hi}", l, ffn_w_gate[li], ffn_w_up[li], ffn_w_down[li], fs * 128, nf, "dense", 0, None, False, False)
            else:
                li = l // 2
                gates_t = (gates_tile, Trk())
                router_phase(f"r{l}", l, gates_t)
                nf_all = DFE // 128
                npart = 4 if nf_all % 4 == 0 else 2
                halves = [(i * (nf_all // npart), nf_all // npart) for i in range(npart)]
                for e in range(NE):
                    for hi, (fs, nf) in enumerate(halves):
                        first = (e == 0 and hi == 0)
                        last = (e == NE - 1 and hi == len(halves) - 1)
                        ffn_pass(f"m{l}_{e}_{hi}", l, moe_w_gate[li, e], moe_w_up[li, e], moe_w_down[li, e], fs * 128, nf, "moe", e,
                                 (gates_tile, Trk()), first, last)
        final_phase("fin", xres)
    return nc


def _consts():
    ident = np.eye(128, dtype=np.float32)
    pd = np.zeros((128, 128), np.float32)
    pg = np.zeros((128, 128), np.float32)
    for b in range(2):
        o = b * 64
        for i in range(8):
            pd[o + i + 8, o + i] = 1.0
            pd[o + i, o + i + 8] = 1.0
        for base in (0, 32):
            for i in range(16):
                pg[o + base + i + 16, o + base + i] = 1.0
                pg[o + base + i, o + base + i + 16] = 1.0
    bones = np.zeros((128, 128), np.float32)
    bones[0:64, 0:64] = 1.0
    bones[64:128, 64:128] = 1.0
    ones = np.ones((128, 128), np.float32)
    return np.ascontiguousarray(np.stack([ident, pd, pg, bones, ones], axis=1))


def _rope_tables(pos):
    pos = np.asarray(pos)
    n = pos.shape[0]
    tab = np.zeros((4, 128, n), np.float32)
    tab[0] = 1.0
    posf = pos.astype(np.float32)
    rowf = (pos // GRID_W).astype(np.float32)
    colf = (pos % GRID_W).astype(np.float32)
    invd = np.exp(-math.log(DIFF_THETA) * np.arange(8, dtype=np.float32) / 8).astype(np.float32)
    invg = np.exp(-math.log(AX_THETA) * np.arange(16, dtype=np.float32) / 16).astype(np.float32)
    for p in range(128):
        d = p % 64
        if d < 16:
            ang = posf * invd[d % 8]
            tab[0, p] = np.cos(ang)
            tab[1, p] = -np.sin(ang) if d < 8 else np.sin(ang)
        if d < 32:
            ang = rowf * invg[d % 16]
            sgn = -1.0 if d < 16 else 1.0
        else:
            ang = colf * invg[(d - 32) % 16]
            sgn = -1.0 if d < 48 else 1.0
        tab[2, p] = np.cos(ang)
        tab[3, p] = sgn * np.sin(ang)
    return tab


_WNAMES = ["w_ada", "b_ada", "norm1", "w_in", "diff_lambda", "diff_subln", "gqa_q_norm", "gqa_k_norm", "w_out", "norm2",
           "ffn_w_gate", "ffn_w_up", "ffn_w_down", "router_w", "moe_w_gate", "moe_w_up", "moe_w_down", "final_norm"]


def kernel(**inputs):
    xp = np.asarray(inputs["x_prompt"], np.float32)
    xs = np.asarray(inputs["x_sample"], np.float32)
    cp = np.asarray(inputs["c_prompt"], np.float32)
    cs = np.asarray(inputs["c_sample"], np.float32)
    B1, S1, _ = xp.shape
    B2, S2, _ = xs.shape
    L = inputs["w_in"].shape[0]
    DFF = inputs["ffn_w_gate"].shape[2]
    DFE = inputs["moe_w_gate"].shape[3]
    n = 8
    per = n // B2
    S2L = S2
    cfg = Cfg(L, S1, S2, S2L, DFF, DFE)
    nc = build(cfg)
    cm = _consts()
    tab = np.ascontiguousarray(np.concatenate([_rope_tables(np.arange(S1)), _rope_tables(np.arange(S2L))], axis=2))
    wts = {k: np.ascontiguousarray(np.asarray(inputs[k], np.float32)) for k in _WNAMES}
    in_maps = []
    for c in range(n):
        b2 = c // per
        m = {"x": np.ascontiguousarray(np.concatenate([xp[c % B1], xs[b2]], axis=0)),
             "cvec": np.ascontiguousarray(np.stack([cp[c % B1], cs[b2]], axis=0)),
             "cmats": cm, "tabs": tab}
        m.update(wts)
        in_maps.append(m)
    res = run_bass_kernel_spmd(nc, in_maps, core_ids=list(range(n)))
    outs = [np.asarray(r["y"], np.float32) for r in res.results]
    y_prompt = np.stack([outs[c][:S1] for c in range(B1)], axis=0)
    y_sample = np.stack([outs[b * per][S1:S1 + S2] for b in range(B2)], axis=0)
    return (y_prompt, y_sample)
```

```python
import math
from contextlib import ExitStack

import numpy as np
import concourse.bass as bass
import concourse.mybir as mybir
from concourse.bass_utils import run_bass_kernel_spmd

F32 = mybir.dt.float32
BF16 = mybir.dt.bfloat16
AF = mybir.ActivationFunctionType
ALU = mybir.AluOpType
AX = mybir.AxisListType

D = 1024
KC = 8
EPS = 1e-6
DIFF_THETA = 500000.0
AX_THETA = 10000.0
GRID_W = 64
NE = 8


class Cfg:
    def __init__(self, L, T, DFF, DFE):
        self.L, self.T, self.DFF, self.DFE = L, T, DFF, DFE
        self.HALF = T // 2


class Trk:
    __slots__ = ("w", "r")

    def __init__(self):
        self.w = []
        self.r = {}


class Sched:
    ENG = ("pe", "act", "dve", "pool", "sp")
    ND = 6

    _shared = None

    def __init__(self, nc, es, tag):
        self.nc = nc
        self.lists = {e: [] for e in self.ENG}
        self.pend = {e: [] for e in self.ENG}
        sh = Sched._shared
        if sh is None or sh["nc"] is not nc:
            gs = Sched._gs
            sh = {"nc": nc,
                  "sem": {e: gs.enter_context(nc.semaphore(f"s_{e}")) for e in self.ENG},
                  "cnt": {e: 0 for e in self.ENG},
                  "waited": {e: {} for e in self.ENG},
                  "dsem": {q: [gs.enter_context(nc.semaphore(f"d_{q}{i}")) for i in range(self.ND)] for q in ("sp", "pool")},
                  "dcnt": {q: [0] * self.ND for q in ("sp", "pool")},
                  "dlast": {q: [None] * self.ND for q in ("sp", "pool")},
                  "dnext": {q: 0 for q in ("sp", "pool")}}
            Sched._shared = sh
        self.sem, self.cnt, self.waited = sh["sem"], sh["cnt"], sh["waited"]
        self.dsem, self.dcnt, self.dlast, self.dnext = sh["dsem"], sh["dcnt"], sh["dlast"], sh["dnext"]

    def _wait(self, e, toks):
        for t in toks:
            if t is None:
                continue
            sem, val = t
            k = id(sem)
            if self.waited[e].get(k, 0) < val:
                self.waited[e][k] = val
                self.lists[e].append(lambda E, sem=sem, val=val: E.wait_ge(sem, val))

    @staticmethod
    def _deps(reads, writes):
        deps = []
        for b in reads:
            deps += b.w
        for b in writes:
            deps += b.w
            deps += list(b.r.values())
        return deps

    @staticmethod
    def _commit(tok, reads, writes):
        k = id(tok[0])
        for b in reads:
            b.r[k] = tok
        for b in writes:
            b.w = [tok]
            b.r = {}

    def op(self, e, fn, reads=(), writes=(), sig=True):
        self._wait(e, self._deps(reads, writes))
        self.pend[e].append((reads, writes))
        if not sig:
            self.lists[e].append(fn)
            return None
        self.cnt[e] += 1
        sem = self.sem[e]
        tok = (sem, self.cnt[e])
        self.lists[e].append(lambda E, fn=fn, sem=sem: fn(E).then_inc(sem, 1))
        for rs, ws in self.pend[e]:
            self._commit(tok, rs, ws)
        self.pend[e] = []
        return tok

    def dma(self, q, out, in_, reads=(), writes=()):
        i = self.dnext[q]
        self.dnext[q] = (i + 1) % self.ND
        self._wait(q, self._deps(reads, writes) + [self.dlast[q][i]])
        self.dcnt[q][i] += 16
        sem = self.dsem[q][i]
        tok = (sem, self.dcnt[q][i])
        self.dlast[q][i] = tok
        self.lists[q].append(lambda E, out=out, in_=in_, sem=sem: E.dma_start(out=out, in_=in_).then_inc(sem, 16))
        self._commit(tok, reads, writes)
        return tok

    def emit(self):
        allt = [t for q in ("sp", "pool") for t in self.dlast[q]]
        allt += [(self.sem[e], self.cnt[e]) for e in self.ENG if self.cnt[e] > 0 and e != "sp"]
        for e in self.ENG:
            self._wait(e, allt)
        with self.nc.Block() as blk:
            def run(key):
                def f(E):
                    for g in self.lists[key]:
                        g(E)
                return f
            blk.tensor(run("pe"))
            blk.scalar(run("act"))
            blk.vector(run("dve"))
            blk.gpsimd(run("pool"))
            blk.sync(run("sp"))


_UID = [0]


class Ring:
    def __init__(self, nc, es, name, shape, dtype, n, psum=False):
        mk = nc.psum_tensor if psum else nc.sbuf_tensor
        _UID[0] += 1
        self.t = [es.enter_context(mk(f"{name}_{_UID[0]}_{i}", shape, dtype)) for i in range(n)]
        self.k = [Trk() for _ in range(n)]
        self.i = 0

    def next(self):
        i = self.i
        self.i = (i + 1) % len(self.t)
        return self.t[i], self.k[i]


def one(nc, es, name, shape, dtype, psum=False):
    r = Ring(nc, es, name, shape, dtype, 1, psum)
    return r.t[0], r.k[0]


def build(cfg):
    nc = bass.Bass("TRN2", target_bir_lowering=False)
    L, T, DFF, DFE, HALF = cfg.L, cfg.T, cfg.DFF, cfg.DFE, cfg.HALF
    TK = T
    NCH = T // 512

    def din(name, shape):
        return nc.dram_tensor(name, list(shape), F32, kind="ExternalInput").ap()

    x_in = din("x", [T, D])
    cvec = din("cvec", [2, D])
    cmats = din("cmats", [128, 5, 128])
    tabs = din("tabs", [4, 128, T])
    maskb = din("maskb", [128, (T // 128) * (T // 512)])
    w_ada = din("w_ada", [L, D, 6 * D])
    b_ada = din("b_ada", [L, 6 * D])
    norm1 = din("norm1", [L, D])
    w_in = din("w_in", [L, D, 6656])
    diff_lambda = din("diff_lambda", [L, 4, 64])
    diff_subln = din("diff_subln", [L, 128])
    gqa_q_norm = din("gqa_q_norm", [L, 64])
    gqa_k_norm = din("gqa_k_norm", [L, 64])
    w_out = din("w_out", [L, D, D])
    norm2 = din("norm2", [L, D])
    ND_, NM_ = (L + 1) // 2, L // 2
    ffn_w_gate = din("ffn_w_gate", [ND_, D, DFF])
    ffn_w_up = din("ffn_w_up", [ND_, D, DFF])
    ffn_w_down = din("ffn_w_down", [ND_, DFF, D])
    router_w = din("router_w", [max(NM_, 1), D, NE])
    moe_w_gate = din("moe_w_gate", [max(NM_, 1), NE, D, DFE])
    moe_w_up = din("moe_w_up", [max(NM_, 1), NE, D, DFE])
    moe_w_down = din("moe_w_down", [max(NM_, 1), NE, DFE, D])
    final_norm = din("final_norm", [D])
    y_out = nc.dram_tensor("y", [T, D], F32, kind="ExternalOutput").ap()

    def scr(name, shape, dt):
        return nc.dram_tensor(name, list(shape), dt).ap()

    xres = scr("xres", [T, D], F32)
    hT = scr("hT", [128, KC, T], BF16)
    QT = scr("QT", [16, 128, T], BF16)
    KT = scr("KT", [12, 128, TK], BF16)
    Vs = scr("Vs", [TK, 1280], BF16)
    GT = scr("GT", [16, 128, T], BF16)
    mT = scr("mT", [128, KC, T], BF16)
    modg = scr("modg", [L, 2, 2, 128, D], F32)
    yacc = scr("yacc", [T, D], F32)

    def slot_of(tok0):
        return tok0 // HALF

    def key_off(tok0):
        return tok0

    with ExitStack() as gs:
        Sched._gs = gs
        Sched._shared = None
        cm = gs.enter_context(nc.sbuf_tensor("cm", [128, 5, 128], BF16))
        IDENT, PD, PG, BONES, ONES = (cm[:, i, :] for i in range(5))
        modf = gs.enter_context(nc.sbuf_tensor("modf", [128, L, 4, KC, 2], F32))
        fnorm = gs.enter_context(nc.sbuf_tensor("fnorm", [128, KC], F32))
        wsub = gs.enter_context(nc.sbuf_tensor("wsub", [128, L], F32))
        nlam = gs.enter_context(nc.sbuf_tensor("nlam", [128, L], F32))
        gq8 = gs.enter_context(nc.sbuf_tensor("gq8", [128, L], F32))
        gk8 = gs.enter_context(nc.sbuf_tensor("gk8", [128, L], F32))

        with ExitStack() as es, nc.allow_non_contiguous_dma("tiny feature-major parameter loads"):
            S = Sched(nc, es, "p0")
            E = es.enter_context
            kcm, kmodf, kmisc = Trk(), Trk(), Trk()
            S.dma("pool", cm[:], cmats, writes=[kcm])
            cT32, kcT32 = one(nc, es, "cT32", [128, 2, KC], F32)
            cTb, kcTb = one(nc, es, "cTb", [128, 2, KC], BF16)
            crep, kcrep = one(nc, es, "crep", [128, KC, 2, 128], BF16)
            n1, kn1 = one(nc, es, "n1", [128, L, KC], F32)
            n2, kn2 = one(nc, es, "n2", [128, L, KC], F32)
            bfm, kbfm = one(nc, es, "bfm", [128, L, 48], F32)
            sub, ksub = one(nc, es, "sub", [128, L], F32)
            gqn, kgqn = one(nc, es, "gqn", [128, L], F32)
            gkn, kgkn = one(nc, es, "gkn", [128, L], F32)
            dl, kdl = one(nc, es, "dl", [128, L, 4, 64], F32)
            for s_ in range(2):
                S.dma("sp", cT32[:, s_, :], cvec[s_].rearrange("(c p) -> p c", p=128), writes=[kcT32])
            S.dma("sp", n1[:], norm1.rearrange("l (c p) -> p l c", p=128), writes=[kn1])
            S.dma("sp", n2[:], norm2.rearrange("l (c p) -> p l c", p=128), writes=[kn2])
            S.dma("sp", fnorm[:], final_norm.rearrange("(c p) -> p c", p=128), writes=[kmisc])
            S.dma("sp", bfm[:], b_ada.rearrange("l (c p) -> p l c", p=128), writes=[kbfm])
            S.dma("sp", sub[:], diff_subln.rearrange("l p -> p l"), writes=[ksub])
            for hh in range(2):
                S.dma("sp", gqn[hh * 64:(hh + 1) * 64, :], gqa_q_norm.rearrange("l p -> p l"), writes=[kgqn])
                S.dma("sp", gkn[hh * 64:(hh + 1) * 64, :], gqa_k_norm.rearrange("l p -> p l"), writes=[kgkn])
            S.dma("sp", dl[:].rearrange("p l a d -> p (l a d)"),
                  diff_lambda.rearrange("l a d -> (l a d)").partition_broadcast(128), writes=[kdl])
            S.op("act", lambda A: A.activation(out=cTb[:], in_=cT32[:], func=AF.Silu), reads=[kcT32], writes=[kcTb])
            for s in range(2):
                S.op("dve", lambda V, s=s: V.tensor_copy(crep[:, :, s, :], cTb[:, s, :].unsqueeze(2).to_broadcast([128, KC, 128])),
                     reads=[kcTb], writes=[kcrep])
            tmp, ktmp = one(nc, es, "p0tmp", [128, 64], F32)
            sc4, ksc4 = one(nc, es, "sc4", [128, 4], F32)
            for l in range(L):
                lam_init = 0.8 - 0.6 * math.exp(-0.3 * l)
                for m in range(2):
                    S.op("dve", lambda V, l=l, m=m: V.tensor_tensor(out=tmp[:], in0=dl[:, l, 2 * m, :], in1=dl[:, l, 2 * m + 1, :], op=ALU.mult),
                         reads=[kdl], writes=[ktmp])
                    S.op("dve", lambda V, m=m: V.tensor_reduce(out=sc4[:, m:m + 1], in_=tmp[:], axis=AX.X, op=ALU.add),
                         reads=[ktmp], writes=[ksc4])
                S.op("act", lambda A: A.activation(out=sc4[:, 2:4], in_=sc4[:, 0:2], func=AF.Exp), reads=[ksc4], writes=[ksc4])
                S.op("dve", lambda V, l=l, li=lam_init: V.scalar_tensor_tensor(out=nlam[:, l:l + 1], in0=sc4[:, 3:4], scalar=-li, in1=sc4[:, 2:3],
                                                                            op0=ALU.add, op1=ALU.subtract),
                     reads=[ksc4], writes=[kmisc])
                S.op("dve", lambda V, l=l, li=lam_init: V.tensor_scalar(out=wsub[:, l:l + 1], in0=sub[:, l:l + 1], scalar1=(1.0 - li) * math.sqrt(128.0),
                                                                         scalar2=None, op0=ALU.mult),
                     reads=[ksub], writes=[kmisc])
                S.op("dve", lambda V, l=l: V.tensor_scalar(out=gq8[:, l:l + 1], in0=gqn[:, l:l + 1], scalar1=8.0, scalar2=None, op0=ALU.mult),
                     reads=[kgqn], writes=[kmisc])
                S.op("dve", lambda V, l=l: V.tensor_scalar(out=gk8[:, l:l + 1], in0=gkn[:, l:l + 1], scalar1=8.0, scalar2=None, op0=ALU.mult),
                     reads=[kgkn], writes=[kmisc])
            wr = Ring(nc, es, "wada", [128, KC, 512], BF16, 2)
            br = Ring(nc, es, "bbc", [128, 512], F32, 2)
            gr = Ring(nc, es, "gout", [128, 512], F32, 2)
            pr = Ring(nc, es, "p0ps", [128, 512], F32, 2, psum=True)
            V4 = {0: 0, 1: 1, 3: 2, 4: 3}
            for l in range(L):
                for j in range(12):
                    vec, half = j // 2, j % 2
                    wt, kw = wr.next()
                    S.dma("pool", wt[:], w_ada[l, :, j * 512:(j + 1) * 512].rearrange("(kc p) n -> p kc n", p=128), writes=[kw])
                    if vec in (2, 5):
                        which = 0 if vec == 2 else 1
                        bb, kb = br.next()
                        S.dma("sp", bb[:], b_ada[l, j * 512:(j + 1) * 512].partition_broadcast(128), writes=[kb])
                        for s in range(2):
                            ps, kp = pr.next()
                            for kc in range(KC):
                                S.op("pe", lambda P, ps=ps, wt=wt, s=s, kc=kc: P.matmul(ps[:], crep[:, kc, s, :], wt[:, kc, :], start=(kc == 0), stop=(kc == KC - 1)),
                                     reads=[kcrep, kw], writes=[kp], sig=(kc == KC - 1))
                            go, kg = gr.next()
                            S.op("dve", lambda V, go=go, ps=ps, bb=bb: V.tensor_tensor(out=go[:], in0=ps[:], in1=bb[:], op=ALU.add),
                                 reads=[kp, kb], writes=[kg])
                            S.dma("sp", modg[l, which, s, :, half * 512:(half + 1) * 512], go[:], reads=[kg])
                    else:
                        v4 = V4[vec]
                        for cb in range(4):
                            c = half * 4 + cb
                            ps, kp = pr.next()
                            for kc in range(KC):
                                S.op("pe", lambda P, ps=ps, wt=wt, cb=cb, kc=kc: P.matmul(ps[:, 0:2], wt[:, kc, cb * 128:(cb + 1) * 128], cTb[:, :, kc], start=(kc == 0), stop=(kc == KC - 1)),
                                     reads=[kcTb, kw], writes=[kp], sig=(kc == KC - 1))
                            S.op("dve", lambda V, ps=ps, l=l, v4=v4, c=c, vec=vec: V.tensor_scalar(out=modf[:, l, v4, c, :], in0=ps[:, 0:2], scalar1=bfm[:, l, vec * 8 + c:vec * 8 + c + 1],
                                                                                                   scalar2=None, op0=ALU.add),
                                 reads=[kp, kbfm], writes=[kmodf])
                for v4, nn, kn in ((1, n1, kn1), (3, n2, kn2)):
                    for s in range(2):
                        S.op("dve", lambda V, l=l, v4=v4, s=s, nn=nn: V.scalar_tensor_tensor(out=modf[:, l, v4, :, s], in0=modf[:, l, v4, :, s], scalar=1.0, in1=nn[:, l, :],
                                                                                           op0=ALU.add, op1=ALU.mult),
                             reads=[kmodf, kn], writes=[kmodf])
            S.emit()

        def norm_phase(tag, src, l, v_sh, v_s):
            with ExitStack() as es:
                S = Sched(nc, es, tag)
                xr = Ring(nc, es, "nx", [128, D], F32, 3)
                jr = Ring(nc, es, "njunk", [128, D], BF16, 1)
                sr = Ring(nc, es, "nss", [128, 4], F32, 4)
                xnr = Ring(nc, es, "nxn", [128, D], BF16, 3)
                tr = Ring(nc, es, "nT", [128, KC, 512], BF16, 1, psum=True)
                hr = Ring(nc, es, "nh", [128, KC, 512], BF16, 2)
                for ci in range(NCH):
                    s = slot_of(ci * 512)
                    pT, kT = tr.next()
                    for tt in range(4):
                        r0 = ci * 512 + tt * 128
                        xt, kx = xr.next()
                        S.dma("sp", xt[:], src[r0:r0 + 128, :], writes=[kx])
                        jt, kj = jr.next()
                        st, ks = sr.next()
                        S.op("dve", lambda V, st=st: V.memset(st[:, 0:1], 0.0), writes=[ks])
                        S.op("act", lambda A, jt=jt, xt=xt, st=st: A.activation(out=jt[:], in_=xt[:], func=AF.Square, accum_out=st[:, 0:1]),
                             reads=[kx], writes=[kj, ks])
                        S.op("act", lambda A, st=st: A.activation(out=st[:, 1:2], in_=st[:, 0:1], func=AF.Sqrt, scale=1.0 / D, bias=EPS),
                             reads=[ks], writes=[ks])
                        S.op("dve", lambda V, st=st: V.reciprocal(st[:, 2:3], st[:, 1:2]), reads=[ks], writes=[ks])
                        xn, kxn = xnr.next()
                        S.op("pool", lambda G, xn=xn, xt=xt, st=st: G.tensor_scalar(out=xn[:], in0=xt[:], scalar1=st[:, 2:3], scalar2=None, op0=ALU.mult),
                             reads=[kx, ks], writes=[kxn])
                        for c in range(KC):
                            S.op("pe", lambda P, pT=pT, xn=xn, c=c, tt=tt: P.transpose(pT[:, c, tt * 128:(tt + 1) * 128], xn[:, c * 128:(c + 1) * 128], IDENT),
                                 reads=[kxn], writes=[kT], sig=(c == KC - 1))
                    ht, kh = hr.next()
                    for c in range(KC):
                        if c % 2 == 0:
                            S.op("dve", lambda V, ht=ht, pT=pT, c=c, s=s: V.tensor_scalar(out=ht[:, c, :], in0=pT[:, c, :], scalar1=modf[:, l, v_s, c, s:s + 1],
                                                                                         scalar2=modf[:, l, v_sh, c, s:s + 1], op0=ALU.mult, op1=ALU.add),
                                 reads=[kT], writes=[kh])
                        else:
                            S.op("act", lambda A, ht=ht, pT=pT, c=c, s=s: A.activation(out=ht[:, c, :], in_=pT[:, c, :], func=AF.Identity,
                                                                                      scale=modf[:, l, v_s, c, s:s + 1], bias=modf[:, l, v_sh, c, s:s + 1]),
                                 reads=[kT], writes=[kh])
                    S.dma("sp", hT[:, :, ci * 512:(ci + 1) * 512], ht[:], reads=[kh])
                S.emit()

        def proj_phase(tag, l):
            with ExitStack() as es:
                S = Sched(nc, es, tag + "a")
                wq, kwq = one(nc, es, "wq", [128, KC, 3584], BF16)
                wsrc = w_in[l].rearrange("(kc p) n -> p kc n", p=128)
                S.dma("pool", wq[:, :, 0:2048], wsrc[:, :, 0:2048], writes=[kwq])
                S.dma("pool", wq[:, :, 2048:3072], wsrc[:, :, 3072:4096], writes=[kwq])
                for g in range(4):
                    for hh in range(2):
                        c0 = 3072 + g * 128 + hh * 64
                        S.dma("pool", wq[:, :, c0:c0 + 64], wsrc[:, :, 4096 + g * 64:4096 + (g + 1) * 64], writes=[kwq])
                hr = Ring(nc, es, "ph", [128, KC, 512], BF16, 3)
                tbr = Ring(nc, es, "ptab", [128, 4, 512], F32, 3)
                pr = Ring(nc, es, "pps", [128, 512], F32, 4, psum=True)
                p2 = Ring(nc, es, "pps2", [128, 512], F32, 3, psum=True)
                qsr = Ring(nc, es, "pqs", [128, 512], BF16, 8)
                sqr = Ring(nc, es, "psq", [128, 512], BF16, 3)
                rrr = Ring(nc, es, "prr", [128, 512], F32, 3)
                t1r = Ring(nc, es, "pt1", [128, 512], F32, 4)
                t2r = Ring(nc, es, "pt2", [128, 512], F32, 4)
                outr = Ring(nc, es, "pout", [128, 512], BF16, 4)
                def blk_gen(blk, ht, kh, tb, ktb, t0, k0):
                    ps, kp = pr.next()
                    for kc in range(KC):
                        S.op("pe", lambda P, ps=ps, ht=ht, blk=blk, kc=kc: P.matmul(ps[:], wq[:, kc, blk * 128:(blk + 1) * 128], ht[:, kc, :], start=(kc == 0), stop=(kc == KC - 1)),
                             reads=[kwq, kh], writes=[kp], sig=(kc == KC - 1))
                    yield
                    qs, kq = qsr.next()
                    if blk < 16:
                        S.op("act", lambda A, qs=qs, ps=ps: A.copy(qs[:], ps[:]), reads=[kp], writes=[kq])
                        PM, ti = PD, 0
                        dst = QT[blk, :, t0:t0 + 512] if blk < 8 else KT[blk - 8, :, k0:k0 + 512]
                        yield
                    else:
                        sq, ksq = sqr.next()
                        S.op("act", lambda A, sq=sq, ps=ps: A.activation(out=sq[:], in_=ps[:], func=AF.Square), reads=[kp], writes=[ksq])
                        yield
                        pss, kps = p2.next()
                        S.op("pe", lambda P, pss=pss, sq=sq: P.matmul(pss[:], BONES, sq[:], start=True, stop=True), reads=[ksq], writes=[kps])
                        yield
                        rr, krr = rrr.next()
                        S.op("act", lambda A, rr=rr, pss=pss: A.activation(out=rr[:], in_=pss[:], func=AF.Sqrt, scale=1.0, bias=64.0 * EPS),
                             reads=[kps], writes=[krr])
                        S.op("dve", lambda V, rr=rr: V.reciprocal(rr[:], rr[:]), reads=[krr], writes=[krr])
                        gv = gq8 if blk < 24 else gk8
                        S.op("dve", lambda V, qs=qs, ps=ps, rr=rr, gv=gv: V.scalar_tensor_tensor(out=qs[:], in0=ps[:], scalar=gv[:, l:l + 1], in1=rr[:], op0=ALU.mult, op1=ALU.mult),
                             reads=[kp, krr], writes=[kq])
                        PM, ti = PG, 2
                        dst = QT[8 + blk - 16, :, t0:t0 + 512] if blk < 24 else KT[8 + blk - 24, :, k0:k0 + 512]
                        yield
                    pp, kpp = p2.next()
                    S.op("pe", lambda P, pp=pp, qs=qs, PM=PM: P.matmul(pp[:], PM, qs[:], start=True, stop=True), reads=[kq], writes=[kpp])
                    yield
                    t1, kt1 = t1r.next()
                    S.op("pool", lambda G, t1=t1, qs=qs, tb=tb, ti=ti: G.tensor_tensor(out=t1[:], in0=qs[:], in1=tb[:, ti, :], op=ALU.mult),
                         reads=[kq, ktb], writes=[kt1])
                    t2, kt2 = t2r.next()
                    S.op("dve", lambda V, t2=t2, pp=pp, tb=tb, ti=ti: V.tensor_tensor(out=t2[:], in0=pp[:], in1=tb[:, ti + 1, :], op=ALU.mult),
                         reads=[kpp, ktb], writes=[kt2])
                    yield
                    ot, ko = outr.next()
                    S.op("pool", lambda G, ot=ot, t1=t1, t2=t2: G.tensor_tensor(out=ot[:], in0=t1[:], in1=t2[:], op=ALU.add),
                         reads=[kt1, kt2], writes=[ko])
                    S.dma("sp", dst, ot[:], reads=[ko])

                active = []

                def step_all():
                    for g_ in list(active):
                        try:
                            next(g_)
                        except StopIteration:
                            active.remove(g_)

                for ci in range(NCH):
                    t0 = ci * 512
                    k0 = key_off(t0)
                    ht, kh = hr.next()
                    S.dma("sp", ht[:], hT[:, :, t0:t0 + 512], writes=[kh])
                    tb, ktb = tbr.next()
                    S.dma("sp", tb[:], tabs[:, :, t0:t0 + 512].rearrange("a p t -> p a t"), writes=[ktb])
                    for blk in range(28):
                        g_ = blk_gen(blk, ht, kh, tb, ktb, t0, k0)
                        next(g_)
                        step_all()
                        active.append(g_)
                while active:
                    step_all()
                S.emit()
            with ExitStack() as es:
                S = Sched(nc, es, tag + "b")
                wg, kwg = one(nc, es, "wg", [128, KC, 3328], BF16)
                wsrc = w_in[l].rearrange("(kc p) n -> p kc n", p=128)
                S.dma("pool", wg[:, :, 0:2048], wsrc[:, :, 4608:6656], writes=[kwg])
                S.dma("pool", wg[:, :, 2048:3072], wsrc[:, :, 2048:3072], writes=[kwg])
                S.dma("pool", wg[:, :, 3072:3328], wsrc[:, :, 4352:4608], writes=[kwg])
                hr = Ring(nc, es, "qh", [128, KC, 512], BF16, 2)
                pr = Ring(nc, es, "qps", [128, 512], F32, 6, psum=True)
                outr = Ring(nc, es, "qout", [128, 512], BF16, 4)
                vr = Ring(nc, es, "qv", [128, 1280], BF16, 3)
                for ci in range(NCH):
                    t0 = ci * 512
                    k0 = key_off(t0)
                    ht, kh = hr.next()
                    S.dma("sp", ht[:], hT[:, :, t0:t0 + 512], writes=[kh])
                    for blk in range(16):
                        ps, kp = pr.next()
                        for kc in range(KC):
                            S.op("pe", lambda P, ps=ps, ht=ht, blk=blk, kc=kc: P.matmul(ps[:], wg[:, kc, blk * 128:(blk + 1) * 128], ht[:, kc, :], start=(kc == 0), stop=(kc == KC - 1)),
                                 reads=[kwg, kh], writes=[kp], sig=(kc == KC - 1))
                        ot, ko = outr.next()
                        S.op("act", lambda A, ot=ot, ps=ps: A.activation(out=ot[:], in_=ps[:], func=AF.Sigmoid), reads=[kp], writes=[ko])
                        S.dma("sp", GT[blk, :, t0:t0 + 512], ot[:], reads=[ko])
                    for tt in range(4):
                        vt, kv = vr.next()
                        for n0, nn in ((0, 512), (512, 512), (1024, 256)):
                            ps, kp = pr.next()
                            for kc in range(KC):
                                S.op("pe", lambda P, ps=ps, ht=ht, tt=tt, kc=kc, n0=n0, nn=nn: P.matmul(ps[:, 0:nn], ht[:, kc, tt * 128:(tt + 1) * 128], wg[:, kc, 2048 + n0:2048 + n0 + nn],
                                                                                                        start=(kc == 0), stop=(kc == KC - 1)),
                                     reads=[kwg, kh], writes=[kp], sig=(kc == KC - 1))
                            eng = "dve" if n0 == 512 else "act"
                            if eng == "dve":
                                S.op("dve", lambda V, vt=vt, ps=ps, n0=n0, nn=nn: V.tensor_copy(vt[:, n0:n0 + nn], ps[:, 0:nn]), reads=[kp], writes=[kv])
                            else:
                                S.op("act", lambda A, vt=vt, ps=ps, n0=n0, nn=nn: A.copy(vt[:, n0:n0 + nn], ps[:, 0:nn]), reads=[kp], writes=[kv])
                        S.dma("sp", Vs[k0 + tt * 128:k0 + (tt + 1) * 128, :], vt[:], reads=[kv])
                S.emit()

        def attn_phase(tag, l):
            with ExitStack() as es:
                S = Sched(nc, es, tag)
                SKM = T
                NQC = T // 512
                mb, kmb = one(nc, es, "amb", [128, (T // 128) * NQC], F32)
                S.dma("sp", mb[:], maskb, writes=[kmb])
                ktd = Ring(nc, es, "aktd", [128, SKM], BF16, 2)
                ktg = Ring(nc, es, "aktg", [128, SKM], BF16, 2)
                vdr = Ring(nc, es, "avd", [128, SKM // 128, 128], BF16, 2)
                vgr = Ring(nc, es, "avg", [128, SKM // 128, 64], BF16, 2)
                qdr = Ring(nc, es, "aqd", [128, 512], BF16, 2)
                qgr = Ring(nc, es, "aqg", [128, 512], BF16, 2)
                gar = Ring(nc, es, "aga", [128, 512], BF16, 2)
                gbr = Ring(nc, es, "agb", [128, 512], BF16, 2)
                scr_ = Ring(nc, es, "asc", [128, 2, 512], F32, 2, psum=True)
                acc = Ring(nc, es, "aacc", [128, 512], F32, 4, psum=True)
                er = Ring(nc, es, "ae", [128, 2, 512], BF16, 3)
                fr = Ring(nc, es, "af", [128, 512], F32, 8)
                sqr = Ring(nc, es, "asq", [128, 512], BF16, 2)
                mor = Ring(nc, es, "amo", [128, 512], BF16, 2)
                for s in range(1):
                    SK = T
                    kb = 0
                    q_lo, q_hi = 0, T
                    nkt = SK // 128
                    kg_t = vg_t = None
                    for j in range(8):
                        kd, kkd = ktd.next()
                        S.dma("sp", kd[:, 0:SK], KT[j, :, kb:kb + SK], writes=[kkd])
                        vd, kvd = vdr.next()
                        S.dma("sp", vd[:, 0:nkt, :], Vs[kb:kb + SK, j * 128:(j + 1) * 128].rearrange("(kt p) e -> p kt e", p=128), writes=[kvd])
                        if j % 2 == 0:
                            g = j // 2
                            kg_t, kkg = ktg.next()
                            S.dma("sp", kg_t[:, 0:SK], KT[8 + g, :, kb:kb + SK], writes=[kkg])
                            vg_t, kvg = vgr.next()
                            S.dma("sp", vg_t[:, 0:nkt, :], Vs[kb:kb + SK, 1024 + g * 64:1024 + (g + 1) * 64].rearrange("(kt p) e -> p kt e", p=128), writes=[kvg])
                        for t0 in range(q_lo, q_hi, 512):
                            qc = t0 // 512
                            qd, kqd = qdr.next()
                            S.dma("sp", qd[:], QT[j, :, t0:t0 + 512], writes=[kqd])
                            qg, kqg = qgr.next()
                            S.dma("sp", qg[:], QT[8 + j, :, t0:t0 + 512], writes=[kqg])
                            ga, kga = gar.next()
                            S.dma("sp", ga[:], GT[j, :, t0:t0 + 512], writes=[kga])
                            gb, kgb = gbr.next()
                            S.dma("sp", gb[:], GT[8 + j, :, t0:t0 + 512], writes=[kgb])
                            ov1, ko1 = acc.next()
                            ov2, ko2 = acc.next()
                            rb1, kr1 = acc.next()
                            rb2, kr2 = acc.next()
                            def qk_d(kt, kd=kd, qd=qd, kkd=kkd, kqd=kqd):
                                sc, ksc = scr_.next()
                                for m in range(2):
                                    S.op("pe", lambda P, sc=sc, kt=kt, m=m: P.matmul(sc[:, m, :], kd[m * 64:(m + 1) * 64, kt * 128:(kt + 1) * 128], qd[m * 64:(m + 1) * 64, :],
                                                                                    start=True, stop=True),
                                         reads=[kkd, kqd], writes=[ksc], sig=(m == 1))
                                return sc, ksc
                            nxt = qk_d(0)
                            for kt in range(nkt):
                                sc, ksc = nxt
                                if kt + 1 < nkt:
                                    nxt = qk_d(kt + 1)
                                et, ke = er.next()
                                S.op("act", lambda A, et=et, sc=sc, kt=kt, qc=qc: A.activation(out=et[:], in_=sc[:], func=AF.Exp, scale=0.125, bias=mb[:, kt * NQC + qc:kt * NQC + qc + 1]), reads=[ksc, kmb], writes=[ke])
                                st, sp_ = (kt == 0), (kt == nkt - 1)
                                S.op("pe", lambda P, ov1=ov1, vd=vd, et=et, kt=kt, st=st, sp_=sp_: P.matmul(ov1[:], vd[:, kt, :], et[:, 0, :], start=st, stop=sp_),
                                     reads=[kvd, ke], writes=[ko1], sig=False)
                                S.op("pe", lambda P, ov2=ov2, vd=vd, et=et, kt=kt, st=st, sp_=sp_: P.matmul(ov2[:], vd[:, kt, :], et[:, 1, :], start=st, stop=sp_),
                                     reads=[kvd, ke], writes=[ko2], sig=False)
                                S.op("pe", lambda P, rb1=rb1, et=et, st=st, sp_=sp_: P.matmul(rb1[:], ONES, et[:, 0, :], start=st, stop=sp_),
                                     reads=[ke], writes=[kr1], sig=False)
                                S.op("pe", lambda P, rb2=rb2, et=et, st=st, sp_=sp_: P.matmul(rb2[:], ONES, et[:, 1, :], start=st, stop=sp_),
                                     reads=[ke], writes=[kr2], sig=True)
                            r1, k1 = fr.next()
                            S.op("dve", lambda V, r1=r1, rb1=rb1: V.reciprocal(r1[:], rb1[:]), reads=[kr1], writes=[k1])
                            r2, k2 = fr.next()
                            S.op("dve", lambda V, r2=r2, rb2=rb2: V.reciprocal(r2[:], rb2[:]), reads=[kr2], writes=[k2])
                            a1, ka1 = fr.next()
                            S.op("dve", lambda V, a1=a1, ov1=ov1, r1=r1: V.tensor_tensor(out=a1[:], in0=ov1[:], in1=r1[:], op=ALU.mult), reads=[ko1, k1], writes=[ka1])
                            a2, ka2 = fr.next()
                            S.op("dve", lambda V, a2=a2, ov2=ov2, r2=r2: V.tensor_tensor(out=a2[:], in0=ov2[:], in1=r2[:], op=ALU.mult), reads=[ko2, k2], writes=[ka2])
                            S.op("dve", lambda G, a1=a1, a2=a2: G.scalar_tensor_tensor(out=a1[:], in0=a2[:], scalar=nlam[:, l:l + 1], in1=a1[:], op0=ALU.mult, op1=ALU.add),
                                 reads=[ka2, ka1], writes=[ka1])
                            sq, ksq = sqr.next()
                            S.op("pool", lambda G, sq=sq, a1=a1: G.tensor_tensor(out=sq[:], in0=a1[:], in1=a1[:], op=ALU.mult), reads=[ka1], writes=[ksq])
                            ssq, kss = acc.next()
                            S.op("pe", lambda P, ssq=ssq, sq=sq: P.matmul(ssq[:], ONES, sq[:], start=True, stop=True), reads=[ksq], writes=[kss])
                            rn, krn = fr.next()
                            S.op("act", lambda A, rn=rn, ssq=ssq: A.activation(out=rn[:], in_=ssq[:], func=AF.Sqrt, scale=1.0, bias=128.0 * EPS), reads=[kss], writes=[krn])
                            S.op("dve", lambda V, rn=rn: V.reciprocal(rn[:], rn[:]), reads=[krn], writes=[krn])
                            S.op("dve", lambda V, a1=a1, rn=rn: V.scalar_tensor_tensor(out=a1[:], in0=a1[:], scalar=wsub[:, l:l + 1], in1=rn[:], op0=ALU.mult, op1=ALU.mult),
                                 reads=[ka1, krn], writes=[ka1])
                            S.op("pool", lambda G, a1=a1, ga=ga: G.tensor_tensor(out=a1[:], in0=a1[:], in1=ga[:], op=ALU.mult), reads=[ka1, kga], writes=[ka1])
                            ovg, kog = acc.next()
                            rbg, krg = acc.next()
                            def qk_g(kt, kg_t=kg_t, qg=qg, kkg=kkg, kqg=kqg):
                                sc, ksc = scr_.next()
                                for m in range(2):
                                    S.op("pe", lambda P, sc=sc, kt=kt, m=m: P.matmul(sc[:, m, :], kg_t[m * 64:(m + 1) * 64, kt * 128:(kt + 1) * 128], qg[m * 64:(m + 1) * 64, :],
                                                                                    start=True, stop=True),
                                         reads=[kkg, kqg], writes=[ksc], sig=(m == 1))
                                return sc, ksc
                            nxt = qk_g(0)
                            for kt in range(nkt):
                                sc, ksc = nxt
                                if kt + 1 < nkt:
                                    nxt = qk_g(kt + 1)
                                et, ke = er.next()
                                S.op("act", lambda A, et=et, sc=sc, kt=kt, qc=qc: A.activation(out=et[:], in_=sc[:], func=AF.Exp, scale=0.125, bias=mb[:, kt * NQC + qc:kt * NQC + qc + 1]), reads=[ksc, kmb], writes=[ke])
                                st, sp_ = (kt == 0), (kt == nkt - 1)
                                for m in range(2):
                                    S.op("pe", lambda P, ovg=ovg, vg_t=vg_t, et=et, kt=kt, m=m, st=st, sp_=sp_: P.matmul(ovg[m * 64:(m + 1) * 64, :], vg_t[:, kt, :], et[:, m, :], start=st, stop=sp_),
                                         reads=[kvg, ke], writes=[kog], sig=False)
                                for m in range(2):
                                    S.op("pe", lambda P, rbg=rbg, et=et, m=m, st=st, sp_=sp_: P.matmul(rbg[m * 64:(m + 1) * 64, :], cm[:, 4, 0:64], et[:, m, :], start=st, stop=sp_),
                                         reads=[ke], writes=[krg], sig=(m == 1))
                            rg, krg2 = fr.next()
                            S.op("dve", lambda V, rg=rg, rbg=rbg: V.reciprocal(rg[:], rbg[:]), reads=[krg], writes=[krg2])
                            bt, kbt = fr.next()
                            S.op("dve", lambda V, bt=bt, ovg=ovg, rg=rg: V.tensor_tensor(out=bt[:], in0=ovg[:], in1=rg[:], op=ALU.mult), reads=[kog, krg2], writes=[kbt])
                            S.op("pool", lambda G, bt=bt, gb=gb: G.tensor_tensor(out=bt[:], in0=bt[:], in1=gb[:], op=ALU.mult), reads=[kbt, kgb], writes=[kbt])
                            mo, kmo = mor.next()
                            S.op("pool", lambda G, mo=mo, bt=bt, a1=a1: G.tensor_tensor(out=mo[:], in0=bt[:], in1=a1[:], op=ALU.add), reads=[kbt, ka1], writes=[kmo])
                            S.dma("sp", mT[:, j, t0:t0 + 512], mo[:], reads=[kmo])
                S.emit()

        def outproj_phase(tag, l, src):
            with ExitStack() as es:
                S = Sched(nc, es, tag)
                wo, kwo = one(nc, es, "wo", [128, KC, D], BF16)
                S.dma("pool", wo[:], w_out[l].rearrange("(kc p) n -> p kc n", p=128), writes=[kwo])
                g1 = [one(nc, es, f"g1_{s}", [128, D], F32) for s in range(2)]
                for s in range(2):
                    S.dma("sp", g1[s][0][:], modg[l, 0, s], writes=[g1[s][1]])
                mr = Ring(nc, es, "om", [128, KC, 512], BF16, 2)
                xr = Ring(nc, es, "ox", [128, D], F32, 3)
                pr = Ring(nc, es, "ops", [128, 2, 512], F32, 2, psum=True)
                yr = Ring(nc, es, "oy", [128, D], F32, 3)
                for ci in range(NCH):
                    t0 = ci * 512
                    s = slot_of(t0)
                    mt, kmt = mr.next()
                    S.dma("sp", mt[:], mT[:, :, t0:t0 + 512], writes=[kmt])
                    for tt in range(4):
                        r0 = t0 + tt * 128
                        xt, kx = xr.next()
                        S.dma("sp", xt[:], src[r0:r0 + 128, :], writes=[kx])
                        ps, kp = pr.next()
                        for h in range(2):
                            for kc in range(KC):
                                S.op("pe", lambda P, ps=ps, mt=mt, tt=tt, kc=kc, h=h: P.matmul(ps[:, h, :], mt[:, kc, tt * 128:(tt + 1) * 128], wo[:, kc, h * 512:(h + 1) * 512],
                                                                                              start=(kc == 0), stop=(kc == KC - 1)),
                                     reads=[kmt, kwo], writes=[kp], sig=(h == 1 and kc == KC - 1))
                        yt, ky = yr.next()
                        S.op("dve", lambda V, yt=yt, ps=ps, s=s: V.tensor_tensor(out=yt[:].rearrange("p (a b) -> p a b", a=2), in0=ps[:], in1=g1[s][0][:].rearrange("p (a b) -> p a b", a=2), op=ALU.mult),
                             reads=[kp, g1[s][1]], writes=[ky])
                        S.op("pool", lambda G, yt=yt, xt=xt: G.tensor_tensor(out=yt[:], in0=yt[:], in1=xt[:], op=ALU.add), reads=[ky, kx], writes=[ky])
                        S.dma("sp", xres[r0:r0 + 128, :], yt[:], reads=[ky])
                S.emit()

        def ffn_pass(tag, l, wg_src, wu_src, wd_src, f0, nf, mode, e_idx, gates_t, first, last):
            with ExitStack() as es:
                S = Sched(nc, es, tag)
                wg, kwg = one(nc, es, "fwg", [128, KC, nf * 128], BF16)
                wu, kwu = one(nc, es, "fwu", [128, KC, nf * 128], BF16)
                wd, kwd = one(nc, es, "fwd", [128, nf, D], BF16)
                S.dma("pool", wg[:], wg_src[:, f0:f0 + nf * 128].rearrange("(kc p) n -> p kc n", p=128), writes=[kwg])
                S.dma("pool", wu[:], wu_src[:, f0:f0 + nf * 128].rearrange("(kc p) n -> p kc n", p=128), writes=[kwu])
                S.dma("pool", wd[:], wd_src[f0:f0 + nf * 128, :].rearrange("(f p) n -> p f n", p=128), writes=[kwd])
                g2 = [one(nc, es, f"g2_{s}", [128, D], F32) for s in range(2)]
                if mode == "dense" or last:
                    for s in range(2):
                        S.dma("sp", g2[s][0][:], modg[l, 1, s], writes=[g2[s][1]])
                hr = Ring(nc, es, "fh", [128, KC, 512], BF16, 2)
                pr = Ring(nc, es, "fps", [128, 2, 512], F32, 2, psum=True)
                po = Ring(nc, es, "fpo", [128, 2, 512], F32, 2, psum=True)
                sr = Ring(nc, es, "fsg", [128, 512], F32, 2)
                ar = Ring(nc, es, "fact", [128, nf, 512], BF16, 2)
                xr = Ring(nc, es, "fx", [128, D], F32, 2)
                yr = Ring(nc, es, "fy", [128, D], F32, 2)
                for ci in range(NCH):
                    t0 = ci * 512
                    s = slot_of(t0)
                    ht, kh = hr.next()
                    S.dma("sp", ht[:], hT[:, :, t0:t0 + 512], writes=[kh])
                    at, ka = ar.next()
                    for f in range(nf):
                        ps, kp = pr.next()
                        for wi, (ww, kw) in enumerate(((wg, kwg), (wu, kwu))):
                            for kc in range(KC):
                                S.op("pe", lambda P, ps=ps, ht=ht, ww=ww, wi=wi, f=f, kc=kc: P.matmul(ps[:, wi, :], ww[:, kc, f * 128:(f + 1) * 128], ht[:, kc, :], start=(kc == 0), stop=(kc == KC - 1)),
                                     reads=[kw, kh], writes=[kp], sig=(wi == 1 and kc == KC - 1))
                        sg, ksg = sr.next()
                        S.op("act", lambda A, sg=sg, ps=ps: A.activation(out=sg[:], in_=ps[:, 0, :], func=AF.Silu), reads=[kp], writes=[ksg])
                        S.op("dve", lambda V, at=at, f=f, sg=sg, ps=ps: V.tensor_tensor(out=at[:, f, :], in0=ps[:, 1, :], in1=sg[:], op=ALU.mult), reads=[kp, ksg], writes=[ka])
                    for tt in range(4):
                        r0 = t0 + tt * 128
                        ps, kp = po.next()
                        for h in range(2):
                            for f in range(nf):
                                S.op("pe", lambda P, ps=ps, at=at, tt=tt, f=f, h=h: P.matmul(ps[:, h, :], at[:, f, tt * 128:(tt + 1) * 128], wd[:, f, h * 512:(h + 1) * 512], start=(f == 0), stop=(f == nf - 1)),
                                     reads=[ka, kwd], writes=[kp], sig=(h == 1 and f == nf - 1))
                        psf = ps[:]
                        v3 = lambda t: t[:].rearrange("p (a b) -> p a b", a=2)
                        yt, ky = yr.next()
                        xt, kx = xr.next()
                        if mode == "dense":
                            S.dma("sp", xt[:], xres[r0:r0 + 128, :], writes=[kx])
                            S.op("dve", lambda V, yt=yt, psf=psf, s=s: V.tensor_tensor(out=v3(yt), in0=psf, in1=v3(g2[s][0]), op=ALU.mult), reads=[kp, g2[s][1]], writes=[ky])
                            S.op("pool", lambda G, yt=yt, xt=xt: G.tensor_tensor(out=yt[:], in0=yt[:], in1=xt[:], op=ALU.add), reads=[ky, kx], writes=[ky])
                            S.dma("sp", xres[r0:r0 + 128, :], yt[:], reads=[ky])
                        else:
                            gcol = gates_t[0][:, r0 // 128, e_idx:e_idx + 1]
                            if first:
                                S.op("dve", lambda V, yt=yt, psf=psf, gcol=gcol: V.tensor_scalar(out=v3(yt), in0=psf, scalar1=gcol, scalar2=None, op0=ALU.mult),
                                     reads=[kp, gates_t[1]], writes=[ky])
                            else:
                                S.dma("sp", xt[:], yacc[r0:r0 + 128, :], writes=[kx])
                                S.op("dve", lambda V, yt=yt, psf=psf, gcol=gcol, xt=xt: V.scalar_tensor_tensor(out=v3(yt), in0=psf, scalar=gcol, in1=v3(xt), op0=ALU.mult, op1=ALU.add),
                                     reads=[kp, gates_t[1], kx], writes=[ky])
                            if last:
                                x2, kx2 = xr.next()
                                S.dma("sp", x2[:], xres[r0:r0 + 128, :], writes=[kx2])
                                S.op("pool", lambda G, yt=yt, s=s: G.tensor_tensor(out=yt[:], in0=yt[:], in1=g2[s][0][:], op=ALU.mult), reads=[ky, g2[s][1]], writes=[ky])
                                S.op("pool", lambda G, yt=yt, x2=x2: G.tensor_tensor(out=yt[:], in0=yt[:], in1=x2[:], op=ALU.add), reads=[ky, kx2], writes=[ky])
                                S.dma("sp", xres[r0:r0 + 128, :], yt[:], reads=[ky])
                            else:
                                S.dma("sp", yacc[r0:r0 + 128, :], yt[:], reads=[ky])
                S.emit()

        def router_phase(tag, l, gates_t):
            li = l // 2
            with ExitStack() as es:
                S = Sched(nc, es, tag)
                rw, krw = one(nc, es, "rw", [128, KC, NE], F32)
                with nc.allow_non_contiguous_dma("router weights are tiny"):
                    S.dma("sp", rw[:], router_w[li].rearrange("(kc p) e -> p kc e", p=128), writes=[krw])
                idf, kidf = one(nc, es, "idf", [128, 128], F32)
                S.dma("sp", idf[:], cmats[:, 0, :], writes=[kidf])
                xr = Ring(nc, es, "rx", [128, D], F32, 3)
                jr = Ring(nc, es, "rjunk", [128, D], BF16, 1)
                sr = Ring(nc, es, "rss", [128, 4], F32, 4)
                pT = Ring(nc, es, "rT", [128, KC, 128], F32, 2, psum=True)
                hr = Ring(nc, es, "rh", [128, KC, 128], F32, 2)
                pl = Ring(nc, es, "rpl", [128, 512], F32, 2, psum=True)
                lr = Ring(nc, es, "rl", [128, 4, NE], F32, 3)
                mr = Ring(nc, es, "rm", [128, 8], F32, 3)
                gt, kg = gates_t
                for ti in range(T // 128):
                    r0 = ti * 128
                    s = slot_of(r0)
                    xt, kx = xr.next()
                    S.dma("sp", xt[:], xres[r0:r0 + 128, :], writes=[kx])
                    jt, kj = jr.next()
                    st, ks = sr.next()
                    S.op("dve", lambda V, st=st: V.memset(st[:, 0:1], 0.0), writes=[ks])
                    S.op("act", lambda A, jt=jt, xt=xt, st=st: A.activation(out=jt[:], in_=xt[:], func=AF.Square, accum_out=st[:, 0:1]), reads=[kx], writes=[kj, ks])
                    S.op("act", lambda A, st=st: A.activation(out=st[:, 1:2], in_=st[:, 0:1], func=AF.Sqrt, scale=1.0 / D, bias=EPS), reads=[ks], writes=[ks])
                    S.op("dve", lambda V, st=st: V.reciprocal(st[:, 2:3], st[:, 1:2]), reads=[ks], writes=[ks])
                    S.op("pool", lambda G, xt=xt, st=st: G.tensor_scalar(out=xt[:], in0=xt[:], scalar1=st[:, 2:3], scalar2=None, op0=ALU.mult), reads=[kx, ks], writes=[kx])
                    pt, kpt = pT.next()
                    for c in range(KC):
                        S.op("pe", lambda P, pt=pt, xt=xt, c=c: P.transpose(pt[:, c, :], xt[:, c * 128:(c + 1) * 128], idf[:]), reads=[kx, kidf], writes=[kpt], sig=(c == KC - 1))
                    ht, kh = hr.next()
                    for c in range(KC):
                        S.op("dve", lambda V, ht=ht, pt=pt, c=c, s=s: V.tensor_scalar(out=ht[:, c, :], in0=pt[:, c, :], scalar1=modf[:, l, 3, c, s:s + 1], scalar2=modf[:, l, 2, c, s:s + 1],
                                                                                     op0=ALU.mult, op1=ALU.add), reads=[kpt], writes=[kh])
                    lg, klg = pl.next()
                    for kc in range(KC):
                        S.op("pe", lambda P, lg=lg, ht=ht, kc=kc: P.matmul(lg[:, 0:NE], ht[:, kc, :], rw[:, kc, :], start=(kc == 0), stop=(kc == KC - 1)),
                             reads=[kh, krw], writes=[klg], sig=(kc == KC - 1))
                    lt, klt = lr.next()
                    mt, kmt = mr.next()
                    S.op("dve", lambda V, lt=lt, lg=lg: V.tensor_copy(lt[:, 0, :], lg[:, 0:NE]), reads=[klg], writes=[klt])
                    S.op("dve", lambda V, lt=lt, mt=mt: V.tensor_reduce(out=mt[:, 0:1], in_=lt[:, 0, :], axis=AX.X, op=ALU.max), reads=[klt], writes=[kmt])
                    S.op("dve", lambda V, lt=lt, mt=mt: V.tensor_scalar(out=lt[:, 1, :], in0=lt[:, 0, :], scalar1=mt[:, 0:1], scalar2=None, op0=ALU.is_ge), reads=[klt, kmt], writes=[klt])
                    S.op("dve", lambda V, lt=lt: V.scalar_tensor_tensor(out=lt[:, 2, :], in0=lt[:, 1, :], scalar=-1e30, in1=lt[:, 0, :], op0=ALU.mult, op1=ALU.add), reads=[klt], writes=[klt])
                    S.op("dve", lambda V, lt=lt, mt=mt: V.tensor_reduce(out=mt[:, 1:2], in_=lt[:, 2, :], axis=AX.X, op=ALU.max), reads=[klt], writes=[kmt])
                    S.op("dve", lambda V, lt=lt, mt=mt: V.tensor_scalar(out=lt[:, 3, :], in0=lt[:, 0, :], scalar1=mt[:, 1:2], scalar2=None, op0=ALU.is_ge), reads=[klt, kmt], writes=[klt])
                    S.op("dve", lambda V, mt=mt: V.tensor_scalar(out=mt[:, 2:3], in0=mt[:, 0:1], scalar1=-1.0, scalar2=None, op0=ALU.mult), reads=[kmt], writes=[kmt])
                    S.op("act", lambda A, lt=lt, mt=mt: A.activation(out=lt[:, 1, :], in_=lt[:, 0, :], func=AF.Exp, bias=mt[:, 2:3], scale=1.0), reads=[klt, kmt], writes=[klt])
                    S.op("dve", lambda V, lt=lt: V.tensor_tensor(out=lt[:, 2, :], in0=lt[:, 1, :], in1=lt[:, 3, :], op=ALU.mult), reads=[klt], writes=[klt])
                    S.op("dve", lambda V, lt=lt, mt=mt: V.tensor_reduce(out=mt[:, 3:4], in_=lt[:, 2, :], axis=AX.X, op=ALU.add), reads=[klt], writes=[kmt])
                    S.op("dve", lambda V, mt=mt: V.reciprocal(mt[:, 4:5], mt[:, 3:4]), reads=[kmt], writes=[kmt])
                    S.op("dve", lambda V, lt=lt, mt=mt, ti=ti: V.tensor_scalar(out=gt[:, ti, :], in0=lt[:, 2, :], scalar1=mt[:, 4:5], scalar2=None, op0=ALU.mult), reads=[klt, kmt], writes=[kg])
                S.emit()

        def final_phase(tag, src):
            with ExitStack() as es:
                S = Sched(nc, es, tag)
                fb, kfb = one(nc, es, "fnb", [128, D], F32)
                S.dma("sp", fb[:], final_norm.partition_broadcast(128), writes=[kfb])
                xr = Ring(nc, es, "zx", [128, D], F32, 3)
                jr = Ring(nc, es, "zjunk", [128, D], BF16, 1)
                sr = Ring(nc, es, "zss", [128, 4], F32, 4)
                for ti in range(T // 128):
                    r0 = ti * 128
                    xt, kx = xr.next()
                    S.dma("sp", xt[:], src[r0:r0 + 128, :], writes=[kx])
                    jt, kj = jr.next()
                    st, ks = sr.next()
                    S.op("dve", lambda V, st=st: V.memset(st[:, 0:1], 0.0), writes=[ks])
                    S.op("act", lambda A, jt=jt, xt=xt, st=st: A.activation(out=jt[:], in_=xt[:], func=AF.Square, accum_out=st[:, 0:1]), reads=[kx], writes=[kj, ks])
                    S.op("act", lambda A, st=st: A.activation(out=st[:, 1:2], in_=st[:, 0:1], func=AF.Sqrt, scale=1.0 / D, bias=EPS), reads=[ks], writes=[ks])
                    S.op("dve", lambda V, st=st: V.reciprocal(st[:, 2:3], st[:, 1:2]), reads=[ks], writes=[ks])
                    S.op("dve", lambda V, xt=xt, st=st: V.scalar_tensor_tensor(out=xt[:], in0=xt[:], scalar=st[:, 2:3], in1=fb[:], op0=ALU.mult, op1=ALU.mult), reads=[kx, ks, kfb], writes=[kx])
                    S.dma("sp", y_out[r0:r0 + 128, :], xt[:], reads=[kx])
                S.emit()

        gates_tile = gs.enter_context(nc.sbuf_tensor("gatesw", [128, T // 128, NE], F32))
        gates_t = (gates_tile, Trk())
        for l in range(L):
            src = x_in if l == 0 else xres
            norm_phase(f"n{l}a", src, l, 0, 1)
            proj_phase(f"p{l}", l)
            attn_phase(f"a{l}", l)
            outproj_phase(f"o{l}", l, src)
            norm_phase(f"n{l}b", xres, l, 2, 3)
            if l % 2 == 0:
                li = l // 2
                nf_all = DFF // 128
                halves = [(0, nf_all // 2), (nf_all // 2, nf_all - nf_all // 2)]
                for hi, (fs, nf) in enumerate(halves):
                    ffn_pass(f"f{l}_{hi}", l, ffn_w_gate[li], ffn_w_up[li], ffn_w_down[li], fs * 128, nf, "dense", 0, None, False, False)
            else:
                li = l // 2
                gates_t = (gates_tile, Trk())
                router_phase(f"r{l}", l, gates_t)
                nf_all = DFE // 128
                npart = 4 if nf_all % 4 == 0 else 2
                halves = [(i * (nf_all // npart), nf_all // npart) for i in range(npart)]
                for e in range(NE):
                    for hi, (fs, nf) in enumerate(halves):
                        first = (e == 0 and hi == 0)
                        last = (e == NE - 1 and hi == len(halves) - 1)
                        ffn_pass(f"m{l}_{e}_{hi}", l, moe_w_gate[li, e], moe_w_up[li, e], moe_w_down[li, e], fs * 128, nf, "moe", e,
                                 (gates_tile, Trk()), first, last)
        final_phase("fin", xres)
    return nc


def _consts():
    ident = np.eye(128, dtype=np.float32)
    pd = np.zeros((128, 128), np.float32)
    pg = np.zeros((128, 128), np.float32)
    for b in range(2):
        o = b * 64
        for i in range(8):
            pd[o + i + 8, o + i] = 1.0
            pd[o + i, o + i + 8] = 1.0
        for base in (0, 32):
            for i in range(16):
                pg[o + base + i + 16, o + base + i] = 1.0
                pg[o + base + i, o + base + i + 16] = 1.0
    bones = np.zeros((128, 128), np.float32)
    bones[0:64, 0:64] = 1.0
    bones[64:128, 64:128] = 1.0
    ones = np.ones((128, 128), np.float32)
    return np.ascontiguousarray(np.stack([ident, pd, pg, bones, ones], axis=1))


def _rope_tables(pos):
    pos = np.asarray(pos)
    n = pos.shape[0]
    tab = np.zeros((4, 128, n), np.float32)
    tab[0] = 1.0
    posf = pos.astype(np.float32)
    rowf = (pos // GRID_W).astype(np.float32)
    colf = (pos % GRID_W).astype(np.float32)
    invd = np.exp(-math.log(DIFF_THETA) * np.arange(8, dtype=np.float32) / 8).astype(np.float32)
    invg = np.exp(-math.log(AX_THETA) * np.arange(16, dtype=np.float32) / 16).astype(np.float32)
    for p in range(128):
        d = p % 64
        if d < 16:
            ang = posf * invd[d % 8]
            tab[0, p] = np.cos(ang)
            tab[1, p] = -np.sin(ang) if d < 8 else np.sin(ang)
        if d < 32:
            ang = rowf * invg[d % 16]
            sgn = -1.0 if d < 16 else 1.0
        else:
            ang = colf * invg[(d - 32) % 16]
            sgn = -1.0 if d < 48 else 1.0
        tab[2, p] = np.cos(ang)
        tab[3, p] = sgn * np.sin(ang)
    return tab


_WNAMES = ["w_ada", "b_ada", "norm1", "w_in", "diff_lambda", "diff_subln", "gqa_q_norm", "gqa_k_norm", "w_out", "norm2",
           "ffn_w_gate", "ffn_w_up", "ffn_w_down", "router_w", "moe_w_gate", "moe_w_up", "moe_w_down", "final_norm"]


def kernel(**inputs):
    xp = np.asarray(inputs["x_prompt"], np.float32)
    xs = np.asarray(inputs["x_sample"], np.float32)
    cp = np.asarray(inputs["c_prompt"], np.float32)
    cs = np.asarray(inputs["c_sample"], np.float32)
    B1, S1, _ = xp.shape
    B2, S2, _ = xs.shape
    assert S2 == 2 * S1 and B1 % 2 == 0
    L = inputs["w_in"].shape[0]
    DFF = inputs["ffn_w_gate"].shape[2]
    DFE = inputs["moe_w_gate"].shape[3]
    n = 8
    T = S2
    npair = B1 // 2
    assert B2 + npair <= n
    cfg = Cfg(L, T, DFF, DFE)
    nc = build(cfg)
    cm = _consts()
    tab_s = np.ascontiguousarray(_rope_tables(np.arange(T)))
    tab_p = np.ascontiguousarray(_rope_tables(np.concatenate([np.arange(S1), np.arange(S1)])))
    nkt, nqc = T // 128, T // 512
    mb_s = np.zeros((128, nkt * nqc), np.float32)
    mb_p = np.zeros((nkt, nqc), np.float32)
    for kt in range(nkt):
        for qc in range(nqc):
            if (kt * 128) // S1 != (qc * 512) // S1:
                mb_p[kt, qc] = -30000.0
    mb_p = np.ascontiguousarray(np.broadcast_to(mb_p.reshape(1, -1), (128, nkt * nqc)))
    wts = {k: np.ascontiguousarray(np.asarray(inputs[k], np.float32)) for k in _WNAMES}
    in_maps = []
    for c in range(n):
        if c < B2:
            m = {"x": np.ascontiguousarray(xs[c]), "cvec": np.ascontiguousarray(np.stack([cs[c], cs[c]], axis=0)),
                 "tabs": tab_s, "maskb": mb_s}
        else:
            i = min(c - B2, npair - 1)
            m = {"x": np.ascontiguousarray(np.concatenate([xp[2 * i], xp[2 * i + 1]], axis=0)),
                 "cvec": np.ascontiguousarray(np.stack([cp[2 * i], cp[2 * i + 1]], axis=0)),
                 "tabs": tab_p, "maskb": mb_p}
        m["cmats"] = cm
        m.update(wts)
        in_maps.append(m)
    res = run_bass_kernel_spmd(nc, in_maps, core_ids=list(range(n)))
    outs = [np.asarray(r["y"], np.float32) for r in res.results]
    y_sample = np.stack([outs[b] for b in range(B2)], axis=0)
    y_prompt = np.stack([outs[B2 + j // 2][(j % 2) * S1:(j % 2 + 1) * S1] for j in range(B1)], axis=0)
    return (y_prompt, y_sample)
```

```python
import math
from contextlib import ExitStack

import numpy as np
import concourse.bass as bass
import concourse.mybir as mybir
from concourse.bass_utils import run_bass_kernel_spmd

F32 = mybir.dt.float32
BF16 = mybir.dt.bfloat16
AF = mybir.ActivationFunctionType
ALU = mybir.AluOpType
AX = mybir.AxisListType

D = 1024
KC = 8
EPS = 1e-6
DIFF_THETA = 500000.0
AX_THETA = 10000.0
GRID_W = 64
NE = 8


class Cfg:
    def __init__(self, L, T, DFF, DFE):
        self.L, self.T, self.DFF, self.DFE = L, T, DFF, DFE
        self.HALF = T // 2


class Trk:
    __slots__ = ("w", "r")

    def __init__(self):
        self.w = []
        self.r = {}


class Sched:
    ENG = ("pe", "act", "dve", "pool", "sp")
    ND = 6

    _shared = None

    def __init__(self, nc, es, tag):
        self.nc = nc
        self.lists = {e: [] for e in self.ENG}
        self.pend = {e: [] for e in self.ENG}
        sh = Sched._shared
        if sh is None or sh["nc"] is not nc:
            gs = Sched._gs
            sh = {"nc": nc,
                  "sem": {e: gs.enter_context(nc.semaphore(f"s_{e}")) for e in self.ENG},
                  "cnt": {e: 0 for e in self.ENG},
                  "waited": {e: {} for e in self.ENG},
                  "dsem": {q: [gs.enter_context(nc.semaphore(f"d_{q}{i}")) for i in range(self.ND)] for q in ("sp", "pool")},
                  "dcnt": {q: [0] * self.ND for q in ("sp", "pool")},
                  "dlast": {q: [None] * self.ND for q in ("sp", "pool")},
                  "dnext": {q: 0 for q in ("sp", "pool")}}
            Sched._shared = sh
        self.sem, self.cnt, self.waited = sh["sem"], sh["cnt"], sh["waited"]
        self.dsem, self.dcnt, self.dlast, self.dnext = sh["dsem"], sh["dcnt"], sh["dlast"], sh["dnext"]

    def _wait(self, e, toks):
        for t in toks:
            if t is None:
                continue
            sem, val = t
            k = id(sem)
            if self.waited[e].get(k, 0) < val:
                self.waited[e][k] = val
                self.lists[e].append(lambda E, sem=sem, val=val: E.wait_ge(sem, val))

    @staticmethod
    def _deps(reads, writes):
        deps = []
        for b in reads:
            deps += b.w
        for b in writes:
            deps += b.w
            deps += list(b.r.values())
        return deps

    @staticmethod
    def _commit(tok, reads, writes):
        k = id(tok[0])
        for b in reads:
            b.r[k] = tok
        for b in writes:
            b.w = [tok]
            b.r = {}

    def op(self, e, fn, reads=(), writes=(), sig=True):
        self._wait(e, self._deps(reads, writes))
        self.pend[e].append((reads, writes))
        if not sig:
            self.lists[e].append(fn)
            return None
        self.cnt[e] += 1
        sem = self.sem[e]
        tok = (sem, self.cnt[e])
        self.lists[e].append(lambda E, fn=fn, sem=sem: fn(E).then_inc(sem, 1))
        for rs, ws in self.pend[e]:
            self._commit(tok, rs, ws)
        self.pend[e] = []
        return tok

    def dma(self, q, out, in_, reads=(), writes=()):
        i = self.dnext[q]
        self.dnext[q] = (i + 1) % self.ND
        self._wait(q, self._deps(reads, writes) + [self.dlast[q][i]])
        self.dcnt[q][i] += 16
        sem = self.dsem[q][i]
        tok = (sem, self.dcnt[q][i])
        self.dlast[q][i] = tok
        self.lists[q].append(lambda E, out=out, in_=in_, sem=sem: E.dma_start(out=out, in_=in_).then_inc(sem, 16))
        self._commit(tok, reads, writes)
        return tok

    def emit(self):
        allt = [t for q in ("sp", "pool") for t in self.dlast[q]]
        allt += [(self.sem[e], self.cnt[e]) for e in self.ENG if self.cnt[e] > 0 and e != "sp"]
        for e in self.ENG:
            self._wait(e, allt)
        with self.nc.Block() as blk:
            def run(key):
                def f(E):
                    for g in self.lists[key]:
                        g(E)
                return f
            blk.tensor(run("pe"))
            blk.scalar(run("act"))
            blk.vector(run("dve"))
            blk.gpsimd(run("pool"))
            blk.sync(run("sp"))


_UID = [0]


class Ring:
    def __init__(self, nc, es, name, shape, dtype, n, psum=False):
        mk = nc.psum_tensor if psum else nc.sbuf_tensor
        _UID[0] += 1
        self.t = [es.enter_context(mk(f"{name}_{_UID[0]}_{i}", shape, dtype)) for i in range(n)]
        self.k = [Trk() for _ in range(n)]
        self.i = 0

    def next(self):
        i = self.i
        self.i = (i + 1) % len(self.t)
        return self.t[i], self.k[i]


def one(nc, es, name, shape, dtype, psum=False):
    r = Ring(nc, es, name, shape, dtype, 1, psum)
    return r.t[0], r.k[0]


def build(cfg):
    nc = bass.Bass("TRN2", target_bir_lowering=False)
    L, T, DFF, DFE, HALF = cfg.L, cfg.T, cfg.DFF, cfg.DFE, cfg.HALF
    TK = T
    NCH = T // 512

    def din(name, shape):
        return nc.dram_tensor(name, list(shape), F32, kind="ExternalInput").ap()

    x_in = din("x", [T, D])
    cvec = din("cvec", [2, D])
    cmats = din("cmats", [128, 5, 128])
    tabs = din("tabs", [4, 128, T])
    maskb = din("maskb", [128, (T // 128) * (T // 512)])
    w_ada = din("w_ada", [L, D, 6 * D])
    b_ada = din("b_ada", [L, 6 * D])
    norm1 = din("norm1", [L, D])
    w_in = din("w_in", [L, D, 6656])
    diff_lambda = din("diff_lambda", [L, 4, 64])
    diff_subln = din("diff_subln", [L, 128])
    gqa_q_norm = din("gqa_q_norm", [L, 64])
    gqa_k_norm = din("gqa_k_norm", [L, 64])
    w_out = din("w_out", [L, D, D])
    norm2 = din("norm2", [L, D])
    ND_, NM_ = (L + 1) // 2, L // 2
    ffn_w_gate = din("ffn_w_gate", [ND_, D, DFF])
    ffn_w_up = din("ffn_w_up", [ND_, D, DFF])
    ffn_w_down = din("ffn_w_down", [ND_, DFF, D])
    router_w = din("router_w", [max(NM_, 1), D, NE])
    moe_w_gate = din("moe_w_gate", [max(NM_, 1), NE, D, DFE])
    moe_w_up = din("moe_w_up", [max(NM_, 1), NE, D, DFE])
    moe_w_down = din("moe_w_down", [max(NM_, 1), NE, DFE, D])
    final_norm = din("final_norm", [D])
    y_out = nc.dram_tensor("y", [T, D], F32, kind="ExternalOutput").ap()

    def scr(name, shape, dt):
        return nc.dram_tensor(name, list(shape), dt).ap()

    xres = scr("xres", [T, D], F32)
    hT = scr("hT", [128, KC, T], BF16)
    QT = scr("QT", [16, 128, T], BF16)
    KT = scr("KT", [12, 128, TK], BF16)
    Vs = scr("Vs", [TK, 1280], BF16)
    GT = scr("GT", [16, 128, T], BF16)
    mT = scr("mT", [128, KC, T], BF16)
    modg = scr("modg", [L, 2, 2, 128, D], F32)
    yacc = scr("yacc", [T, D], F32)

    def slot_of(tok0):
        return tok0 // HALF

    def key_off(tok0):
        return tok0

    with ExitStack() as gs:
        Sched._gs = gs
        Sched._shared = None
        cm = gs.enter_context(nc.sbuf_tensor("cm", [128, 5, 128], BF16))
        IDENT, PD, PG, BONES, ONES = (cm[:, i, :] for i in range(5))
        modf = gs.enter_context(nc.sbuf_tensor("modf", [128, L, 4, KC, 2], F32))
        fnorm = gs.enter_context(nc.sbuf_tensor("fnorm", [128, KC], F32))
        wsub = gs.enter_context(nc.sbuf_tensor("wsub", [128, L], F32))
        nlam = gs.enter_context(nc.sbuf_tensor("nlam", [128, L], F32))
        gq8 = gs.enter_context(nc.sbuf_tensor("gq8", [128, L], F32))
        gk8 = gs.enter_context(nc.sbuf_tensor("gk8", [128, L], F32))

        with ExitStack() as es, nc.allow_non_contiguous_dma("tiny feature-major parameter loads"):
            S = Sched(nc, es, "p0")
            E = es.enter_context
            kcm, kmodf, kmisc = Trk(), Trk(), Trk()
            S.dma("pool", cm[:], cmats, writes=[kcm])
            cT32, kcT32 = one(nc, es, "cT32", [128, 2, KC], F32)
            cTb, kcTb = one(nc, es, "cTb", [128, 2, KC], BF16)
            crep, kcrep = one(nc, es, "crep", [128, KC, 2, 128], BF16)
            n1, kn1 = one(nc, es, "n1", [128, L, KC], F32)
            n2, kn2 = one(nc, es, "n2", [128, L, KC], F32)
            bfm, kbfm = one(nc, es, "bfm", [128, L, 48], F32)
            sub, ksub = one(nc, es, "sub", [128, L], F32)
            gqn, kgqn = one(nc, es, "gqn", [128, L], F32)
            gkn, kgkn = one(nc, es, "gkn", [128, L], F32)
            dl, kdl = one(nc, es, "dl", [128, L, 4, 64], F32)
            for s_ in range(2):
                S.dma("sp", cT32[:, s_, :], cvec[s_].rearrange("(c p) -> p c", p=128), writes=[kcT32])
            S.dma("sp", n1[:], norm1.rearrange("l (c p) -> p l c", p=128), writes=[kn1])
            S.dma("sp", n2[:], norm2.rearrange("l (c p) -> p l c", p=128), writes=[kn2])
            S.dma("sp", fnorm[:], final_norm.rearrange("(c p) -> p c", p=128), writes=[kmisc])
            S.dma("sp", bfm[:], b_ada.rearrange("l (c p) -> p l c", p=128), writes=[kbfm])
            S.dma("sp", sub[:], diff_subln.rearrange("l p -> p l"), writes=[ksub])
            for hh in range(2):
                S.dma("sp", gqn[hh * 64:(hh + 1) * 64, :], gqa_q_norm.rearrange("l p -> p l"), writes=[kgqn])
                S.dma("sp", gkn[hh * 64:(hh + 1) * 64, :], gqa_k_norm.rearrange("l p -> p l"), writes=[kgkn])
            S.dma("sp", dl[:].rearrange("p l a d -> p (l a d)"),
                  diff_lambda.rearrange("l a d -> (l a d)").partition_broadcast(128), writes=[kdl])
            S.op("act", lambda A: A.activation(out=cTb[:], in_=cT32[:], func=AF.Silu), reads=[kcT32], writes=[kcTb])
            for s in range(2):
                S.op("dve", lambda V, s=s: V.tensor_copy(crep[:, :, s, :], cTb[:, s, :].unsqueeze(2).to_broadcast([128, KC, 128])),
                     reads=[kcTb], writes=[kcrep])
            tmp, ktmp = one(nc, es, "p0tmp", [128, 64], F32)
            sc4, ksc4 = one(nc, es, "sc4", [128, 4], F32)
            for l in range(L):
                lam_init = 0.8 - 0.6 * math.exp(-0.3 * l)
                for m in range(2):
                    S.op("dve", lambda V, l=l, m=m: V.tensor_tensor(out=tmp[:], in0=dl[:, l, 2 * m, :], in1=dl[:, l, 2 * m + 1, :], op=ALU.mult),
                         reads=[kdl], writes=[ktmp])
                    S.op("dve", lambda V, m=m: V.tensor_reduce(out=sc4[:, m:m + 1], in_=tmp[:], axis=AX.X, op=ALU.add),
                         reads=[ktmp], writes=[ksc4])
                S.op("act", lambda A: A.activation(out=sc4[:, 2:4], in_=sc4[:, 0:2], func=AF.Exp), reads=[ksc4], writes=[ksc4])
                S.op("dve", lambda V, l=l, li=lam_init: V.scalar_tensor_tensor(out=nlam[:, l:l + 1], in0=sc4[:, 3:4], scalar=-li, in1=sc4[:, 2:3],
                                                                            op0=ALU.add, op1=ALU.subtract),
                     reads=[ksc4], writes=[kmisc])
                S.op("dve", lambda V, l=l, li=lam_init: V.tensor_scalar(out=wsub[:, l:l + 1], in0=sub[:, l:l + 1], scalar1=(1.0 - li) * math.sqrt(128.0),
                                                                         scalar2=None, op0=ALU.mult),
                     reads=[ksub], writes=[kmisc])
                S.op("dve", lambda V, l=l: V.tensor_scalar(out=gq8[:, l:l + 1], in0=gqn[:, l:l + 1], scalar1=8.0, scalar2=None, op0=ALU.mult),
                     reads=[kgqn], writes=[kmisc])
                S.op("dve", lambda V, l=l: V.tensor_scalar(out=gk8[:, l:l + 1], in0=gkn[:, l:l + 1], scalar1=8.0, scalar2=None, op0=ALU.mult),
                     reads=[kgkn], writes=[kmisc])
            wr = Ring(nc, es, "wada", [128, KC, 512], BF16, 2)
            br = Ring(nc, es, "bbc", [128, 512], F32, 2)
            gr = Ring(nc, es, "gout", [128, 512], F32, 2)
            pr = Ring(nc, es, "p0ps", [128, 512], F32, 2, psum=True)
            V4 = {0: 0, 1: 1, 3: 2, 4: 3}
            for l in range(L):
                for j in range(12):
                    vec, half = j // 2, j % 2
                    wt, kw = wr.next()
                    S.dma("pool", wt[:], w_ada[l, :, j * 512:(j + 1) * 512].rearrange("(kc p) n -> p kc n", p=128), writes=[kw])
                    if vec in (2, 5):
                        which = 0 if vec == 2 else 1
                        bb, kb = br.next()
                        S.dma("sp", bb[:], b_ada[l, j * 512:(j + 1) * 512].partition_broadcast(128), writes=[kb])
                        for s in range(2):
                            ps, kp = pr.next()
                            for kc in range(KC):
                                S.op("pe", lambda P, ps=ps, wt=wt, s=s, kc=kc: P.matmul(ps[:], crep[:, kc, s, :], wt[:, kc, :], start=(kc == 0), stop=(kc == KC - 1)),
                                     reads=[kcrep, kw], writes=[kp], sig=(kc == KC - 1))
                            go, kg = gr.next()
                            S.op("dve", lambda V, go=go, ps=ps, bb=bb: V.tensor_tensor(out=go[:], in0=ps[:], in1=bb[:], op=ALU.add),
                                 reads=[kp, kb], writes=[kg])
                            S.dma("sp", modg[l, which, s, :, half * 512:(half + 1) * 512], go[:], reads=[kg])
                    else:
                        v4 = V4[vec]
                        for cb in range(4):
                            c = half * 4 + cb
                            ps, kp = pr.next()
                            for kc in range(KC):
                                S.op("pe", lambda P, ps=ps, wt=wt, cb=cb, kc=kc: P.matmul(ps[:, 0:2], wt[:, kc, cb * 128:(cb + 1) * 128], cTb[:, :, kc], start=(kc == 0), stop=(kc == KC - 1)),
                                     reads=[kcTb, kw], writes=[kp], sig=(kc == KC - 1))
                            S.op("dve", lambda V, ps=ps, l=l, v4=v4, c=c, vec=vec: V.tensor_scalar(out=modf[:, l, v4, c, :], in0=ps[:, 0:2], scalar1=bfm[:, l, vec * 8 + c:vec * 8 + c + 1],
                                                                                                   scalar2=None, op0=ALU.add),
                                 reads=[kp, kbfm], writes=[kmodf])
                for v4, nn, kn in ((1, n1, kn1), (3, n2, kn2)):
                    for s in range(2):
                        S.op("dve", lambda V, l=l, v4=v4, s=s, nn=nn: V.scalar_tensor_tensor(out=modf[:, l, v4, :, s], in0=modf[:, l, v4, :, s], scalar=1.0, in1=nn[:, l, :],
                                                                                           op0=ALU.add, op1=ALU.mult),
                             reads=[kmodf, kn], writes=[kmodf])
            S.emit()

        def norm_phase(tag, src, l, v_sh, v_s):
            with ExitStack() as es:
                S = Sched(nc, es, tag)
                xr = Ring(nc, es, "nx", [128, D], F32, 3)
                jr = Ring(nc, es, "njunk", [128, D], BF16, 1)
                sr = Ring(nc, es, "nss", [128, 4], F32, 4)
                xnr = Ring(nc, es, "nxn", [128, D], BF16, 3)
                tr = Ring(nc, es, "nT", [128, KC, 512], BF16, 1, psum=True)
                hr = Ring(nc, es, "nh", [128, KC, 512], BF16, 2)
                for ci in range(NCH):
                    s = slot_of(ci * 512)
                    pT, kT = tr.next()
                    for tt in range(4):
                        r0 = ci * 512 + tt * 128
                        xt, kx = xr.next()
                        S.dma("sp", xt[:], src[r0:r0 + 128, :], writes=[kx])
                        jt, kj = jr.next()
                        st, ks = sr.next()
                        S.op("dve", lambda V, st=st: V.memset(st[:, 0:1], 0.0), writes=[ks])
                        S.op("act", lambda A, jt=jt, xt=xt, st=st: A.activation(out=jt[:], in_=xt[:], func=AF.Square, accum_out=st[:, 0:1]),
                             reads=[kx], writes=[kj, ks])
                        S.op("act", lambda A, st=st: A.activation(out=st[:, 1:2], in_=st[:, 0:1], func=AF.Sqrt, scale=1.0 / D, bias=EPS),
                             reads=[ks], writes=[ks])
                        S.op("dve", lambda V, st=st: V.reciprocal(st[:, 2:3], st[:, 1:2]), reads=[ks], writes=[ks])
                        xn, kxn = xnr.next()
                        S.op("pool", lambda G, xn=xn, xt=xt, st=st: G.tensor_scalar(out=xn[:], in0=xt[:], scalar1=st[:, 2:3], scalar2=None, op0=ALU.mult),
                             reads=[kx, ks], writes=[kxn])
                        for c in range(KC):
                            S.op("pe", lambda P, pT=pT, xn=xn, c=c, tt=tt: P.transpose(pT[:, c, tt * 128:(tt + 1) * 128], xn[:, c * 128:(c + 1) * 128], IDENT),
                                 reads=[kxn], writes=[kT], sig=(c == KC - 1))
                    ht, kh = hr.next()
                    for c in range(KC):
                        if c % 2 == 0:
                            S.op("dve", lambda V, ht=ht, pT=pT, c=c, s=s: V.tensor_scalar(out=ht[:, c, :], in0=pT[:, c, :], scalar1=modf[:, l, v_s, c, s:s + 1],
                                                                                         scalar2=modf[:, l, v_sh, c, s:s + 1], op0=ALU.mult, op1=ALU.add),
                                 reads=[kT], writes=[kh])
                        else:
                            S.op("act", lambda A, ht=ht, pT=pT, c=c, s=s: A.activation(out=ht[:, c, :], in_=pT[:, c, :], func=AF.Identity,
                                                                                      scale=modf[:, l, v_s, c, s:s + 1], bias=modf[:, l, v_sh, c, s:s + 1]),
                                 reads=[kT], writes=[kh])
                    S.dma("sp", hT[:, :, ci * 512:(ci + 1) * 512], ht[:], reads=[kh])
                S.emit()

        def proj_phase(tag, l):
            with ExitStack() as es:
                S = Sched(nc, es, tag + "a")
                wq, kwq = one(nc, es, "wq", [128, KC, 3584], BF16)
                wsrc = w_in[l].rearrange("(kc p) n -> p kc n", p=128)
                S.dma("pool", wq[:, :, 0:2048], wsrc[:, :, 0:2048], writes=[kwq])
                S.dma("pool", wq[:, :, 2048:3072], wsrc[:, :, 3072:4096], writes=[kwq])
                for g in range(4):
                    for hh in range(2):
                        c0 = 3072 + g * 128 + hh * 64
                        S.dma("pool", wq[:, :, c0:c0 + 64], wsrc[:, :, 4096 + g * 64:4096 + (g + 1) * 64], writes=[kwq])
                hr = Ring(nc, es, "ph", [128, KC, 512], BF16, 4)
                tbr = Ring(nc, es, "ptab", [128, 4, 512], F32, 4)
                pr = Ring(nc, es, "pps", [128, 512], F32, 4, psum=True)
                p2 = Ring(nc, es, "pps2", [128, 512], F32, 3, psum=True)
                qsr = Ring(nc, es, "pqs", [128, 512], BF16, 8)
                sqr = Ring(nc, es, "psq", [128, 512], BF16, 3)
                rrr = Ring(nc, es, "prr", [128, 512], F32, 3)
                t1r = Ring(nc, es, "pt1", [128, 512], F32, 4)
                t2r = Ring(nc, es, "pt2", [128, 512], F32, 4)
                outr = Ring(nc, es, "pout", [128, 512], BF16, 4)
                def blk_gen(blk, ht, kh, tb, ktb, t0, k0):
                    ps, kp = pr.next()
                    for kc in range(KC):
                        S.op("pe", lambda P, ps=ps, ht=ht, blk=blk, kc=kc: P.matmul(ps[:], wq[:, kc, blk * 128:(blk + 1) * 128], ht[:, kc, :], start=(kc == 0), stop=(kc == KC - 1)),
                             reads=[kwq, kh], writes=[kp], sig=(kc == KC - 1))
                    yield
                    qs, kq = qsr.next()
                    if blk < 16:
                        S.op("act", lambda A, qs=qs, ps=ps: A.copy(qs[:], ps[:]), reads=[kp], writes=[kq])
                        PM, ti = PD, 0
                        dst = QT[blk, :, t0:t0 + 512] if blk < 8 else KT[blk - 8, :, k0:k0 + 512]
                        yield
                    else:
                        sq, ksq = sqr.next()
                        S.op("act", lambda A, sq=sq, ps=ps: A.activation(out=sq[:], in_=ps[:], func=AF.Square), reads=[kp], writes=[ksq])
                        yield
                        pss, kps = p2.next()
                        S.op("pe", lambda P, pss=pss, sq=sq: P.matmul(pss[:], BONES, sq[:], start=True, stop=True), reads=[ksq], writes=[kps])
                        yield
                        rr, krr = rrr.next()
                        S.op("act", lambda A, rr=rr, pss=pss: A.activation(out=rr[:], in_=pss[:], func=AF.Sqrt, scale=1.0, bias=64.0 * EPS),
                             reads=[kps], writes=[krr])
                        S.op("dve", lambda V, rr=rr: V.reciprocal(rr[:], rr[:]), reads=[krr], writes=[krr])
                        gv = gq8 if blk < 24 else gk8
                        S.op("dve", lambda V, qs=qs, ps=ps, rr=rr, gv=gv: V.scalar_tensor_tensor(out=qs[:], in0=ps[:], scalar=gv[:, l:l + 1], in1=rr[:], op0=ALU.mult, op1=ALU.mult),
                             reads=[kp, krr], writes=[kq])
                        PM, ti = PG, 2
                        dst = QT[8 + blk - 16, :, t0:t0 + 512] if blk < 24 else KT[8 + blk - 24, :, k0:k0 + 512]
                        yield
                    pp, kpp = p2.next()
                    S.op("pe", lambda P, pp=pp, qs=qs, PM=PM: P.matmul(pp[:], PM, qs[:], start=True, stop=True), reads=[kq], writes=[kpp])
                    yield
                    t1, kt1 = t1r.next()
                    S.op("pool", lambda G, t1=t1, qs=qs, tb=tb, ti=ti: G.tensor_tensor(out=t1[:], in0=qs[:], in1=tb[:, ti, :], op=ALU.mult),
                         reads=[kq, ktb], writes=[kt1])
                    t2, kt2 = t2r.next()
                    S.op("dve", lambda V, t2=t2, pp=pp, tb=tb, ti=ti: V.tensor_tensor(out=t2[:], in0=pp[:], in1=tb[:, ti + 1, :], op=ALU.mult),
                         reads=[kpp, ktb], writes=[kt2])
                    yield
                    ot, ko = outr.next()
                    S.op("pool", lambda G, ot=ot, t1=t1, t2=t2: G.tensor_tensor(out=ot[:], in0=t1[:], in1=t2[:], op=ALU.add),
                         reads=[kt1, kt2], writes=[ko])
                    S.dma("sp", dst, ot[:], reads=[ko])

                active = []

                def step_all():
                    for g_ in list(active):
                        try:
                            next(g_)
                        except StopIteration:
                            active.remove(g_)

                def load_a(ci):
                    t0 = ci * 512
                    ht, kh = hr.next()
                    S.dma("sp", ht[:], hT[:, :, t0:t0 + 512], writes=[kh])
                    tb, ktb = tbr.next()
                    S.dma("sp", tb[:], tabs[:, :, t0:t0 + 512].rearrange("a p t -> p a t"), writes=[ktb])
                    return ht, kh, tb, ktb

                nxt_a = load_a(0)
                for ci in range(NCH):
                    t0 = ci * 512
                    k0 = key_off(t0)
                    ht, kh, tb, ktb = nxt_a
                    if ci + 1 < NCH:
                        nxt_a = load_a(ci + 1)
                    for blk in range(28):
                        g_ = blk_gen(blk, ht, kh, tb, ktb, t0, k0)
                        next(g_)
                        step_all()
                        active.append(g_)
                while active:
                    step_all()
                S.emit()
            with ExitStack() as es:
                S = Sched(nc, es, tag + "b")
                wg, kwg = one(nc, es, "wg", [128, KC, 3328], BF16)
                wsrc = w_in[l].rearrange("(kc p) n -> p kc n", p=128)
                S.dma("pool", wg[:, :, 0:2048], wsrc[:, :, 4608:6656], writes=[kwg])
                S.dma("pool", wg[:, :, 2048:3072], wsrc[:, :, 2048:3072], writes=[kwg])
                S.dma("pool", wg[:, :, 3072:3328], wsrc[:, :, 4352:4608], writes=[kwg])
                hr = Ring(nc, es, "qh", [128, KC, 512], BF16, 3)
                pr = Ring(nc, es, "qps", [128, 512], F32, 6, psum=True)
                outr = Ring(nc, es, "qout", [128, 512], BF16, 4)
                vr = Ring(nc, es, "qv", [128, 1280], BF16, 3)
                def load_b(ci):
                    ht, kh = hr.next()
                    S.dma("sp", ht[:], hT[:, :, ci * 512:(ci + 1) * 512], writes=[kh])
                    return ht, kh

                nxt_b = load_b(0)
                for ci in range(NCH):
                    t0 = ci * 512
                    k0 = key_off(t0)
                    ht, kh = nxt_b
                    if ci + 1 < NCH:
                        nxt_b = load_b(ci + 1)
                    for blk in range(16):
                        ps, kp = pr.next()
                        for kc in range(KC):
                            S.op("pe", lambda P, ps=ps, ht=ht, blk=blk, kc=kc: P.matmul(ps[:], wg[:, kc, blk * 128:(blk + 1) * 128], ht[:, kc, :], start=(kc == 0), stop=(kc == KC - 1)),
                                 reads=[kwg, kh], writes=[kp], sig=(kc == KC - 1))
                        ot, ko = outr.next()
                        S.op("act", lambda A, ot=ot, ps=ps: A.activation(out=ot[:], in_=ps[:], func=AF.Sigmoid), reads=[kp], writes=[ko])
                        S.dma("sp", GT[blk, :, t0:t0 + 512], ot[:], reads=[ko])
                    for tt in range(4):
                        vt, kv = vr.next()
                        for n0, nn in ((0, 512), (512, 512), (1024, 256)):
                            ps, kp = pr.next()
                            for kc in range(KC):
                                S.op("pe", lambda P, ps=ps, ht=ht, tt=tt, kc=kc, n0=n0, nn=nn: P.matmul(ps[:, 0:nn], ht[:, kc, tt * 128:(tt + 1) * 128], wg[:, kc, 2048 + n0:2048 + n0 + nn],
                                                                                                        start=(kc == 0), stop=(kc == KC - 1)),
                                     reads=[kwg, kh], writes=[kp], sig=(kc == KC - 1))
                            eng = "dve" if n0 == 512 else "act"
                            if eng == "dve":
                                S.op("dve", lambda V, vt=vt, ps=ps, n0=n0, nn=nn: V.tensor_copy(vt[:, n0:n0 + nn], ps[:, 0:nn]), reads=[kp], writes=[kv])
                            else:
                                S.op("act", lambda A, vt=vt, ps=ps, n0=n0, nn=nn: A.copy(vt[:, n0:n0 + nn], ps[:, 0:nn]), reads=[kp], writes=[kv])
                        S.dma("sp", Vs[k0 + tt * 128:k0 + (tt + 1) * 128, :], vt[:], reads=[kv])
                S.emit()

        def attn_phase(tag, l):
            with ExitStack() as es:
                S = Sched(nc, es, tag)
                SKM = T
                NQC = T // 512
                mb, kmb = one(nc, es, "amb", [128, (T // 128) * NQC], F32)
                S.dma("sp", mb[:], maskb, writes=[kmb])
                ktd = Ring(nc, es, "aktd", [128, SKM], BF16, 2)
                ktg = Ring(nc, es, "aktg", [128, SKM], BF16, 2)
                vdr = Ring(nc, es, "avd", [128, SKM // 128, 128], BF16, 2)
                vgr = Ring(nc, es, "avg", [128, SKM // 128, 64], BF16, 2)
                qdr = Ring(nc, es, "aqd", [128, 512], BF16, 3)
                qgr = Ring(nc, es, "aqg", [128, 512], BF16, 3)
                gar = Ring(nc, es, "aga", [128, 512], BF16, 3)
                gbr = Ring(nc, es, "agb", [128, 512], BF16, 3)
                scr_ = Ring(nc, es, "asc", [128, 2, 512], F32, 2, psum=True)
                acc = Ring(nc, es, "aacc", [128, 512], F32, 4, psum=True)
                er = Ring(nc, es, "ae", [128, 2, 512], BF16, 3)
                fr = Ring(nc, es, "af", [128, 512], F32, 8)
                sqr = Ring(nc, es, "asq", [128, 512], BF16, 2)
                mor = Ring(nc, es, "amo", [128, 512], BF16, 2)
                for s in range(1):
                    SK = T
                    kb = 0
                    q_lo, q_hi = 0, T
                    nkt = SK // 128
                    kg_t = vg_t = None
                    for j in range(8):
                        kd, kkd = ktd.next()
                        S.dma("sp", kd[:, 0:SK], KT[j, :, kb:kb + SK], writes=[kkd])
                        vd, kvd = vdr.next()
                        S.dma("sp", vd[:, 0:nkt, :], Vs[kb:kb + SK, j * 128:(j + 1) * 128].rearrange("(kt p) e -> p kt e", p=128), writes=[kvd])
                        if j % 2 == 0:
                            g = j // 2
                            kg_t, kkg = ktg.next()
                            S.dma("sp", kg_t[:, 0:SK], KT[8 + g, :, kb:kb + SK], writes=[kkg])
                            vg_t, kvg = vgr.next()
                            S.dma("sp", vg_t[:, 0:nkt, :], Vs[kb:kb + SK, 1024 + g * 64:1024 + (g + 1) * 64].rearrange("(kt p) e -> p kt e", p=128), writes=[kvg])
                        def load_q(t0, j=j):
                            qd, kqd = qdr.next()
                            S.dma("sp", qd[:], QT[j, :, t0:t0 + 512], writes=[kqd])
                            qg, kqg = qgr.next()
                            S.dma("sp", qg[:], QT[8 + j, :, t0:t0 + 512], writes=[kqg])
                            ga, kga = gar.next()
                            S.dma("sp", ga[:], GT[j, :, t0:t0 + 512], writes=[kga])
                            gb, kgb = gbr.next()
                            S.dma("sp", gb[:], GT[8 + j, :, t0:t0 + 512], writes=[kgb])
                            return qd, kqd, qg, kqg, ga, kga, gb, kgb

                        nxt_q = load_q(q_lo)
                        for t0 in range(q_lo, q_hi, 512):
                            qc = t0 // 512
                            qd, kqd, qg, kqg, ga, kga, gb, kgb = nxt_q
                            if t0 + 512 < q_hi:
                                nxt_q = load_q(t0 + 512)
                            ov1, ko1 = acc.next()
                            ov2, ko2 = acc.next()
                            rb1, kr1 = acc.next()
                            rb2, kr2 = acc.next()
                            def qk_d(kt, kd=kd, qd=qd, kkd=kkd, kqd=kqd):
                                sc, ksc = scr_.next()
                                for m in range(2):
                                    S.op("pe", lambda P, sc=sc, kt=kt, m=m: P.matmul(sc[:, m, :], kd[m * 64:(m + 1) * 64, kt * 128:(kt + 1) * 128], qd[m * 64:(m + 1) * 64, :],
                                                                                    start=True, stop=True),
                                         reads=[kkd, kqd], writes=[ksc], sig=(m == 1))
                                return sc, ksc
                            nxt = qk_d(0)
                            for kt in range(nkt):
                                sc, ksc = nxt
                                if kt + 1 < nkt:
                                    nxt = qk_d(kt + 1)
                                et, ke = er.next()
                                S.op("act", lambda A, et=et, sc=sc, kt=kt, qc=qc: A.activation(out=et[:], in_=sc[:], func=AF.Exp, scale=0.125, bias=mb[:, kt * NQC + qc:kt * NQC + qc + 1]), reads=[ksc, kmb], writes=[ke])
                                st, sp_ = (kt == 0), (kt == nkt - 1)
                                S.op("pe", lambda P, ov1=ov1, vd=vd, et=et, kt=kt, st=st, sp_=sp_: P.matmul(ov1[:], vd[:, kt, :], et[:, 0, :], start=st, stop=sp_),
                                     reads=[kvd, ke], writes=[ko1], sig=False)
                                S.op("pe", lambda P, ov2=ov2, vd=vd, et=et, kt=kt, st=st, sp_=sp_: P.matmul(ov2[:], vd[:, kt, :], et[:, 1, :], start=st, stop=sp_),
                                     reads=[kvd, ke], writes=[ko2], sig=False)
                                S.op("pe", lambda P, rb1=rb1, et=et, st=st, sp_=sp_: P.matmul(rb1[:], ONES, et[:, 0, :], start=st, stop=sp_),
                                     reads=[ke], writes=[kr1], sig=False)
                                S.op("pe", lambda P, rb2=rb2, et=et, st=st, sp_=sp_: P.matmul(rb2[:], ONES, et[:, 1, :], start=st, stop=sp_),
                                     reads=[ke], writes=[kr2], sig=True)
                            r1, k1 = fr.next()
                            S.op("dve", lambda V, r1=r1, rb1=rb1: V.reciprocal(r1[:], rb1[:]), reads=[kr1], writes=[k1])
                            r2, k2 = fr.next()
                            S.op("dve", lambda V, r2=r2, rb2=rb2: V.reciprocal(r2[:], rb2[:]), reads=[kr2], writes=[k2])
                            a1, ka1 = fr.next()
                            S.op("dve", lambda V, a1=a1, ov1=ov1, r1=r1: V.tensor_tensor(out=a1[:], in0=ov1[:], in1=r1[:], op=ALU.mult), reads=[ko1, k1], writes=[ka1])
                            a2, ka2 = fr.next()
                            S.op("dve", lambda V, a2=a2, ov2=ov2, r2=r2: V.tensor_tensor(out=a2[:], in0=ov2[:], in1=r2[:], op=ALU.mult), reads=[ko2, k2], writes=[ka2])
                            S.op("dve", lambda G, a1=a1, a2=a2: G.scalar_tensor_tensor(out=a1[:], in0=a2[:], scalar=nlam[:, l:l + 1], in1=a1[:], op0=ALU.mult, op1=ALU.add),
                                 reads=[ka2, ka1], writes=[ka1])
                            sq, ksq = sqr.next()
                            S.op("pool", lambda G, sq=sq, a1=a1: G.tensor_tensor(out=sq[:], in0=a1[:], in1=a1[:], op=ALU.mult), reads=[ka1], writes=[ksq])
                            ssq, kss = acc.next()
                            S.op("pe", lambda P, ssq=ssq, sq=sq: P.matmul(ssq[:], ONES, sq[:], start=True, stop=True), reads=[ksq], writes=[kss])
                            rn, krn = fr.next()
                            S.op("act", lambda A, rn=rn, ssq=ssq: A.activation(out=rn[:], in_=ssq[:], func=AF.Sqrt, scale=1.0, bias=128.0 * EPS), reads=[kss], writes=[krn])
                            S.op("dve", lambda V, rn=rn: V.reciprocal(rn[:], rn[:]), reads=[krn], writes=[krn])
                            S.op("dve", lambda V, a1=a1, rn=rn: V.scalar_tensor_tensor(out=a1[:], in0=a1[:], scalar=wsub[:, l:l + 1], in1=rn[:], op0=ALU.mult, op1=ALU.mult),
                                 reads=[ka1, krn], writes=[ka1])
                            S.op("pool", lambda G, a1=a1, ga=ga: G.tensor_tensor(out=a1[:], in0=a1[:], in1=ga[:], op=ALU.mult), reads=[ka1, kga], writes=[ka1])
                            ovg, kog = acc.next()
                            rbg, krg = acc.next()
                            def qk_g(kt, kg_t=kg_t, qg=qg, kkg=kkg, kqg=kqg):
                                sc, ksc = scr_.next()
                                for m in range(2):
                                    S.op("pe", lambda P, sc=sc, kt=kt, m=m: P.matmul(sc[:, m, :], kg_t[m * 64:(m + 1) * 64, kt * 128:(kt + 1) * 128], qg[m * 64:(m + 1) * 64, :],
                                                                                    start=True, stop=True),
                                         reads=[kkg, kqg], writes=[ksc], sig=(m == 1))
                                return sc, ksc
                            nxt = qk_g(0)
                            for kt in range(nkt):
                                sc, ksc = nxt
                                if kt + 1 < nkt:
                                    nxt = qk_g(kt + 1)
                                et, ke = er.next()
                                S.op("act", lambda A, et=et, sc=sc, kt=kt, qc=qc: A.activation(out=et[:], in_=sc[:], func=AF.Exp, scale=0.125, bias=mb[:, kt * NQC + qc:kt * NQC + qc + 1]), reads=[ksc, kmb], writes=[ke])
                                st, sp_ = (kt == 0), (kt == nkt - 1)
                                for m in range(2):
                                    S.op("pe", lambda P, ovg=ovg, vg_t=vg_t, et=et, kt=kt, m=m, st=st, sp_=sp_: P.matmul(ovg[m * 64:(m + 1) * 64, :], vg_t[:, kt, :], et[:, m, :], start=st, stop=sp_),
                                         reads=[kvg, ke], writes=[kog], sig=False)
                                for m in range(2):
                                    S.op("pe", lambda P, rbg=rbg, et=et, m=m, st=st, sp_=sp_: P.matmul(rbg[m * 64:(m + 1) * 64, :], cm[:, 4, 0:64], et[:, m, :], start=st, stop=sp_),
                                         reads=[ke], writes=[krg], sig=(m == 1))
                            rg, krg2 = fr.next()
                            S.op("dve", lambda V, rg=rg, rbg=rbg: V.reciprocal(rg[:], rbg[:]), reads=[krg], writes=[krg2])
                            bt, kbt = fr.next()
                            S.op("dve", lambda V, bt=bt, ovg=ovg, rg=rg: V.tensor_tensor(out=bt[:], in0=ovg[:], in1=rg[:], op=ALU.mult), reads=[kog, krg2], writes=[kbt])
                            S.op("pool", lambda G, bt=bt, gb=gb: G.tensor_tensor(out=bt[:], in0=bt[:], in1=gb[:], op=ALU.mult), reads=[kbt, kgb], writes=[kbt])
                            mo, kmo = mor.next()
                            S.op("pool", lambda G, mo=mo, bt=bt, a1=a1: G.tensor_tensor(out=mo[:], in0=bt[:], in1=a1[:], op=ALU.add), reads=[kbt, ka1], writes=[kmo])
                            S.dma("sp", mT[:, j, t0:t0 + 512], mo[:], reads=[kmo])
                S.emit()

        def outproj_phase(tag, l, src):
            with ExitStack() as es:
                S = Sched(nc, es, tag)
                wo, kwo = one(nc, es, "wo", [128, KC, D], BF16)
                S.dma("pool", wo[:], w_out[l].rearrange("(kc p) n -> p kc n", p=128), writes=[kwo])
                g1 = [one(nc, es, f"g1_{s}", [128, D], F32) for s in range(2)]
                for s in range(2):
                    S.dma("sp", g1[s][0][:], modg[l, 0, s], writes=[g1[s][1]])
                mr = Ring(nc, es, "om", [128, KC, 512], BF16, 3)
                xr = Ring(nc, es, "ox", [128, D], F32, 9)
                pr = Ring(nc, es, "ops", [128, 2, 512], F32, 2, psum=True)
                yr = Ring(nc, es, "oy", [128, D], F32, 3)
                def load_o(ci):
                    t0 = ci * 512
                    mt, kmt = mr.next()
                    S.dma("sp", mt[:], mT[:, :, t0:t0 + 512], writes=[kmt])
                    xs_ = []
                    for tt in range(4):
                        xt, kx = xr.next()
                        S.dma("sp", xt[:], src[t0 + tt * 128:t0 + (tt + 1) * 128, :], writes=[kx])
                        xs_.append((xt, kx))
                    return mt, kmt, xs_

                nxt_o = load_o(0)
                for ci in range(NCH):
                    t0 = ci * 512
                    s = slot_of(t0)
                    mt, kmt, xs_ = nxt_o
                    if ci + 1 < NCH:
                        nxt_o = load_o(ci + 1)
                    for tt in range(4):
                        r0 = t0 + tt * 128
                        xt, kx = xs_[tt]
                        ps, kp = pr.next()
                        for h in range(2):
                            for kc in range(KC):
                                S.op("pe", lambda P, ps=ps, mt=mt, tt=tt, kc=kc, h=h: P.matmul(ps[:, h, :], mt[:, kc, tt * 128:(tt + 1) * 128], wo[:, kc, h * 512:(h + 1) * 512],
                                                                                              start=(kc == 0), stop=(kc == KC - 1)),
                                     reads=[kmt, kwo], writes=[kp], sig=(h == 1 and kc == KC - 1))
                        yt, ky = yr.next()
                        S.op("dve", lambda V, yt=yt, ps=ps, s=s: V.tensor_tensor(out=yt[:].rearrange("p (a b) -> p a b", a=2), in0=ps[:], in1=g1[s][0][:].rearrange("p (a b) -> p a b", a=2), op=ALU.mult),
                             reads=[kp, g1[s][1]], writes=[ky])
                        S.op("pool", lambda G, yt=yt, xt=xt: G.tensor_tensor(out=yt[:], in0=yt[:], in1=xt[:], op=ALU.add), reads=[ky, kx], writes=[ky])
                        S.dma("sp", xres[r0:r0 + 128, :], yt[:], reads=[ky])
                S.emit()

        def ffn_pass(tag, l, wg_src, wu_src, wd_src, f0, nf, mode, e_idx, gates_t, first, last):
            with ExitStack() as es:
                S = Sched(nc, es, tag)
                wg, kwg = one(nc, es, "fwg", [128, KC, nf * 128], BF16)
                wu, kwu = one(nc, es, "fwu", [128, KC, nf * 128], BF16)
                wd, kwd = one(nc, es, "fwd", [128, nf, D], BF16)
                S.dma("pool", wg[:], wg_src[:, f0:f0 + nf * 128].rearrange("(kc p) n -> p kc n", p=128), writes=[kwg])
                S.dma("pool", wu[:], wu_src[:, f0:f0 + nf * 128].rearrange("(kc p) n -> p kc n", p=128), writes=[kwu])
                S.dma("pool", wd[:], wd_src[f0:f0 + nf * 128, :].rearrange("(f p) n -> p f n", p=128), writes=[kwd])
                g2 = [one(nc, es, f"g2_{s}", [128, D], F32) for s in range(2)]
                if mode == "dense" or last:
                    for s in range(2):
                        S.dma("sp", g2[s][0][:], modg[l, 1, s], writes=[g2[s][1]])
                hr = Ring(nc, es, "fh", [128, KC, 512], BF16, 3)
                pr = Ring(nc, es, "fps", [128, 2, 512], F32, 2, psum=True)
                po = Ring(nc, es, "fpo", [128, 2, 512], F32, 2, psum=True)
                sr = Ring(nc, es, "fsg", [128, 512], F32, 2)
                ar = Ring(nc, es, "fact", [128, nf, 512], BF16, 2)
                xr = Ring(nc, es, "fx", [128, D], F32, 10 if mode == "moe" else 2)
                x2r = Ring(nc, es, "fx2", [128, D], F32, 2)
                yr = Ring(nc, es, "fy", [128, D], F32, 2)
                pre_acc = (mode == "moe" and not first)

                def load_f(ci):
                    t0 = ci * 512
                    ht, kh = hr.next()
                    S.dma("sp", ht[:], hT[:, :, t0:t0 + 512], writes=[kh])
                    return ht, kh

                def load_x(ci):
                    t0 = ci * 512
                    xs_ = []
                    if pre_acc:
                        for tt in range(4):
                            xt, kx = xr.next()
                            S.dma("sp", xt[:], yacc[t0 + tt * 128:t0 + (tt + 1) * 128, :], writes=[kx])
                            xs_.append((xt, kx))
                    return xs_

                def stage_a(ci, ht, kh):
                    at, ka = ar.next()
                    for f in range(nf):
                        ps, kp = pr.next()
                        for wi, (ww, kw) in enumerate(((wg, kwg), (wu, kwu))):
                            for kc in range(KC):
                                S.op("pe", lambda P, ps=ps, ht=ht, ww=ww, wi=wi, f=f, kc=kc: P.matmul(ps[:, wi, :], ww[:, kc, f * 128:(f + 1) * 128], ht[:, kc, :], start=(kc == 0), stop=(kc == KC - 1)),
                                     reads=[kw, kh], writes=[kp], sig=(wi == 1 and kc == KC - 1))
                        sg, ksg = sr.next()
                        S.op("act", lambda A, sg=sg, ps=ps: A.activation(out=sg[:], in_=ps[:, 0, :], func=AF.Silu), reads=[kp], writes=[ksg])
                        S.op("dve", lambda V, at=at, f=f, sg=sg, ps=ps: V.tensor_tensor(out=at[:, f, :], in0=ps[:, 1, :], in1=sg[:], op=ALU.mult), reads=[kp, ksg], writes=[ka])
                    return at, ka

                def stage_b(ci, at, ka, xs_):
                    t0 = ci * 512
                    s = slot_of(t0)
                    for tt in range(4):
                        r0 = t0 + tt * 128
                        ps, kp = po.next()
                        for h in range(2):
                            for f in range(nf):
                                S.op("pe", lambda P, ps=ps, at=at, tt=tt, f=f, h=h: P.matmul(ps[:, h, :], at[:, f, tt * 128:(tt + 1) * 128], wd[:, f, h * 512:(h + 1) * 512], start=(f == 0), stop=(f == nf - 1)),
                                     reads=[ka, kwd], writes=[kp], sig=(h == 1 and f == nf - 1))
                        psf = ps[:]
                        v3 = lambda t: t[:].rearrange("p (a b) -> p a b", a=2)
                        yt, ky = yr.next()
                        if pre_acc:
                            xt, kx = xs_[tt]
                        else:
                            xt, kx = xr.next()
                        if mode == "dense":
                            S.dma("sp", xt[:], xres[r0:r0 + 128, :], writes=[kx])
                            S.op("dve", lambda V, yt=yt, psf=psf, s=s: V.tensor_tensor(out=v3(yt), in0=psf, in1=v3(g2[s][0]), op=ALU.mult), reads=[kp, g2[s][1]], writes=[ky])
                            S.op("pool", lambda G, yt=yt, xt=xt: G.tensor_tensor(out=yt[:], in0=yt[:], in1=xt[:], op=ALU.add), reads=[ky, kx], writes=[ky])
                            S.dma("sp", xres[r0:r0 + 128, :], yt[:], reads=[ky])
                        else:
                            gcol = gates_t[0][:, r0 // 128, e_idx:e_idx + 1]
                            if first:
                                S.op("dve", lambda V, yt=yt, psf=psf, gcol=gcol: V.tensor_scalar(out=v3(yt), in0=psf, scalar1=gcol, scalar2=None, op0=ALU.mult),
                                     reads=[kp, gates_t[1]], writes=[ky])
                            else:
                                S.op("dve", lambda V, yt=yt, psf=psf, gcol=gcol, xt=xt: V.scalar_tensor_tensor(out=v3(yt), in0=psf, scalar=gcol, in1=v3(xt), op0=ALU.mult, op1=ALU.add),
                                     reads=[kp, gates_t[1], kx], writes=[ky])
                            if last:
                                x2, kx2 = x2r.next()
                                S.dma("sp", x2[:], xres[r0:r0 + 128, :], writes=[kx2])
                                S.op("pool", lambda G, yt=yt, s=s: G.tensor_tensor(out=yt[:], in0=yt[:], in1=g2[s][0][:], op=ALU.mult), reads=[ky, g2[s][1]], writes=[ky])
                                S.op("pool", lambda G, yt=yt, x2=x2: G.tensor_tensor(out=yt[:], in0=yt[:], in1=x2[:], op=ALU.add), reads=[ky, kx2], writes=[ky])
                                S.dma("sp", xres[r0:r0 + 128, :], yt[:], reads=[ky])
                            else:
                                S.dma("sp", yacc[r0:r0 + 128, :], yt[:], reads=[ky])

                ld = {0: load_f(0)}
                if NCH > 1:
                    ld[1] = load_f(1)
                lx = {0: load_x(0)}
                ats = {0: stage_a(0, ld[0][0], ld[0][1])}
                for ci in range(NCH):
                    if ci + 2 < NCH:
                        ld[ci + 2] = load_f(ci + 2)
                    if ci + 1 < NCH:
                        lx[ci + 1] = load_x(ci + 1)
                        ats[ci + 1] = stage_a(ci + 1, ld[ci + 1][0], ld[ci + 1][1])
                    stage_b(ci, ats[ci][0], ats[ci][1], lx[ci])
                S.emit()

        def router_phase(tag, l, gates_t):
            li = l // 2
            with ExitStack() as es:
                S = Sched(nc, es, tag)
                rw, krw = one(nc, es, "rw", [128, KC, NE], F32)
                with nc.allow_non_contiguous_dma("router weights are tiny"):
                    S.dma("sp", rw[:], router_w[li].rearrange("(kc p) e -> p kc e", p=128), writes=[krw])
                idf, kidf = one(nc, es, "idf", [128, 128], F32)
                S.dma("sp", idf[:], cmats[:, 0, :], writes=[kidf])
                xr = Ring(nc, es, "rx", [128, D], F32, 3)
                jr = Ring(nc, es, "rjunk", [128, D], BF16, 1)
                sr = Ring(nc, es, "rss", [128, 4], F32, 4)
                pT = Ring(nc, es, "rT", [128, KC, 128], F32, 2, psum=True)
                hr = Ring(nc, es, "rh", [128, KC, 128], F32, 2)
                pl = Ring(nc, es, "rpl", [128, 512], F32, 2, psum=True)
                lr = Ring(nc, es, "rl", [128, 4, NE], F32, 3)
                mr = Ring(nc, es, "rm", [128, 8], F32, 3)
                gt, kg = gates_t
                for ti in range(T // 128):
                    r0 = ti * 128
                    s = slot_of(r0)
                    xt, kx = xr.next()
                    S.dma("sp", xt[:], xres[r0:r0 + 128, :], writes=[kx])
                    jt, kj = jr.next()
                    st, ks = sr.next()
                    S.op("dve", lambda V, st=st: V.memset(st[:, 0:1], 0.0), writes=[ks])
                    S.op("act", lambda A, jt=jt, xt=xt, st=st: A.activation(out=jt[:], in_=xt[:], func=AF.Square, accum_out=st[:, 0:1]), reads=[kx], writes=[kj, ks])
                    S.op("act", lambda A, st=st: A.activation(out=st[:, 1:2], in_=st[:, 0:1], func=AF.Sqrt, scale=1.0 / D, bias=EPS), reads=[ks], writes=[ks])
                    S.op("dve", lambda V, st=st: V.reciprocal(st[:, 2:3], st[:, 1:2]), reads=[ks], writes=[ks])
                    S.op("pool", lambda G, xt=xt, st=st: G.tensor_scalar(out=xt[:], in0=xt[:], scalar1=st[:, 2:3], scalar2=None, op0=ALU.mult), reads=[kx, ks], writes=[kx])
                    pt, kpt = pT.next()
                    for c in range(KC):
                        S.op("pe", lambda P, pt=pt, xt=xt, c=c: P.transpose(pt[:, c, :], xt[:, c * 128:(c + 1) * 128], idf[:]), reads=[kx, kidf], writes=[kpt], sig=(c == KC - 1))
                    ht, kh = hr.next()
                    for c in range(KC):
                        S.op("dve", lambda V, ht=ht, pt=pt, c=c, s=s: V.tensor_scalar(out=ht[:, c, :], in0=pt[:, c, :], scalar1=modf[:, l, 3, c, s:s + 1], scalar2=modf[:, l, 2, c, s:s + 1],
                                                                                     op0=ALU.mult, op1=ALU.add), reads=[kpt], writes=[kh])
                    lg, klg = pl.next()
                    for kc in range(KC):
                        S.op("pe", lambda P, lg=lg, ht=ht, kc=kc: P.matmul(lg[:, 0:NE], ht[:, kc, :], rw[:, kc, :], start=(kc == 0), stop=(kc == KC - 1)),
                             reads=[kh, krw], writes=[klg], sig=(kc == KC - 1))
                    lt, klt = lr.next()
                    mt, kmt = mr.next()
                    S.op("dve", lambda V, lt=lt, lg=lg: V.tensor_copy(lt[:, 0, :], lg[:, 0:NE]), reads=[klg], writes=[klt])
                    S.op("dve", lambda V, lt=lt, mt=mt: V.tensor_reduce(out=mt[:, 0:1], in_=lt[:, 0, :], axis=AX.X, op=ALU.max), reads=[klt], writes=[kmt])
                    S.op("dve", lambda V, lt=lt, mt=mt: V.tensor_scalar(out=lt[:, 1, :], in0=lt[:, 0, :], scalar1=mt[:, 0:1], scalar2=None, op0=ALU.is_ge), reads=[klt, kmt], writes=[klt])
                    S.op("dve", lambda V, lt=lt: V.scalar_tensor_tensor(out=lt[:, 2, :], in0=lt[:, 1, :], scalar=-1e30, in1=lt[:, 0, :], op0=ALU.mult, op1=ALU.add), reads=[klt], writes=[klt])
                    S.op("dve", lambda V, lt=lt, mt=mt: V.tensor_reduce(out=mt[:, 1:2], in_=lt[:, 2, :], axis=AX.X, op=ALU.max), reads=[klt], writes=[kmt])
                    S.op("dve", lambda V, lt=lt, mt=mt: V.tensor_scalar(out=lt[:, 3, :], in0=lt[:, 0, :], scalar1=mt[:, 1:2], scalar2=None, op0=ALU.is_ge), reads=[klt, kmt], writes=[klt])
                    S.op("dve", lambda V, mt=mt: V.tensor_scalar(out=mt[:, 2:3], in0=mt[:, 0:1], scalar1=-1.0, scalar2=None, op0=ALU.mult), reads=[kmt], writes=[kmt])
                    S.op("act", lambda A, lt=lt, mt=mt: A.activation(out=lt[:, 1, :], in_=lt[:, 0, :], func=AF.Exp, bias=mt[:, 2:3], scale=1.0), reads=[klt, kmt], writes=[klt])
                    S.op("dve", lambda V, lt=lt: V.tensor_tensor(out=lt[:, 2, :], in0=lt[:, 1, :], in1=lt[:, 3, :], op=ALU.mult), reads=[klt], writes=[klt])
                    S.op("dve", lambda V, lt=lt, mt=mt: V.tensor_reduce(out=mt[:, 3:4], in_=lt[:, 2, :], axis=AX.X, op=ALU.add), reads=[klt], writes=[kmt])
                    S.op("dve", lambda V, mt=mt: V.reciprocal(mt[:, 4:5], mt[:, 3:4]), reads=[kmt], writes=[kmt])
                    S.op("dve", lambda V, lt=lt, mt=mt, ti=ti: V.tensor_scalar(out=gt[:, ti, :], in0=lt[:, 2, :], scalar1=mt[:, 4:5], scalar2=None, op0=ALU.mult), reads=[klt, kmt], writes=[kg])
                S.emit()

        def final_phase(tag, src):
            with ExitStack() as es:
                S = Sched(nc, es, tag)
                fb, kfb = one(nc, es, "fnb", [128, D], F32)
                S.dma("sp", fb[:], final_norm.partition_broadcast(128), writes=[kfb])
                xr = Ring(nc, es, "zx", [128, D], F32, 3)
                jr = Ring(nc, es, "zjunk", [128, D], BF16, 1)
                sr = Ring(nc, es, "zss", [128, 4], F32, 4)
                for ti in range(T // 128):
                    r0 = ti * 128
                    xt, kx = xr.next()
                    S.dma("sp", xt[:], src[r0:r0 + 128, :], writes=[kx])
                    jt, kj = jr.next()
                    st, ks = sr.next()
                    S.op("dve", lambda V, st=st: V.memset(st[:, 0:1], 0.0), writes=[ks])
                    S.op("act", lambda A, jt=jt, xt=xt, st=st: A.activation(out=jt[:], in_=xt[:], func=AF.Square, accum_out=st[:, 0:1]), reads=[kx], writes=[kj, ks])
                    S.op("act", lambda A, st=st: A.activation(out=st[:, 1:2], in_=st[:, 0:1], func=AF.Sqrt, scale=1.0 / D, bias=EPS), reads=[ks], writes=[ks])
                    S.op("dve", lambda V, st=st: V.reciprocal(st[:, 2:3], st[:, 1:2]), reads=[ks], writes=[ks])
                    S.op("dve", lambda V, xt=xt, st=st: V.scalar_tensor_tensor(out=xt[:], in0=xt[:], scalar=st[:, 2:3], in1=fb[:], op0=ALU.mult, op1=ALU.mult), reads=[kx, ks, kfb], writes=[kx])
                    S.dma("sp", y_out[r0:r0 + 128, :], xt[:], reads=[kx])
                S.emit()

        gates_tile = gs.enter_context(nc.sbuf_tensor("gatesw", [128, T // 128, NE], F32))
        gates_t = (gates_tile, Trk())
        for l in range(L):
            src = x_in if l == 0 else xres
            norm_phase(f"n{l}a", src, l, 0, 1)
            proj_phase(f"p{l}", l)
            attn_phase(f"a{l}", l)
            outproj_phase(f"o{l}", l, src)
            norm_phase(f"n{l}b", xres, l, 2, 3)
            if l % 2 == 0:
                li = l // 2
                nf_all = DFF // 128
                halves = [(0, nf_all // 2), (nf_all // 2, nf_all - nf_all // 2)]
                for hi, (fs, nf) in enumerate(halves):
                    ffn_pass(f"f{l}_{hi}", l, ffn_w_gate[li], ffn_w_up[li], ffn_w_down[li], fs * 128, nf, "dense", 0, None, False, False)
            else:
                li = l // 2
                gates_t = (gates_tile, Trk())
                router_phase(f"r{l}", l, gates_t)
                nf_all = DFE // 128
                npart = 4 if nf_all % 4 == 0 else 2
                halves = [(i * (nf_all // npart), nf_all // npart) for i in range(npart)]
                for e in range(NE):
                    for hi, (fs, nf) in enumerate(halves):
                        first = (e == 0 and hi == 0)
                        last = (e == NE - 1 and hi == len(halves) - 1)
                        ffn_pass(f"m{l}_{e}_{hi}", l, moe_w_gate[li, e], moe_w_up[li, e], moe_w_down[li, e], fs * 128, nf, "moe", e,
                                 (gates_tile, Trk()), first, last)
        final_phase("fin", xres)
    return nc


def _consts():
    ident = np.eye(128, dtype=np.float32)
    pd = np.zeros((128, 128), np.float32)
    pg = np.zeros((128, 128), np.float32)
    for b in range(2):
        o = b * 64
        for i in range(8):
            pd[o + i + 8, o + i] = 1.0
            pd[o + i, o + i + 8] = 1.0
        for base in (0, 32):
            for i in range(16):
                pg[o + base + i + 16, o + base + i] = 1.0
                pg[o + base + i, o + base + i + 16] = 1.0
    bones = np.zeros((128, 128), np.float32)
    bones[0:64, 0:64] = 1.0
    bones[64:128, 64:128] = 1.0
    ones = np.ones((128, 128), np.float32)
    return np.ascontiguousarray(np.stack([ident, pd, pg, bones, ones], axis=1))


def _rope_tables(pos):
    pos = np.asarray(pos)
    n = pos.shape[0]
    tab = np.zeros((4, 128, n), np.float32)
    tab[0] = 1.0
    posf = pos.astype(np.float32)
    rowf = (pos // GRID_W).astype(np.float32)
    colf = (pos % GRID_W).astype(np.float32)
    invd = np.exp(-math.log(DIFF_THETA) * np.arange(8, dtype=np.float32) / 8).astype(np.float32)
    invg = np.exp(-math.log(AX_THETA) * np.arange(16, dtype=np.float32) / 16).astype(np.float32)
    for p in range(128):
        d = p % 64
        if d < 16:
            ang = posf * invd[d % 8]
            tab[0, p] = np.cos(ang)
            tab[1, p] = -np.sin(ang) if d < 8 else np.sin(ang)
        if d < 32:
            ang = rowf * invg[d % 16]
            sgn = -1.0 if d < 16 else 1.0
        else:
            ang = colf * invg[(d - 32) % 16]
            sgn = -1.0 if d < 48 else 1.0
        tab[2, p] = np.cos(ang)
        tab[3, p] = sgn * np.sin(ang)
    return tab


_WNAMES = ["w_ada", "b_ada", "norm1", "w_in", "diff_lambda", "diff_subln", "gqa_q_norm", "gqa_k_norm", "w_out", "norm2",
           "ffn_w_gate", "ffn_w_up", "ffn_w_down", "router_w", "moe_w_gate", "moe_w_up", "moe_w_down", "final_norm"]


def kernel(**inputs):
    xp = np.asarray(inputs["x_prompt"], np.float32)
    xs = np.asarray(inputs["x_sample"], np.float32)
    cp = np.asarray(inputs["c_prompt"], np.float32)
    cs = np.asarray(inputs["c_sample"], np.float32)
    B1, S1, _ = xp.shape
    B2, S2, _ = xs.shape
    assert S2 == 2 * S1 and B1 % 2 == 0
    L = inputs["w_in"].shape[0]
    DFF = inputs["ffn_w_gate"].shape[2]
    DFE = inputs["moe_w_gate"].shape[3]
    n = 8
    T = S2
    npair = B1 // 2
    assert B2 + npair <= n
    cfg = Cfg(L, T, DFF, DFE)
    nc = build(cfg)
    cm = _consts()
    tab_s = np.ascontiguousarray(_rope_tables(np.arange(T)))
    tab_p = np.ascontiguousarray(_rope_tables(np.concatenate([np.arange(S1), np.arange(S1)])))
    nkt, nqc = T // 128, T // 512
    mb_s = np.zeros((128, nkt * nqc), np.float32)
    mb_p = np.zeros((nkt, nqc), np.float32)
    for kt in range(nkt):
        for qc in range(nqc):
            if (kt * 128) // S1 != (qc * 512) // S1:
                mb_p[kt, qc] = -30000.0
    mb_p = np.ascontiguousarray(np.broadcast_to(mb_p.reshape(1, -1), (128, nkt * nqc)))
    wts = {k: np.ascontiguousarray(np.asarray(inputs[k], np.float32)) for k in _WNAMES}
    in_maps = []
    for c in range(n):
        if c < B2:
            m = {"x": np.ascontiguousarray(xs[c]), "cvec": np.ascontiguousarray(np.stack([cs[c], cs[c]], axis=0)),
                 "tabs": tab_s, "maskb": mb_s}
        else:
            i = min(c - B2, npair - 1)
            m = {"x": np.ascontiguousarray(np.concatenate([xp[2 * i], xp[2 * i + 1]], axis=0)),
                 "cvec": np.ascontiguousarray(np.stack([cp[2 * i], cp[2 * i + 1]], axis=0)),
                 "tabs": tab_p, "maskb": mb_p}
        m["cmats"] = cm
        m.update(wts)
        in_maps.append(m)
    res = run_bass_kernel_spmd(nc, in_maps, core_ids=list(range(n)))
    outs = [np.asarray(r["y"], np.float32) for r in res.results]
    y_sample = np.stack([outs[b] for b in range(B2)], axis=0)
    y_prompt = np.stack([outs[B2 + j // 2][(j % 2) * S1:(j % 2 + 1) * S1] for j in range(B1)], axis=0)
    return (y_prompt, y_sample)
```

```python
import math
from contextlib import ExitStack

import numpy as np
import concourse.bass as bass
import concourse.mybir as mybir
from concourse.bass_utils import run_bass_kernel_spmd

F32 = mybir.dt.float32
BF16 = mybir.dt.bfloat16
AF = mybir.ActivationFunctionType
ALU = mybir.AluOpType
AX = mybir.AxisListType

D = 1024
KC = 8
EPS = 1e-6
DIFF_THETA = 500000.0
AX_THETA = 10000.0
GRID_W = 64
NE = 8


class Cfg:
    def __init__(self, L, T, DFF, DFE):
        self.L, self.T, self.DFF, self.DFE = L, T, DFF, DFE
        self.HALF = T // 2


class Trk:
    __slots__ = ("w", "r")

    def __init__(self):
        self.w = []
        self.r = {}


class Sched:
    ENG = ("pe", "act", "dve", "pool", "sp")
    ND = 6

    _shared = None

    def __init__(self, nc, es, tag):
        self.nc = nc
        self.lists = {e: [] for e in self.ENG}
        self.pend = {e: [] for e in self.ENG}
        sh = Sched._shared
        if sh is None or sh["nc"] is not nc:
            gs = Sched._gs
            sh = {"nc": nc,
                  "sem": {e: gs.enter_context(nc.semaphore(f"s_{e}")) for e in self.ENG},
                  "cnt": {e: 0 for e in self.ENG},
                  "waited": {e: {} for e in self.ENG},
                  "dsem": {q: [gs.enter_context(nc.semaphore(f"d_{q}{i}")) for i in range(self.ND)] for q in ("sp", "pool")},
                  "dcnt": {q: [0] * self.ND for q in ("sp", "pool")},
                  "dlast": {q: [None] * self.ND for q in ("sp", "pool")},
                  "dnext": {q: 0 for q in ("sp", "pool")}}
            Sched._shared = sh
        self.sem, self.cnt, self.waited = sh["sem"], sh["cnt"], sh["waited"]
        self.dsem, self.dcnt, self.dlast, self.dnext = sh["dsem"], sh["dcnt"], sh["dlast"], sh["dnext"]

    def _wait(self, e, toks):
        for t in toks:
            if t is None:
                continue
            sem, val = t
            k = id(sem)
            if self.waited[e].get(k, 0) < val:
                self.waited[e][k] = val
                self.lists[e].append(lambda E, sem=sem, val=val: E.wait_ge(sem, val))

    @staticmethod
    def _deps(reads, writes):
        deps = []
        for b in reads:
            deps += b.w
        for b in writes:
            deps += b.w
            deps += list(b.r.values())
        return deps

    @staticmethod
    def _commit(tok, reads, writes):
        k = id(tok[0])
        for b in reads:
            b.r[k] = tok
        for b in writes:
            b.w = [tok]
            b.r = {}

    def op(self, e, fn, reads=(), writes=(), sig=True):
        self._wait(e, self._deps(reads, writes))
        self.pend[e].append((reads, writes))
        if not sig:
            self.lists[e].append(fn)
            return None
        self.cnt[e] += 1
        sem = self.sem[e]
        tok = (sem, self.cnt[e])
        self.lists[e].append(lambda E, fn=fn, sem=sem: fn(E).then_inc(sem, 1))
        for rs, ws in self.pend[e]:
            self._commit(tok, rs, ws)
        self.pend[e] = []
        return tok

    def dma(self, q, out, in_, reads=(), writes=()):
        i = self.dnext[q]
        self.dnext[q] = (i + 1) % self.ND
        self._wait(q, self._deps(reads, writes) + [self.dlast[q][i]])
        self.dcnt[q][i] += 16
        sem = self.dsem[q][i]
        tok = (sem, self.dcnt[q][i])
        self.dlast[q][i] = tok
        self.lists[q].append(lambda E, out=out, in_=in_, sem=sem: E.dma_start(out=out, in_=in_).then_inc(sem, 16))
        self._commit(tok, reads, writes)
        return tok

    def emit(self):
        allt = [t for q in ("sp", "pool") for t in self.dlast[q]]
        allt += [(self.sem[e], self.cnt[e]) for e in self.ENG if self.cnt[e] > 0 and e != "sp"]
        for e in self.ENG:
            self._wait(e, allt)
        with self.nc.Block() as blk:
            def run(key):
                def f(E):
                    for g in self.lists[key]:
                        g(E)
                return f
            blk.tensor(run("pe"))
            blk.scalar(run("act"))
            blk.vector(run("dve"))
            blk.gpsimd(run("pool"))
            blk.sync(run("sp"))


_UID = [0]


class Ring:
    def __init__(self, nc, es, name, shape, dtype, n, psum=False):
        mk = nc.psum_tensor if psum else nc.sbuf_tensor
        _UID[0] += 1
        self.t = [es.enter_context(mk(f"{name}_{_UID[0]}_{i}", shape, dtype)) for i in range(n)]
        self.k = [Trk() for _ in range(n)]
        self.i = 0

    def next(self):
        i = self.i
        self.i = (i + 1) % len(self.t)
        return self.t[i], self.k[i]


def one(nc, es, name, shape, dtype, psum=False):
    r = Ring(nc, es, name, shape, dtype, 1, psum)
    return r.t[0], r.k[0]


def build(cfg):
    nc = bass.Bass("TRN2", target_bir_lowering=False)
    L, T, DFF, DFE, HALF = cfg.L, cfg.T, cfg.DFF, cfg.DFE, cfg.HALF
    TK = T
    NCH = T // 512

    def din(name, shape):
        return nc.dram_tensor(name, list(shape), F32, kind="ExternalInput").ap()

    x_in = din("x", [T, D])
    cvec = din("cvec", [2, D])
    cmats = din("cmats", [128, 5, 128])
    tabs = din("tabs", [4, 128, T])
    maskb = din("maskb", [128, (T // 128) * (T // 512)])
    w_ada = din("w_ada", [L, D, 6 * D])
    b_ada = din("b_ada", [L, 6 * D])
    norm1 = din("norm1", [L, D])
    w_in = din("w_in", [L, D, 6656])
    diff_lambda = din("diff_lambda", [L, 4, 64])
    diff_subln = din("diff_subln", [L, 128])
    gqa_q_norm = din("gqa_q_norm", [L, 64])
    gqa_k_norm = din("gqa_k_norm", [L, 64])
    w_out = din("w_out", [L, D, D])
    norm2 = din("norm2", [L, D])
    ND_, NM_ = (L + 1) // 2, L // 2
    ffn_w_gate = din("ffn_w_gate", [ND_, D, DFF])
    ffn_w_up = din("ffn_w_up", [ND_, D, DFF])
    ffn_w_down = din("ffn_w_down", [ND_, DFF, D])
    router_w = din("router_w", [max(NM_, 1), D, NE])
    moe_w_gate = din("moe_w_gate", [max(NM_, 1), NE, D, DFE])
    moe_w_up = din("moe_w_up", [max(NM_, 1), NE, D, DFE])
    moe_w_down = din("moe_w_down", [max(NM_, 1), NE, DFE, D])
    final_norm = din("final_norm", [D])
    y_out = nc.dram_tensor("y", [T, D], F32, kind="ExternalOutput").ap()

    def scr(name, shape, dt):
        return nc.dram_tensor(name, list(shape), dt).ap()

    xres = scr("xres", [T, D], F32)
    hT = scr("hT", [128, KC, T], BF16)
    QT = scr("QT", [16, 128, T], BF16)
    KT = scr("KT", [12, 128, TK], BF16)
    Vs = scr("Vs", [TK, 1280], BF16)
    GT = scr("GT", [16, 128, T], BF16)
    mT = scr("mT", [128, KC, T], BF16)
    modg = scr("modg", [L, 2, 2, 128, D], F32)
    yacc = scr("yacc", [T, D], F32)

    def slot_of(tok0):
        return tok0 // HALF

    def key_off(tok0):
        return tok0

    with ExitStack() as gs:
        Sched._gs = gs
        Sched._shared = None
        cm = gs.enter_context(nc.sbuf_tensor("cm", [128, 5, 128], BF16))
        IDENT, PD, PG, BONES, ONES = (cm[:, i, :] for i in range(5))
        modf = gs.enter_context(nc.sbuf_tensor("modf", [128, L, 4, KC, 2], F32))
        fnorm = gs.enter_context(nc.sbuf_tensor("fnorm", [128, KC], F32))
        wsub = gs.enter_context(nc.sbuf_tensor("wsub", [128, L], F32))
        nlam = gs.enter_context(nc.sbuf_tensor("nlam", [128, L], F32))
        gq8 = gs.enter_context(nc.sbuf_tensor("gq8", [128, L], F32))
        gk8 = gs.enter_context(nc.sbuf_tensor("gk8", [128, L], F32))

        with ExitStack() as es, nc.allow_non_contiguous_dma("tiny feature-major parameter loads"):
            S = Sched(nc, es, "p0")
            E = es.enter_context
            kcm, kmodf, kmisc = Trk(), Trk(), Trk()
            S.dma("pool", cm[:], cmats, writes=[kcm])
            cT32, kcT32 = one(nc, es, "cT32", [128, 2, KC], F32)
            cTb, kcTb = one(nc, es, "cTb", [128, 2, KC], BF16)
            crep, kcrep = one(nc, es, "crep", [128, KC, 2, 128], BF16)
            n1, kn1 = one(nc, es, "n1", [128, L, KC], F32)
            n2, kn2 = one(nc, es, "n2", [128, L, KC], F32)
            bfm, kbfm = one(nc, es, "bfm", [128, L, 48], F32)
            sub, ksub = one(nc, es, "sub", [128, L], F32)
            gqn, kgqn = one(nc, es, "gqn", [128, L], F32)
            gkn, kgkn = one(nc, es, "gkn", [128, L], F32)
            dl, kdl = one(nc, es, "dl", [128, L, 4, 64], F32)
            for s_ in range(2):
                S.dma("sp", cT32[:, s_, :], cvec[s_].rearrange("(c p) -> p c", p=128), writes=[kcT32])
            S.dma("sp", n1[:], norm1.rearrange("l (c p) -> p l c", p=128), writes=[kn1])
            S.dma("sp", n2[:], norm2.rearrange("l (c p) -> p l c", p=128), writes=[kn2])
            S.dma("sp", fnorm[:], final_norm.rearrange("(c p) -> p c", p=128), writes=[kmisc])
            S.dma("sp", bfm[:], b_ada.rearrange("l (c p) -> p l c", p=128), writes=[kbfm])
            S.dma("sp", sub[:], diff_subln.rearrange("l p -> p l"), writes=[ksub])
            for hh in range(2):
                S.dma("sp", gqn[hh * 64:(hh + 1) * 64, :], gqa_q_norm.rearrange("l p -> p l"), writes=[kgqn])
                S.dma("sp", gkn[hh * 64:(hh + 1) * 64, :], gqa_k_norm.rearrange("l p -> p l"), writes=[kgkn])
            S.dma("sp", dl[:].rearrange("p l a d -> p (l a d)"),
                  diff_lambda.rearrange("l a d -> (l a d)").partition_broadcast(128), writes=[kdl])
            S.op("act", lambda A: A.activation(out=cTb[:], in_=cT32[:], func=AF.Silu), reads=[kcT32], writes=[kcTb])
            for s in range(2):
                S.op("dve", lambda V, s=s: V.tensor_copy(crep[:, :, s, :], cTb[:, s, :].unsqueeze(2).to_broadcast([128, KC, 128])),
                     reads=[kcTb], writes=[kcrep])
            tmp, ktmp = one(nc, es, "p0tmp", [128, 64], F32)
            sc4, ksc4 = one(nc, es, "sc4", [128, 4], F32)
            for l in range(L):
                lam_init = 0.8 - 0.6 * math.exp(-0.3 * l)
                for m in range(2):
                    S.op("dve", lambda V, l=l, m=m: V.tensor_tensor(out=tmp[:], in0=dl[:, l, 2 * m, :], in1=dl[:, l, 2 * m + 1, :], op=ALU.mult),
                         reads=[kdl], writes=[ktmp])
                    S.op("dve", lambda V, m=m: V.tensor_reduce(out=sc4[:, m:m + 1], in_=tmp[:], axis=AX.X, op=ALU.add),
                         reads=[ktmp], writes=[ksc4])
                S.op("act", lambda A: A.activation(out=sc4[:, 2:4], in_=sc4[:, 0:2], func=AF.Exp), reads=[ksc4], writes=[ksc4])
                S.op("dve", lambda V, l=l, li=lam_init: V.scalar_tensor_tensor(out=nlam[:, l:l + 1], in0=sc4[:, 3:4], scalar=-li, in1=sc4[:, 2:3],
                                                                            op0=ALU.add, op1=ALU.subtract),
                     reads=[ksc4], writes=[kmisc])
                S.op("dve", lambda V, l=l, li=lam_init: V.tensor_scalar(out=wsub[:, l:l + 1], in0=sub[:, l:l + 1], scalar1=(1.0 - li) * math.sqrt(128.0),
                                                                         scalar2=None, op0=ALU.mult),
                     reads=[ksub], writes=[kmisc])
                S.op("dve", lambda V, l=l: V.tensor_scalar(out=gq8[:, l:l + 1], in0=gqn[:, l:l + 1], scalar1=8.0, scalar2=None, op0=ALU.mult),
                     reads=[kgqn], writes=[kmisc])
                S.op("dve", lambda V, l=l: V.tensor_scalar(out=gk8[:, l:l + 1], in0=gkn[:, l:l + 1], scalar1=8.0, scalar2=None, op0=ALU.mult),
                     reads=[kgkn], writes=[kmisc])
            wr = Ring(nc, es, "wada", [128, KC, 512], BF16, 2)
            br = Ring(nc, es, "bbc", [128, 512], F32, 2)
            gr = Ring(nc, es, "gout", [128, 512], F32, 2)
            pr = Ring(nc, es, "p0ps", [128, 512], F32, 2, psum=True)
            V4 = {0: 0, 1: 1, 3: 2, 4: 3}
            for l in range(L):
                for j in range(12):
                    vec, half = j // 2, j % 2
                    wt, kw = wr.next()
                    S.dma("pool", wt[:], w_ada[l, :, j * 512:(j + 1) * 512].rearrange("(kc p) n -> p kc n", p=128), writes=[kw])
                    if vec in (2, 5):
                        which = 0 if vec == 2 else 1
                        bb, kb = br.next()
                        S.dma("sp", bb[:], b_ada[l, j * 512:(j + 1) * 512].partition_broadcast(128), writes=[kb])
                        for s in range(2):
                            ps, kp = pr.next()
                            for kc in range(KC):
                                S.op("pe", lambda P, ps=ps, wt=wt, s=s, kc=kc: P.matmul(ps[:], crep[:, kc, s, :], wt[:, kc, :], start=(kc == 0), stop=(kc == KC - 1)),
                                     reads=[kcrep, kw], writes=[kp], sig=(kc == KC - 1))
                            go, kg = gr.next()
                            S.op("dve", lambda V, go=go, ps=ps, bb=bb: V.tensor_tensor(out=go[:], in0=ps[:], in1=bb[:], op=ALU.add),
                                 reads=[kp, kb], writes=[kg])
                            S.dma("sp", modg[l, which, s, :, half * 512:(half + 1) * 512], go[:], reads=[kg])
                    else:
                        v4 = V4[vec]
                        for cb in range(4):
                            c = half * 4 + cb
                            ps, kp = pr.next()
                            for kc in range(KC):
                                S.op("pe", lambda P, ps=ps, wt=wt, cb=cb, kc=kc: P.matmul(ps[:, 0:2], wt[:, kc, cb * 128:(cb + 1) * 128], cTb[:, :, kc], start=(kc == 0), stop=(kc == KC - 1)),
                                     reads=[kcTb, kw], writes=[kp], sig=(kc == KC - 1))
                            S.op("dve", lambda V, ps=ps, l=l, v4=v4, c=c, vec=vec: V.tensor_scalar(out=modf[:, l, v4, c, :], in0=ps[:, 0:2], scalar1=bfm[:, l, vec * 8 + c:vec * 8 + c + 1],
                                                                                                   scalar2=None, op0=ALU.add),
                                 reads=[kp, kbfm], writes=[kmodf])
                for v4, nn, kn in ((1, n1, kn1), (3, n2, kn2)):
                    for s in range(2):
                        S.op("dve", lambda V, l=l, v4=v4, s=s, nn=nn: V.scalar_tensor_tensor(out=modf[:, l, v4, :, s], in0=modf[:, l, v4, :, s], scalar=1.0, in1=nn[:, l, :],
                                                                                           op0=ALU.add, op1=ALU.mult),
                             reads=[kmodf, kn], writes=[kmodf])
            S.emit()

        def norm_phase(tag, src, l, v_sh, v_s):
            with ExitStack() as es:
                S = Sched(nc, es, tag)
                xr = Ring(nc, es, "nx", [128, D], F32, 3)
                jr = Ring(nc, es, "njunk", [128, D], BF16, 1)
                sr = Ring(nc, es, "nss", [128, 4], F32, 4)
                xnr = Ring(nc, es, "nxn", [128, D], BF16, 3)
                tr = Ring(nc, es, "nT", [128, KC, 512], BF16, 2, psum=True)
                hr = Ring(nc, es, "nh", [128, KC, 512], BF16, 2)
                for ci in range(NCH):
                    s = slot_of(ci * 512)
                    pT, kT = tr.next()
                    for tt in range(4):
                        r0 = ci * 512 + tt * 128
                        xt, kx = xr.next()
                        S.dma("sp", xt[:], src[r0:r0 + 128, :], writes=[kx])
                        jt, kj = jr.next()
                        st, ks = sr.next()
                        S.op("dve", lambda V, st=st: V.memset(st[:, 0:1], 0.0), writes=[ks])
                        S.op("act", lambda A, jt=jt, xt=xt, st=st: A.activation(out=jt[:], in_=xt[:], func=AF.Square, accum_out=st[:, 0:1]),
                             reads=[kx], writes=[kj, ks])
                        S.op("act", lambda A, st=st: A.activation(out=st[:, 1:2], in_=st[:, 0:1], func=AF.Sqrt, scale=1.0 / D, bias=EPS),
                             reads=[ks], writes=[ks])
                        S.op("dve", lambda V, st=st: V.reciprocal(st[:, 2:3], st[:, 1:2]), reads=[ks], writes=[ks])
                        xn, kxn = xnr.next()
                        S.op("pool", lambda G, xn=xn, xt=xt, st=st: G.tensor_scalar(out=xn[:], in0=xt[:], scalar1=st[:, 2:3], scalar2=None, op0=ALU.mult),
                             reads=[kx, ks], writes=[kxn])
                        for c in range(KC):
                            S.op("pe", lambda P, pT=pT, xn=xn, c=c, tt=tt: P.transpose(pT[:, c, tt * 128:(tt + 1) * 128], xn[:, c * 128:(c + 1) * 128], IDENT),
                                 reads=[kxn], writes=[kT], sig=(c == KC - 1))
                    ht, kh = hr.next()
                    for c in range(KC):
                        if c % 2 == 0:
                            S.op("dve", lambda V, ht=ht, pT=pT, c=c, s=s: V.tensor_scalar(out=ht[:, c, :], in0=pT[:, c, :], scalar1=modf[:, l, v_s, c, s:s + 1],
                                                                                         scalar2=modf[:, l, v_sh, c, s:s + 1], op0=ALU.mult, op1=ALU.add),
                                 reads=[kT], writes=[kh])
                        else:
                            S.op("act", lambda A, ht=ht, pT=pT, c=c, s=s: A.activation(out=ht[:, c, :], in_=pT[:, c, :], func=AF.Identity,
                                                                                      scale=modf[:, l, v_s, c, s:s + 1], bias=modf[:, l, v_sh, c, s:s + 1]),
                                 reads=[kT], writes=[kh])
                    S.dma("sp", hT[:, :, ci * 512:(ci + 1) * 512], ht[:], reads=[kh])
                S.emit()

        def proj_phase(tag, l):
            with ExitStack() as es:
                S = Sched(nc, es, tag + "a")
                wq, kwq = one(nc, es, "wq", [128, KC, 3584], BF16)
                wsrc = w_in[l].rearrange("(kc p) n -> p kc n", p=128)
                S.dma("pool", wq[:, :, 0:2048], wsrc[:, :, 0:2048], writes=[kwq])
                S.dma("pool", wq[:, :, 2048:3072], wsrc[:, :, 3072:4096], writes=[kwq])
                for g in range(4):
                    for hh in range(2):
                        c0 = 3072 + g * 128 + hh * 64
                        S.dma("pool", wq[:, :, c0:c0 + 64], wsrc[:, :, 4096 + g * 64:4096 + (g + 1) * 64], writes=[kwq])
                hr = Ring(nc, es, "ph", [128, KC, 512], BF16, 4)
                tbr = Ring(nc, es, "ptab", [128, 4, 512], F32, 4)
                pr = Ring(nc, es, "pps", [128, 512], F32, 4, psum=True)
                p2 = Ring(nc, es, "pps2", [128, 512], F32, 3, psum=True)
                qsr = Ring(nc, es, "pqs", [128, 512], BF16, 8)
                sqr = Ring(nc, es, "psq", [128, 512], BF16, 3)
                rrr = Ring(nc, es, "prr", [128, 512], F32, 3)
                t1r = Ring(nc, es, "pt1", [128, 512], F32, 4)
                t2r = Ring(nc, es, "pt2", [128, 512], F32, 4)
                outr = Ring(nc, es, "pout", [128, 512], BF16, 4)
                def blk_gen(blk, ht, kh, tb, ktb, t0, k0):
                    ps, kp = pr.next()
                    for kc in range(KC):
                        S.op("pe", lambda P, ps=ps, ht=ht, blk=blk, kc=kc: P.matmul(ps[:], wq[:, kc, blk * 128:(blk + 1) * 128], ht[:, kc, :], start=(kc == 0), stop=(kc == KC - 1)),
                             reads=[kwq, kh], writes=[kp], sig=(kc == KC - 1))
                    yield
                    qs, kq = qsr.next()
                    if blk < 16:
                        S.op("act", lambda A, qs=qs, ps=ps: A.copy(qs[:], ps[:]), reads=[kp], writes=[kq])
                        PM, ti = PD, 0
                        dst = QT[blk, :, t0:t0 + 512] if blk < 8 else KT[blk - 8, :, k0:k0 + 512]
                        yield
                    else:
                        sq, ksq = sqr.next()
                        S.op("act", lambda A, sq=sq, ps=ps: A.activation(out=sq[:], in_=ps[:], func=AF.Square), reads=[kp], writes=[ksq])
                        yield
                        pss, kps = p2.next()
                        S.op("pe", lambda P, pss=pss, sq=sq: P.matmul(pss[:], BONES, sq[:], start=True, stop=True), reads=[ksq], writes=[kps])
                        yield
                        rr, krr = rrr.next()
                        S.op("act", lambda A, rr=rr, pss=pss: A.activation(out=rr[:], in_=pss[:], func=AF.Sqrt, scale=1.0, bias=64.0 * EPS),
                             reads=[kps], writes=[krr])
                        S.op("dve", lambda V, rr=rr: V.reciprocal(rr[:], rr[:]), reads=[krr], writes=[krr])
                        gv = gq8 if blk < 24 else gk8
                        S.op("dve", lambda V, qs=qs, ps=ps, rr=rr, gv=gv: V.scalar_tensor_tensor(out=qs[:], in0=ps[:], scalar=gv[:, l:l + 1], in1=rr[:], op0=ALU.mult, op1=ALU.mult),
                             reads=[kp, krr], writes=[kq])
                        PM, ti = PG, 2
                        dst = QT[8 + blk - 16, :, t0:t0 + 512] if blk < 24 else KT[8 + blk - 24, :, k0:k0 + 512]
                        yield
                    pp, kpp = p2.next()
                    S.op("pe", lambda P, pp=pp, qs=qs, PM=PM: P.matmul(pp[:], PM, qs[:], start=True, stop=True), reads=[kq], writes=[kpp])
                    yield
                    t1, kt1 = t1r.next()
                    S.op("pool", lambda G, t1=t1, qs=qs, tb=tb, ti=ti: G.tensor_tensor(out=t1[:], in0=qs[:], in1=tb[:, ti, :], op=ALU.mult),
                         reads=[kq, ktb], writes=[kt1])
                    t2, kt2 = t2r.next()
                    S.op("dve", lambda V, t2=t2, pp=pp, tb=tb, ti=ti: V.tensor_tensor(out=t2[:], in0=pp[:], in1=tb[:, ti + 1, :], op=ALU.mult),
                         reads=[kpp, ktb], writes=[kt2])
                    yield
                    ot, ko = outr.next()
                    S.op("pool", lambda G, ot=ot, t1=t1, t2=t2: G.tensor_tensor(out=ot[:], in0=t1[:], in1=t2[:], op=ALU.add),
                         reads=[kt1, kt2], writes=[ko])
                    S.dma("sp", dst, ot[:], reads=[ko])

                active = []

                def step_all():
                    for g_ in list(active):
                        try:
                            next(g_)
                        except StopIteration:
                            active.remove(g_)

                def load_a(ci):
                    t0 = ci * 512
                    ht, kh = hr.next()
                    S.dma("sp", ht[:], hT[:, :, t0:t0 + 512], writes=[kh])
                    tb, ktb = tbr.next()
                    S.dma("sp", tb[:], tabs[:, :, t0:t0 + 512].rearrange("a p t -> p a t"), writes=[ktb])
                    return ht, kh, tb, ktb

                nxt_a = load_a(0)
                for ci in range(NCH):
                    t0 = ci * 512
                    k0 = key_off(t0)
                    ht, kh, tb, ktb = nxt_a
                    if ci + 1 < NCH:
                        nxt_a = load_a(ci + 1)
                    for blk in range(28):
                        g_ = blk_gen(blk, ht, kh, tb, ktb, t0, k0)
                        next(g_)
                        step_all()
                        active.append(g_)
                while active:
                    step_all()
                S.emit()
            with ExitStack() as es:
                S = Sched(nc, es, tag + "b")
                wg, kwg = one(nc, es, "wg", [128, KC, 3328], BF16)
                wsrc = w_in[l].rearrange("(kc p) n -> p kc n", p=128)
                S.dma("pool", wg[:, :, 0:2048], wsrc[:, :, 4608:6656], writes=[kwg])
                S.dma("pool", wg[:, :, 2048:3072], wsrc[:, :, 2048:3072], writes=[kwg])
                S.dma("pool", wg[:, :, 3072:3328], wsrc[:, :, 4352:4608], writes=[kwg])
                hr = Ring(nc, es, "qh", [128, KC, 512], BF16, 3)
                pr = Ring(nc, es, "qps", [128, 512], F32, 6, psum=True)
                outr = Ring(nc, es, "qout", [128, 512], BF16, 4)
                vr = Ring(nc, es, "qv", [128, 1280], BF16, 3)
                def load_b(ci):
                    ht, kh = hr.next()
                    S.dma("sp", ht[:], hT[:, :, ci * 512:(ci + 1) * 512], writes=[kh])
                    return ht, kh

                nxt_b = load_b(0)
                for ci in range(NCH):
                    t0 = ci * 512
                    k0 = key_off(t0)
                    ht, kh = nxt_b
                    if ci + 1 < NCH:
                        nxt_b = load_b(ci + 1)
                    for blk in range(16):
                        ps, kp = pr.next()
                        for kc in range(KC):
                            S.op("pe", lambda P, ps=ps, ht=ht, blk=blk, kc=kc: P.matmul(ps[:], wg[:, kc, blk * 128:(blk + 1) * 128], ht[:, kc, :], start=(kc == 0), stop=(kc == KC - 1)),
                                 reads=[kwg, kh], writes=[kp], sig=(kc == KC - 1))
                        ot, ko = outr.next()
                        S.op("act", lambda A, ot=ot, ps=ps: A.activation(out=ot[:], in_=ps[:], func=AF.Sigmoid), reads=[kp], writes=[ko])
                        S.dma("sp", GT[blk, :, t0:t0 + 512], ot[:], reads=[ko])
                    for tt in range(4):
                        vt, kv = vr.next()
                        for n0, nn in ((0, 512), (512, 512), (1024, 256)):
                            ps, kp = pr.next()
                            for kc in range(KC):
                                S.op("pe", lambda P, ps=ps, ht=ht, tt=tt, kc=kc, n0=n0, nn=nn: P.matmul(ps[:, 0:nn], ht[:, kc, tt * 128:(tt + 1) * 128], wg[:, kc, 2048 + n0:2048 + n0 + nn],
                                                                                                        start=(kc == 0), stop=(kc == KC - 1)),
                                     reads=[kwg, kh], writes=[kp], sig=(kc == KC - 1))
                            eng = "dve" if n0 == 512 else "act"
                            if eng == "dve":
                                S.op("dve", lambda V, vt=vt, ps=ps, n0=n0, nn=nn: V.tensor_copy(vt[:, n0:n0 + nn], ps[:, 0:nn]), reads=[kp], writes=[kv])
                            else:
                                S.op("act", lambda A, vt=vt, ps=ps, n0=n0, nn=nn: A.copy(vt[:, n0:n0 + nn], ps[:, 0:nn]), reads=[kp], writes=[kv])
                        S.dma("sp", Vs[k0 + tt * 128:k0 + (tt + 1) * 128, :], vt[:], reads=[kv])
                S.emit()

        def attn_phase(tag, l):
            with ExitStack() as es:
                S = Sched(nc, es, tag)
                SKM = T
                NQC = T // 512
                mb, kmb = one(nc, es, "amb", [128, (T // 128) * NQC], F32)
                S.dma("sp", mb[:], maskb, writes=[kmb])
                ktd = Ring(nc, es, "aktd", [128, SKM], BF16, 2)
                ktg = Ring(nc, es, "aktg", [128, SKM], BF16, 2)
                vdr = Ring(nc, es, "avd", [128, SKM // 128, 128], BF16, 2)
                vgr = Ring(nc, es, "avg", [128, SKM // 128, 64], BF16, 2)
                qdr = Ring(nc, es, "aqd", [128, 512], BF16, 3)
                qgr = Ring(nc, es, "aqg", [128, 512], BF16, 3)
                gar = Ring(nc, es, "aga", [128, 512], BF16, 3)
                gbr = Ring(nc, es, "agb", [128, 512], BF16, 3)
                scr_ = Ring(nc, es, "asc", [128, 2, 512], F32, 2, psum=True)
                acc = Ring(nc, es, "aacc", [128, 512], F32, 4, psum=True)
                er = Ring(nc, es, "ae", [128, 2, 512], BF16, 3)
                fr = Ring(nc, es, "af", [128, 512], F32, 8)
                sqr = Ring(nc, es, "asq", [128, 512], BF16, 2)
                mor = Ring(nc, es, "amo", [128, 512], BF16, 2)
                for s in range(1):
                    SK = T
                    kb = 0
                    q_lo, q_hi = 0, T
                    nkt = SK // 128
                    kg_t = vg_t = kkg = kvg = None

                    def load_kv(j):
                        kd, kkd = ktd.next()
                        S.dma("sp", kd[:, 0:SK], KT[j, :, kb:kb + SK], writes=[kkd])
                        vd, kvd = vdr.next()
                        S.dma("sp", vd[:, 0:nkt, :], Vs[kb:kb + SK, j * 128:(j + 1) * 128].rearrange("(kt p) e -> p kt e", p=128), writes=[kvd])
                        gg = None
                        if j % 2 == 0:
                            g = j // 2
                            kg_, kkg_ = ktg.next()
                            S.dma("sp", kg_[:, 0:SK], KT[8 + g, :, kb:kb + SK], writes=[kkg_])
                            vg_, kvg_ = vgr.next()
                            S.dma("sp", vg_[:, 0:nkt, :], Vs[kb:kb + SK, 1024 + g * 64:1024 + (g + 1) * 64].rearrange("(kt p) e -> p kt e", p=128), writes=[kvg_])
                            gg = (kg_, kkg_, vg_, kvg_)
                        return kd, kkd, vd, kvd, gg

                    nxt_kv = load_kv(0)
                    for j in range(8):
                        kd, kkd, vd, kvd, gg = nxt_kv
                        if gg is not None:
                            kg_t, kkg, vg_t, kvg = gg
                        if j + 1 < 8:
                            nxt_kv = load_kv(j + 1)
                        def load_q(t0, j=j):
                            qd, kqd = qdr.next()
                            S.dma("sp", qd[:], QT[j, :, t0:t0 + 512], writes=[kqd])
                            qg, kqg = qgr.next()
                            S.dma("sp", qg[:], QT[8 + j, :, t0:t0 + 512], writes=[kqg])
                            ga, kga = gar.next()
                            S.dma("sp", ga[:], GT[j, :, t0:t0 + 512], writes=[kga])
                            gb, kgb = gbr.next()
                            S.dma("sp", gb[:], GT[8 + j, :, t0:t0 + 512], writes=[kgb])
                            return qd, kqd, qg, kqg, ga, kga, gb, kgb

                        nxt_q = load_q(q_lo)
                        for t0 in range(q_lo, q_hi, 512):
                            qc = t0 // 512
                            qd, kqd, qg, kqg, ga, kga, gb, kgb = nxt_q
                            if t0 + 512 < q_hi:
                                nxt_q = load_q(t0 + 512)
                            ov1, ko1 = acc.next()
                            ov2, ko2 = acc.next()
                            rb1, kr1 = acc.next()
                            rb2, kr2 = acc.next()
                            def qk_d(kt, kd=kd, qd=qd, kkd=kkd, kqd=kqd):
                                sc, ksc = scr_.next()
                                for m in range(2):
                                    S.op("pe", lambda P, sc=sc, kt=kt, m=m: P.matmul(sc[:, m, :], kd[m * 64:(m + 1) * 64, kt * 128:(kt + 1) * 128], qd[m * 64:(m + 1) * 64, :],
                                                                                    start=True, stop=True),
                                         reads=[kkd, kqd], writes=[ksc], sig=(m == 1))
                                return sc, ksc
                            nxt = qk_d(0)
                            for kt in range(nkt):
                                sc, ksc = nxt
                                if kt + 1 < nkt:
                                    nxt = qk_d(kt + 1)
                                et, ke = er.next()
                                S.op("act", lambda A, et=et, sc=sc, kt=kt, qc=qc: A.activation(out=et[:], in_=sc[:], func=AF.Exp, scale=0.125, bias=mb[:, kt * NQC + qc:kt * NQC + qc + 1]), reads=[ksc, kmb], writes=[ke])
                                st, sp_ = (kt == 0), (kt == nkt - 1)
                                S.op("pe", lambda P, ov1=ov1, vd=vd, et=et, kt=kt, st=st, sp_=sp_: P.matmul(ov1[:], vd[:, kt, :], et[:, 0, :], start=st, stop=sp_),
                                     reads=[kvd, ke], writes=[ko1], sig=False)
                                S.op("pe", lambda P, ov2=ov2, vd=vd, et=et, kt=kt, st=st, sp_=sp_: P.matmul(ov2[:], vd[:, kt, :], et[:, 1, :], start=st, stop=sp_),
                                     reads=[kvd, ke], writes=[ko2], sig=False)
                                S.op("pe", lambda P, rb1=rb1, et=et, st=st, sp_=sp_: P.matmul(rb1[:], ONES, et[:, 0, :], start=st, stop=sp_),
                                     reads=[ke], writes=[kr1], sig=False)
                                S.op("pe", lambda P, rb2=rb2, et=et, st=st, sp_=sp_: P.matmul(rb2[:], ONES, et[:, 1, :], start=st, stop=sp_),
                                     reads=[ke], writes=[kr2], sig=True)
                            r1, k1 = fr.next()
                            S.op("dve", lambda V, r1=r1, rb1=rb1: V.reciprocal(r1[:], rb1[:]), reads=[kr1], writes=[k1])
                            r2, k2 = fr.next()
                            S.op("dve", lambda V, r2=r2, rb2=rb2: V.reciprocal(r2[:], rb2[:]), reads=[kr2], writes=[k2])
                            a1, ka1 = fr.next()
                            S.op("dve", lambda V, a1=a1, ov1=ov1, r1=r1: V.tensor_tensor(out=a1[:], in0=ov1[:], in1=r1[:], op=ALU.mult), reads=[ko1, k1], writes=[ka1])
                            a2, ka2 = fr.next()
                            S.op("dve", lambda V, a2=a2, ov2=ov2, r2=r2: V.tensor_tensor(out=a2[:], in0=ov2[:], in1=r2[:], op=ALU.mult), reads=[ko2, k2], writes=[ka2])
                            S.op("dve", lambda G, a1=a1, a2=a2: G.scalar_tensor_tensor(out=a1[:], in0=a2[:], scalar=nlam[:, l:l + 1], in1=a1[:], op0=ALU.mult, op1=ALU.add),
                                 reads=[ka2, ka1], writes=[ka1])
                            sq, ksq = sqr.next()
                            S.op("pool", lambda G, sq=sq, a1=a1: G.tensor_tensor(out=sq[:], in0=a1[:], in1=a1[:], op=ALU.mult), reads=[ka1], writes=[ksq])
                            ssq, kss = acc.next()
                            S.op("pe", lambda P, ssq=ssq, sq=sq: P.matmul(ssq[:], ONES, sq[:], start=True, stop=True), reads=[ksq], writes=[kss])
                            rn, krn = fr.next()
                            S.op("act", lambda A, rn=rn, ssq=ssq: A.activation(out=rn[:], in_=ssq[:], func=AF.Sqrt, scale=1.0, bias=128.0 * EPS), reads=[kss], writes=[krn])
                            S.op("dve", lambda V, rn=rn: V.reciprocal(rn[:], rn[:]), reads=[krn], writes=[krn])
                            S.op("dve", lambda V, a1=a1, rn=rn: V.scalar_tensor_tensor(out=a1[:], in0=a1[:], scalar=wsub[:, l:l + 1], in1=rn[:], op0=ALU.mult, op1=ALU.mult),
                                 reads=[ka1, krn], writes=[ka1])
                            S.op("pool", lambda G, a1=a1, ga=ga: G.tensor_tensor(out=a1[:], in0=a1[:], in1=ga[:], op=ALU.mult), reads=[ka1, kga], writes=[ka1])
                            ovg, kog = acc.next()
                            rbg, krg = acc.next()
                            def qk_g(kt, kg_t=kg_t, qg=qg, kkg=kkg, kqg=kqg):
                                sc, ksc = scr_.next()
                                for m in range(2):
                                    S.op("pe", lambda P, sc=sc, kt=kt, m=m: P.matmul(sc[:, m, :], kg_t[m * 64:(m + 1) * 64, kt * 128:(kt + 1) * 128], qg[m * 64:(m + 1) * 64, :],
                                                                                    start=True, stop=True),
                                         reads=[kkg, kqg], writes=[ksc], sig=(m == 1))
                                return sc, ksc
                            nxt = qk_g(0)
                            for kt in range(nkt):
                                sc, ksc = nxt
                                if kt + 1 < nkt:
                                    nxt = qk_g(kt + 1)
                                et, ke = er.next()
                                S.op("act", lambda A, et=et, sc=sc, kt=kt, qc=qc: A.activation(out=et[:], in_=sc[:], func=AF.Exp, scale=0.125, bias=mb[:, kt * NQC + qc:kt * NQC + qc + 1]), reads=[ksc, kmb], writes=[ke])
                                st, sp_ = (kt == 0), (kt == nkt - 1)
                                for m in range(2):
                                    S.op("pe", lambda P, ovg=ovg, vg_t=vg_t, et=et, kt=kt, m=m, st=st, sp_=sp_: P.matmul(ovg[m * 64:(m + 1) * 64, :], vg_t[:, kt, :], et[:, m, :], start=st, stop=sp_),
                                         reads=[kvg, ke], writes=[kog], sig=False)
                                for m in range(2):
                                    S.op("pe", lambda P, rbg=rbg, et=et, m=m, st=st, sp_=sp_: P.matmul(rbg[m * 64:(m + 1) * 64, :], cm[:, 4, 0:64], et[:, m, :], start=st, stop=sp_),
                                         reads=[ke], writes=[krg], sig=(m == 1))
                            rg, krg2 = fr.next()
                            S.op("dve", lambda V, rg=rg, rbg=rbg: V.reciprocal(rg[:], rbg[:]), reads=[krg], writes=[krg2])
                            bt, kbt = fr.next()
                            S.op("dve", lambda V, bt=bt, ovg=ovg, rg=rg: V.tensor_tensor(out=bt[:], in0=ovg[:], in1=rg[:], op=ALU.mult), reads=[kog, krg2], writes=[kbt])
                            S.op("pool", lambda G, bt=bt, gb=gb: G.tensor_tensor(out=bt[:], in0=bt[:], in1=gb[:], op=ALU.mult), reads=[kbt, kgb], writes=[kbt])
                            mo, kmo = mor.next()
                            S.op("pool", lambda G, mo=mo, bt=bt, a1=a1: G.tensor_tensor(out=mo[:], in0=bt[:], in1=a1[:], op=ALU.add), reads=[kbt, ka1], writes=[kmo])
                            S.dma("sp", mT[:, j, t0:t0 + 512], mo[:], reads=[kmo])
                S.emit()

        def outproj_phase(tag, l, src):
            with ExitStack() as es:
                S = Sched(nc, es, tag)
                wo, kwo = one(nc, es, "wo", [128, KC, D], BF16)
                S.dma("pool", wo[:], w_out[l].rearrange("(kc p) n -> p kc n", p=128), writes=[kwo])
                g1 = [one(nc, es, f"g1_{s}", [128, D], F32) for s in range(2)]
                for s in range(2):
                    S.dma("sp", g1[s][0][:], modg[l, 0, s], writes=[g1[s][1]])
                mr = Ring(nc, es, "om", [128, KC, 512], BF16, 3)
                xr = Ring(nc, es, "ox", [128, D], F32, 9)
                pr = Ring(nc, es, "ops", [128, 2, 512], F32, 2, psum=True)
                yr = Ring(nc, es, "oy", [128, D], F32, 3)
                def load_o(ci):
                    t0 = ci * 512
                    mt, kmt = mr.next()
                    S.dma("sp", mt[:], mT[:, :, t0:t0 + 512], writes=[kmt])
                    xs_ = []
                    for tt in range(4):
                        xt, kx = xr.next()
                        S.dma("sp", xt[:], src[t0 + tt * 128:t0 + (tt + 1) * 128, :], writes=[kx])
                        xs_.append((xt, kx))
                    return mt, kmt, xs_

                nxt_o = load_o(0)
                for ci in range(NCH):
                    t0 = ci * 512
                    s = slot_of(t0)
                    mt, kmt, xs_ = nxt_o
                    if ci + 1 < NCH:
                        nxt_o = load_o(ci + 1)
                    for tt in range(4):
                        r0 = t0 + tt * 128
                        xt, kx = xs_[tt]
                        ps, kp = pr.next()
                        for h in range(2):
                            for kc in range(KC):
                                S.op("pe", lambda P, ps=ps, mt=mt, tt=tt, kc=kc, h=h: P.matmul(ps[:, h, :], mt[:, kc, tt * 128:(tt + 1) * 128], wo[:, kc, h * 512:(h + 1) * 512],
                                                                                              start=(kc == 0), stop=(kc == KC - 1)),
                                     reads=[kmt, kwo], writes=[kp], sig=(h == 1 and kc == KC - 1))
                        yt, ky = yr.next()
                        S.op("dve", lambda V, yt=yt, ps=ps, s=s: V.tensor_tensor(out=yt[:].rearrange("p (a b) -> p a b", a=2), in0=ps[:], in1=g1[s][0][:].rearrange("p (a b) -> p a b", a=2), op=ALU.mult),
                             reads=[kp, g1[s][1]], writes=[ky])
                        S.op("pool", lambda G, yt=yt, xt=xt: G.tensor_tensor(out=yt[:], in0=yt[:], in1=xt[:], op=ALU.add), reads=[ky, kx], writes=[ky])
                        S.dma("sp", xres[r0:r0 + 128, :], yt[:], reads=[ky])
                S.emit()

        def ffn_pass(tag, l, wg_src, wu_src, wd_src, f0, nf, mode, e_idx, gates_t, first, last):
            with ExitStack() as es:
                S = Sched(nc, es, tag)
                wg, kwg = one(nc, es, "fwg", [128, KC, nf * 128], BF16)
                wu, kwu = one(nc, es, "fwu", [128, KC, nf * 128], BF16)
                wd, kwd = one(nc, es, "fwd", [128, nf, D], BF16)
                S.dma("pool", wg[:], wg_src[:, f0:f0 + nf * 128].rearrange("(kc p) n -> p kc n", p=128), writes=[kwg])
                S.dma("pool", wu[:], wu_src[:, f0:f0 + nf * 128].rearrange("(kc p) n -> p kc n", p=128), writes=[kwu])
                S.dma("pool", wd[:], wd_src[f0:f0 + nf * 128, :].rearrange("(f p) n -> p f n", p=128), writes=[kwd])
                g2 = [one(nc, es, f"g2_{s}", [128, D], F32) for s in range(2)]
                if mode == "dense" or last:
                    for s in range(2):
                        S.dma("sp", g2[s][0][:], modg[l, 1, s], writes=[g2[s][1]])
                hr = Ring(nc, es, "fh", [128, KC, 512], BF16, 3)
                pr = Ring(nc, es, "fps", [128, 2, 512], F32, 2, psum=True)
                po = Ring(nc, es, "fpo", [128, 2, 512], F32, 2, psum=True)
                sr = Ring(nc, es, "fsg", [128, 512], F32, 2)
                ar = Ring(nc, es, "fact", [128, nf, 512], BF16, 2)
                xr = Ring(nc, es, "fx", [128, D], F32, 10 if mode == "moe" else 2)
                x2r = Ring(nc, es, "fx2", [128, D], F32, 2)
                yr = Ring(nc, es, "fy", [128, D], F32, 2)
                pre_acc = (mode == "moe" and not first)

                def load_f(ci):
                    t0 = ci * 512
                    ht, kh = hr.next()
                    S.dma("sp", ht[:], hT[:, :, t0:t0 + 512], writes=[kh])
                    return ht, kh

                def load_x(ci):
                    t0 = ci * 512
                    xs_ = []
                    if pre_acc:
                        for tt in range(4):
                            xt, kx = xr.next()
                            S.dma("sp", xt[:], yacc[t0 + tt * 128:t0 + (tt + 1) * 128, :], writes=[kx])
                            xs_.append((xt, kx))
                    return xs_

                def stage_a(ci, ht, kh):
                    at, ka = ar.next()
                    for f in range(nf):
                        ps, kp = pr.next()
                        for wi, (ww, kw) in enumerate(((wg, kwg), (wu, kwu))):
                            for kc in range(KC):
                                S.op("pe", lambda P, ps=ps, ht=ht, ww=ww, wi=wi, f=f, kc=kc: P.matmul(ps[:, wi, :], ww[:, kc, f * 128:(f + 1) * 128], ht[:, kc, :], start=(kc == 0), stop=(kc == KC - 1)),
                                     reads=[kw, kh], writes=[kp], sig=(wi == 1 and kc == KC - 1))
                        sg, ksg = sr.next()
                        S.op("act", lambda A, sg=sg, ps=ps: A.activation(out=sg[:], in_=ps[:, 0, :], func=AF.Silu), reads=[kp], writes=[ksg])
                        S.op("dve", lambda V, at=at, f=f, sg=sg, ps=ps: V.tensor_tensor(out=at[:, f, :], in0=ps[:, 1, :], in1=sg[:], op=ALU.mult), reads=[kp, ksg], writes=[ka])
                    return at, ka

                def stage_b(ci, at, ka, xs_):
                    t0 = ci * 512
                    s = slot_of(t0)
                    for tt in range(4):
                        r0 = t0 + tt * 128
                        ps, kp = po.next()
                        for h in range(2):
                            for f in range(nf):
                                S.op("pe", lambda P, ps=ps, at=at, tt=tt, f=f, h=h: P.matmul(ps[:, h, :], at[:, f, tt * 128:(tt + 1) * 128], wd[:, f, h * 512:(h + 1) * 512], start=(f == 0), stop=(f == nf - 1)),
                                     reads=[ka, kwd], writes=[kp], sig=(h == 1 and f == nf - 1))
                        psf = ps[:]
                        v3 = lambda t: t[:].rearrange("p (a b) -> p a b", a=2)
                        yt, ky = yr.next()
                        if pre_acc:
                            xt, kx = xs_[tt]
                        else:
                            xt, kx = xr.next()
                        if mode == "dense":
                            S.dma("sp", xt[:], xres[r0:r0 + 128, :], writes=[kx])
                            S.op("dve", lambda V, yt=yt, psf=psf, s=s: V.tensor_tensor(out=v3(yt), in0=psf, in1=v3(g2[s][0]), op=ALU.mult), reads=[kp, g2[s][1]], writes=[ky])
                            S.op("pool", lambda G, yt=yt, xt=xt: G.tensor_tensor(out=yt[:], in0=yt[:], in1=xt[:], op=ALU.add), reads=[ky, kx], writes=[ky])
                            S.dma("sp", xres[r0:r0 + 128, :], yt[:], reads=[ky])
                        else:
                            gcol = gates_t[0][:, r0 // 128, e_idx:e_idx + 1]
                            if first:
                                S.op("dve", lambda V, yt=yt, psf=psf, gcol=gcol: V.tensor_scalar(out=v3(yt), in0=psf, scalar1=gcol, scalar2=None, op0=ALU.mult),
                                     reads=[kp, gates_t[1]], writes=[ky])
                            else:
                                S.op("dve", lambda V, yt=yt, psf=psf, gcol=gcol, xt=xt: V.scalar_tensor_tensor(out=v3(yt), in0=psf, scalar=gcol, in1=v3(xt), op0=ALU.mult, op1=ALU.add),
                                     reads=[kp, gates_t[1], kx], writes=[ky])
                            if last:
                                x2, kx2 = x2r.next()
                                S.dma("sp", x2[:], xres[r0:r0 + 128, :], writes=[kx2])
                                S.op("pool", lambda G, yt=yt, s=s: G.tensor_tensor(out=yt[:], in0=yt[:], in1=g2[s][0][:], op=ALU.mult), reads=[ky, g2[s][1]], writes=[ky])
                                S.op("pool", lambda G, yt=yt, x2=x2: G.tensor_tensor(out=yt[:], in0=yt[:], in1=x2[:], op=ALU.add), reads=[ky, kx2], writes=[ky])
                                S.dma("sp", xres[r0:r0 + 128, :], yt[:], reads=[ky])
                            else:
                                S.dma("sp", yacc[r0:r0 + 128, :], yt[:], reads=[ky])

                ld = {0: load_f(0)}
                if NCH > 1:
                    ld[1] = load_f(1)
                lx = {0: load_x(0)}
                ats = {0: stage_a(0, ld[0][0], ld[0][1])}
                for ci in range(NCH):
                    if ci + 2 < NCH:
                        ld[ci + 2] = load_f(ci + 2)
                    if ci + 1 < NCH:
                        lx[ci + 1] = load_x(ci + 1)
                        ats[ci + 1] = stage_a(ci + 1, ld[ci + 1][0], ld[ci + 1][1])
                    stage_b(ci, ats[ci][0], ats[ci][1], lx[ci])
                S.emit()

        def router_phase(tag, l, gates_t):
            li = l // 2
            with ExitStack() as es:
                S = Sched(nc, es, tag)
                rw, krw = one(nc, es, "rw", [128, KC, NE], F32)
                with nc.allow_non_contiguous_dma("router weights are tiny"):
                    S.dma("sp", rw[:], router_w[li].rearrange("(kc p) e -> p kc e", p=128), writes=[krw])
                idf, kidf = one(nc, es, "idf", [128, 128], F32)
                S.dma("sp", idf[:], cmats[:, 0, :], writes=[kidf])
                xr = Ring(nc, es, "rx", [128, D], F32, 3)
                jr = Ring(nc, es, "rjunk", [128, D], BF16, 1)
                sr = Ring(nc, es, "rss", [128, 4], F32, 4)
                pT = Ring(nc, es, "rT", [128, KC, 128], F32, 2, psum=True)
                hr = Ring(nc, es, "rh", [128, KC, 128], F32, 2)
                pl = Ring(nc, es, "rpl", [128, 512], F32, 2, psum=True)
                lr = Ring(nc, es, "rl", [128, 4, NE], F32, 3)
                mr = Ring(nc, es, "rm", [128, 8], F32, 3)
                gt, kg = gates_t
                for ti in range(T // 128):
                    r0 = ti * 128
                    s = slot_of(r0)
                    xt, kx = xr.next()
                    S.dma("sp", xt[:], xres[r0:r0 + 128, :], writes=[kx])
                    jt, kj = jr.next()
                    st, ks = sr.next()
                    S.op("dve", lambda V, st=st: V.memset(st[:, 0:1], 0.0), writes=[ks])
                    S.op("act", lambda A, jt=jt, xt=xt, st=st: A.activation(out=jt[:], in_=xt[:], func=AF.Square, accum_out=st[:, 0:1]), reads=[kx], writes=[kj, ks])
                    S.op("act", lambda A, st=st: A.activation(out=st[:, 1:2], in_=st[:, 0:1], func=AF.Sqrt, scale=1.0 / D, bias=EPS), reads=[ks], writes=[ks])
                    S.op("dve", lambda V, st=st: V.reciprocal(st[:, 2:3], st[:, 1:2]), reads=[ks], writes=[ks])
                    S.op("pool", lambda G, xt=xt, st=st: G.tensor_scalar(out=xt[:], in0=xt[:], scalar1=st[:, 2:3], scalar2=None, op0=ALU.mult), reads=[kx, ks], writes=[kx])
                    pt, kpt = pT.next()
                    for c in range(KC):
                        S.op("pe", lambda P, pt=pt, xt=xt, c=c: P.transpose(pt[:, c, :], xt[:, c * 128:(c + 1) * 128], idf[:]), reads=[kx, kidf], writes=[kpt], sig=(c == KC - 1))
                    ht, kh = hr.next()
                    for c in range(KC):
                        S.op("dve", lambda V, ht=ht, pt=pt, c=c, s=s: V.tensor_scalar(out=ht[:, c, :], in0=pt[:, c, :], scalar1=modf[:, l, 3, c, s:s + 1], scalar2=modf[:, l, 2, c, s:s + 1],
                                                                                     op0=ALU.mult, op1=ALU.add), reads=[kpt], writes=[kh])
                    lg, klg = pl.next()
                    for kc in range(KC):
                        S.op("pe", lambda P, lg=lg, ht=ht, kc=kc: P.matmul(lg[:, 0:NE], ht[:, kc, :], rw[:, kc, :], start=(kc == 0), stop=(kc == KC - 1)),
                             reads=[kh, krw], writes=[klg], sig=(kc == KC - 1))
                    lt, klt = lr.next()
                    mt, kmt = mr.next()
                    S.op("dve", lambda V, lt=lt, lg=lg: V.tensor_copy(lt[:, 0, :], lg[:, 0:NE]), reads=[klg], writes=[klt])
                    S.op("dve", lambda V, lt=lt, mt=mt: V.tensor_reduce(out=mt[:, 0:1], in_=lt[:, 0, :], axis=AX.X, op=ALU.max), reads=[klt], writes=[kmt])
                    S.op("dve", lambda V, lt=lt, mt=mt: V.tensor_scalar(out=lt[:, 1, :], in0=lt[:, 0, :], scalar1=mt[:, 0:1], scalar2=None, op0=ALU.is_ge), reads=[klt, kmt], writes=[klt])
                    S.op("dve", lambda V, lt=lt: V.scalar_tensor_tensor(out=lt[:, 2, :], in0=lt[:, 1, :], scalar=-1e30, in1=lt[:, 0, :], op0=ALU.mult, op1=ALU.add), reads=[klt], writes=[klt])
                    S.op("dve", lambda V, lt=lt, mt=mt: V.tensor_reduce(out=mt[:, 1:2], in_=lt[:, 2, :], axis=AX.X, op=ALU.max), reads=[klt], writes=[kmt])
                    S.op("dve", lambda V, lt=lt, mt=mt: V.tensor_scalar(out=lt[:, 3, :], in0=lt[:, 0, :], scalar1=mt[:, 1:2], scalar2=None, op0=ALU.is_ge), reads=[klt, kmt], writes=[klt])
                    S.op("dve", lambda V, mt=mt: V.tensor_scalar(out=mt[:, 2:3], in0=mt[:, 0:1], scalar1=-1.0, scalar2=None, op0=ALU.mult), reads=[kmt], writes=[kmt])
                    S.op("act", lambda A, lt=lt, mt=mt: A.activation(out=lt[:, 1, :], in_=lt[:, 0, :], func=AF.Exp, bias=mt[:, 2:3], scale=1.0), reads=[klt, kmt], writes=[klt])
                    S.op("dve", lambda V, lt=lt: V.tensor_tensor(out=lt[:, 2, :], in0=lt[:, 1, :], in1=lt[:, 3, :], op=ALU.mult), reads=[klt], writes=[klt])
                    S.op("dve", lambda V, lt=lt, mt=mt: V.tensor_reduce(out=mt[:, 3:4], in_=lt[:, 2, :], axis=AX.X, op=ALU.add), reads=[klt], writes=[kmt])
                    S.op("dve", lambda V, mt=mt: V.reciprocal(mt[:, 4:5], mt[:, 3:4]), reads=[kmt], writes=[kmt])
                    S.op("dve", lambda V, lt=lt, mt=mt, ti=ti: V.tensor_scalar(out=gt[:, ti, :], in0=lt[:, 2, :], scalar1=mt[:, 4:5], scalar2=None, op0=ALU.mult), reads=[klt, kmt], writes=[kg])
                S.emit()

        def final_phase(tag, src):
            with ExitStack() as es:
                S = Sched(nc, es, tag)
                fb, kfb = one(nc, es, "fnb", [128, D], F32)
                S.dma("sp", fb[:], final_norm.partition_broadcast(128), writes=[kfb])
                xr = Ring(nc, es, "zx", [128, D], F32, 3)
                jr = Ring(nc, es, "zjunk", [128, D], BF16, 1)
                sr = Ring(nc, es, "zss", [128, 4], F32, 4)
                for ti in range(T // 128):
                    r0 = ti * 128
                    xt, kx = xr.next()
                    S.dma("sp", xt[:], src[r0:r0 + 128, :], writes=[kx])
                    jt, kj = jr.next()
                    st, ks = sr.next()
                    S.op("dve", lambda V, st=st: V.memset(st[:, 0:1], 0.0), writes=[ks])
                    S.op("act", lambda A, jt=jt, xt=xt, st=st: A.activation(out=jt[:], in_=xt[:], func=AF.Square, accum_out=st[:, 0:1]), reads=[kx], writes=[kj, ks])
                    S.op("act", lambda A, st=st: A.activation(out=st[:, 1:2], in_=st[:, 0:1], func=AF.Sqrt, scale=1.0 / D, bias=EPS), reads=[ks], writes=[ks])
                    S.op("dve", lambda V, st=st: V.reciprocal(st[:, 2:3], st[:, 1:2]), reads=[ks], writes=[ks])
                    S.op("dve", lambda V, xt=xt, st=st: V.scalar_tensor_tensor(out=xt[:], in0=xt[:], scalar=st[:, 2:3], in1=fb[:], op0=ALU.mult, op1=ALU.mult), reads=[kx, ks, kfb], writes=[kx])
                    S.dma("sp", y_out[r0:r0 + 128, :], xt[:], reads=[kx])
                S.emit()

        gates_tile = gs.enter_context(nc.sbuf_tensor("gatesw", [128, T // 128, NE], F32))
        gates_t = (gates_tile, Trk())
        for l in range(L):
            src = x_in if l == 0 else xres
            norm_phase(f"n{l}a", src, l, 0, 1)
            proj_phase(f"p{l}", l)
            attn_phase(f"a{l}", l)
            outproj_phase(f"o{l}", l, src)
            norm_phase(f"n{l}b", xres, l, 2, 3)
            if l % 2 == 0:
                li = l // 2
                nf_all = DFF // 128
                halves = [(0, nf_all // 2), (nf_all // 2, nf_all - nf_all // 2)]
                for hi, (fs, nf) in enumerate(halves):
                    ffn_pass(f"f{l}_{hi}", l, ffn_w_gate[li], ffn_w_up[li], ffn_w_down[li], fs * 128, nf, "dense", 0, None, False, False)
            else:
                li = l // 2
                gates_t = (gates_tile, Trk())
                router_phase(f"r{l}", l, gates_t)
                nf_all = DFE // 128
                npart = 4 if nf_all % 4 == 0 else 2
                halves = [(i * (nf_all // npart), nf_all // npart) for i in range(npart)]
                for e in range(NE):
                    for hi, (fs, nf) in enumerate(halves):
                        first = (e == 0 and hi == 0)
                        last = (e == NE - 1 and hi == len(halves) - 1)
                        ffn_pass(f"m{l}_{e}_{hi}", l, moe_w_gate[li, e], moe_w_up[li, e], moe_w_down[li, e], fs * 128, nf, "moe", e,
                                 (gates_tile, Trk()), first, last)
        final_phase("fin", xres)
    return nc


def _consts():
    ident = np.eye(128, dtype=np.float32)
    pd = np.zeros((128, 128), np.float32)
    pg = np.zeros((128, 128), np.float32)
    for b in range(2):
        o = b * 64
        for i in range(8):
            pd[o + i + 8, o + i] = 1.0
            pd[o + i, o + i + 8] = 1.0
        for base in (0, 32):
            for i in range(16):
                pg[o + base + i + 16, o + base + i] = 1.0
                pg[o + base + i, o + base + i + 16] = 1.0
    bones = np.zeros((128, 128), np.float32)
    bones[0:64, 0:64] = 1.0
    bones[64:128, 64:128] = 1.0
    ones = np.ones((128, 128), np.float32)
    return np.ascontiguousarray(np.stack([ident, pd, pg, bones, ones], axis=1))


def _rope_tables(pos):
    pos = np.asarray(pos)
    n = pos.shape[0]
    tab = np.zeros((4, 128, n), np.float32)
    tab[0] = 1.0
    posf = pos.astype(np.float32)
    rowf = (pos // GRID_W).astype(np.float32)
    colf = (pos % GRID_W).astype(np.float32)
    invd = np.exp(-math.log(DIFF_THETA) * np.arange(8, dtype=np.float32) / 8).astype(np.float32)
    invg = np.exp(-math.log(AX_THETA) * np.arange(16, dtype=np.float32) / 16).astype(np.float32)
    for p in range(128):
        d = p % 64
        if d < 16:
            ang = posf * invd[d % 8]
            tab[0, p] = np.cos(ang)
            tab[1, p] = -np.sin(ang) if d < 8 else np.sin(ang)
        if d < 32:
            ang = rowf * invg[d % 16]
            sgn = -1.0 if d < 16 else 1.0
        else:
            ang = colf * invg[(d - 32) % 16]
            sgn = -1.0 if d < 48 else 1.0
        tab[2, p] = np.cos(ang)
        tab[3, p] = sgn * np.sin(ang)
    return tab


_WNAMES = ["w_ada", "b_ada", "norm1", "w_in", "diff_lambda", "diff_subln", "gqa_q_norm", "gqa_k_norm", "w_out", "norm2",
           "ffn_w_gate", "ffn_w_up", "ffn_w_down", "router_w", "moe_w_gate", "moe_w_up", "moe_w_down", "final_norm"]


def kernel(**inputs):
    xp = np.asarray(inputs["x_prompt"], np.float32)
    xs = np.asarray(inputs["x_sample"], np.float32)
    cp = np.asarray(inputs["c_prompt"], np.float32)
    cs = np.asarray(inputs["c_sample"], np.float32)
    B1, S1, _ = xp.shape
    B2, S2, _ = xs.shape
    assert S2 == 2 * S1 and B1 % 2 == 0
    L = inputs["w_in"].shape[0]
    DFF = inputs["ffn_w_gate"].shape[2]
    DFE = inputs["moe_w_gate"].shape[3]
    n = 8
    T = S2
    npair = B1 // 2
    assert B2 + npair <= n
    cfg = Cfg(L, T, DFF, DFE)
    nc = build(cfg)
    cm = _consts()
    tab_s = np.ascontiguousarray(_rope_tables(np.arange(T)))
    tab_p = np.ascontiguousarray(_rope_tables(np.concatenate([np.arange(S1), np.arange(S1)])))
    nkt, nqc = T // 128, T // 512
    mb_s = np.zeros((128, nkt * nqc), np.float32)
    mb_p = np.zeros((nkt, nqc), np.float32)
    for kt in range(nkt):
        for qc in range(nqc):
            if (kt * 128) // S1 != (qc * 512) // S1:
                mb_p[kt, qc] = -30000.0
    mb_p = np.ascontiguousarray(np.broadcast_to(mb_p.reshape(1, -1), (128, nkt * nqc)))
    wts = {k: np.ascontiguousarray(np.asarray(inputs[k], np.float32)) for k in _WNAMES}
    in_maps = []
    for c in range(n):
        if c < B2:
            m = {"x": np.ascontiguousarray(xs[c]), "cvec": np.ascontiguousarray(np.stack([cs[c], cs[c]], axis=0)),
                 "tabs": tab_s, "maskb": mb_s}
        else:
            i = min(c - B2, npair - 1)
            m = {"x": np.ascontiguousarray(np.concatenate([xp[2 * i], xp[2 * i + 1]], axis=0)),
                 "cvec": np.ascontiguousarray(np.stack([cp[2 * i], cp[2 * i + 1]], axis=0)),
                 "tabs": tab_p, "maskb": mb_p}
        m["cmats"] = cm
        m.update(wts)
        in_maps.append(m)
    res = run_bass_kernel_spmd(nc, in_maps, core_ids=list(range(n)))
    outs = [np.asarray(r["y"], np.float32) for r in res.results]
    y_sample = np.stack([outs[b] for b in range(B2)], axis=0)
    y_prompt = np.stack([outs[B2 + j // 2][(j % 2) * S1:(j % 2 + 1) * S1] for j in range(B1)], axis=0)
    return (y_prompt, y_sample)
```
